# Optimizing a Trainium2 kernel written in Bass

```python
import math
import jax, jax.numpy as jnp
from jax import lax
import numpy as np

D_MODEL = 1024
BATCH = 8
SEQ = 4096
DEPTH = 1

CHUNK = 64
N_META = 16
EPS = 1e-6
SSM_WIDTH = D_MODEL // 4
SSM_GROUP = 16
SSM_GROUPS = SSM_WIDTH // SSM_GROUP
SSM_STATE = 64
DT_MIN = 1e-3
DT_MAX = 1e-1
RET_WIDTH = D_MODEL - SSM_WIDTH
RET_HEAD_DIM = 128
RET_HEADS = RET_WIDTH // RET_HEAD_DIM
ROPE_BASE = 10000.0
IN_WIDTH = SSM_WIDTH + 4 * RET_WIDTH
N_GROUPS = 4
EXPERTS_PER_GROUP = 4
N_EXPERTS = N_GROUPS * EXPERTS_PER_GROUP
TOP_K_INNER = 2
EXPERT_FF = D_MODEL // 4

kernel_name = "hymba_s5_retention_hiermoe_streaming"


def rms_norm(x, g):
    xf = x.astype(jnp.float32)
    y = xf * lax.rsqrt(jnp.mean(xf * xf, axis=-1, keepdims=True) + EPS)
    return (y * g.astype(jnp.float32)).astype(x.dtype)


def s5_mixer(u, lam_re, lam_im, log_dt, b_re, b_im, c_re, c_im, d_skip, w_glu):
    bsz, L, _ = u.shape
    f32 = jnp.float32
    uf = u.astype(f32).reshape(bsz, L, SSM_GROUPS, SSM_GROUP)
    lam = lax.complex(lam_re.astype(f32), lam_im.astype(f32))
    dt = jnp.exp(log_dt.astype(f32))[:, None]
    lam_bar = jnp.exp(lam * dt)
    b = lax.complex(b_re.astype(f32), b_im.astype(f32))
    b_bar = ((lam_bar - 1.0) / lam)[..., None] * b
    bu = jnp.einsum('blgh,gph->lbgp', uf.astype(jnp.complex64), b_bar)
    a = jnp.broadcast_to(lam_bar, (L, 1) + lam_bar.shape)

    def combine(e1, e2):
        a1, x1 = e1
        a2, x2 = e2
        return a1 * a2, a2 * x1 + x2

    _, states = lax.associative_scan(combine, (a, bu), axis=0)
    c = lax.complex(c_re.astype(f32), c_im.astype(f32))
    y = jnp.real(jnp.einsum('lbgp,ghp->blgh', states, c)) + d_skip.astype(f32) * uf
    y = jax.nn.gelu(y.reshape(bsz, L, SSM_WIDTH))
    y = y * jax.nn.sigmoid(y @ w_glu.astype(f32))
    return y.astype(u.dtype)


def retention_mixer(q, k, v, gate):
    bsz, L, _ = q.shape
    f32 = jnp.float32
    pos = jnp.arange(L, dtype=f32)
    inv_freq = ROPE_BASE ** (-jnp.arange(0, RET_HEAD_DIM, 2, dtype=f32) / RET_HEAD_DIM)
    ang = pos[:, None] * inv_freq[None, :]
    ang = jnp.concatenate([ang, ang], axis=-1)[:, None, :]
    cos, sin = jnp.cos(ang), jnp.sin(ang)

    def heads(t):
        return t.astype(f32).reshape(bsz, L, RET_HEADS, RET_HEAD_DIM)

    def rope(t):
        t1, t2 = jnp.split(t, 2, axis=-1)
        return t * cos + jnp.concatenate([-t2, t1], axis=-1) * sin

    qh = rope(heads(q))
    kh = rope(heads(k)) * (RET_HEAD_DIM ** -0.5)
    vh = heads(v)

    pad = CHUNK - N_META
    n_chunks = (L + pad) // CHUNK

    def chunks(t):
        t = jnp.pad(t, ((0, 0), (pad, 0), (0, 0), (0, 0)))
        return t.reshape(bsz, n_chunks, CHUNK, RET_HEADS, RET_HEAD_DIM)

    qc, kc, vc = chunks(qh), chunks(kh), chunks(vh)
    gamma = 1.0 - 2.0 ** (-5.0 - jnp.arange(RET_HEADS, dtype=f32))
    log_g = jnp.log(gamma)
    idx = jnp.arange(CHUNK, dtype=f32)
    intra_decay = jnp.exp(log_g[:, None, None] * jnp.abs(idx[:, None] - idx[None, :]))
    scores = jnp.einsum('bnihd,bnjhd->bnhij', qc, kc) * intra_decay
    intra = jnp.einsum('bnhij,bnjhd->bnihd', scores, vc)
    k_decay = jnp.exp(log_g[None, :] * (CHUNK - 1.0 - idx)[:, None])
    kv = jnp.einsum('bnchd,bnche->nbhde', kc * k_decay[:, :, None], vc)
    chunk_decay = jnp.exp(log_g * CHUNK)[None, :, None, None]

    def step(state, kv_n):
        return state * chunk_decay + kv_n, state

    init = jnp.zeros((bsz, RET_HEADS, RET_HEAD_DIM, RET_HEAD_DIM), f32)
    _, r_prev = lax.scan(step, init, kv)
    q_decay = jnp.exp(log_g[None, :] * (idx + 1.0)[:, None])
    cross = jnp.einsum('bnchd,nbhde->bnche', qc * q_decay[:, :, None], r_prev)
    o = (intra + cross).reshape(bsz, n_chunks * CHUNK, RET_HEADS, RET_HEAD_DIM)[:, pad:]
    mu = jnp.mean(o, axis=-1, keepdims=True)
    var = jnp.mean(jnp.square(o - mu), axis=-1, keepdims=True)
    o = ((o - mu) * lax.rsqrt(var + EPS)).reshape(bsz, L, RET_WIDTH)
    return (jax.nn.silu(gate.astype(f32)) * o).astype(q.dtype)


def hier_moe(t, w_rg, b_rg, w_re, b_re, w_gate, w_up, w_down):
    bsz, L, d = t.shape
    f32 = jnp.float32
    tf = t.reshape(-1, d)
    p_g = jax.nn.softmax((tf @ w_rg + b_rg).astype(f32), axis=-1)
    g_w, g_idx = lax.top_k(p_g, 1)
    logits_all = jnp.einsum('td,gde->tge', tf, w_re) + b_re
    logits_e = jnp.take_along_axis(logits_all, g_idx[:, :, None], axis=1)[:, 0].astype(f32)
    e_val, e_idx = lax.top_k(logits_e, TOP_K_INNER)
    e_w = jax.nn.softmax(e_val, axis=-1)
    within = jnp.einsum('tk,tke->te', e_w, jax.nn.one_hot(e_idx, EXPERTS_PER_GROUP, dtype=f32))
    group_sel = jax.nn.one_hot(g_idx[:, 0], N_GROUPS, dtype=f32) * g_w
    combine = (group_sel[:, :, None] * within[:, None, :]).reshape(-1, N_EXPERTS)
    hg = jnp.einsum('td,edf->tef', tf, w_gate)
    hu = jnp.einsum('td,edf->tef', tf, w_up)
    act = jax.nn.silu(hg) * hu * combine.astype(hg.dtype)[:, :, None]
    out = jnp.einsum('tef,efd->td', act, w_down)
    return out.reshape(bsz, L, d).astype(t.dtype)


def setup_inputs(seed: int = 0) -> dict:
    key = jax.random.key(seed)
    ks = jax.random.split(key, 24)
    f32 = jnp.float32
    n = lambda k, s, sc: jax.random.normal(k, s, f32) * sc
    G, P, H = SSM_GROUPS, SSM_STATE, SSM_GROUP
    lam_re = -0.5 + 0.01 * jax.random.normal(ks[3], (DEPTH, G, P), f32)
    lam_im = math.pi * jnp.arange(P, dtype=f32)[None, None, :] + 0.01 * jax.random.normal(ks[4], (DEPTH, G, P), f32)
    log_dt = jax.random.uniform(ks[5], (DEPTH, G), f32, math.log(DT_MIN), math.log(DT_MAX))
    return {
        "x": n(ks[0], (BATCH, SEQ, D_MODEL), 1.0),
        "meta_tokens": n(ks[1], (N_META, D_MODEL), 1.0),
        "norm_mix_g": 1.0 + n(ks[2], (DEPTH, D_MODEL), 0.02),
        "w_in": n(ks[6], (DEPTH, D_MODEL, IN_WIDTH), D_MODEL ** -0.5),
        "ssm_lambda_re": lam_re,
        "ssm_lambda_im": lam_im,
        "ssm_log_dt": log_dt,
        "ssm_b_re": n(ks[7], (DEPTH, G, P, H), (2 * H) ** -0.5),
        "ssm_b_im": n(ks[8], (DEPTH, G, P, H), (2 * H) ** -0.5),
        "ssm_c_re": n(ks[9], (DEPTH, G, H, P), (2 * P) ** -0.5),
        "ssm_c_im": n(ks[10], (DEPTH, G, H, P), (2 * P) ** -0.5),
        "ssm_d": n(ks[11], (DEPTH, G, H), 1.0),
        "w_glu": n(ks[12], (DEPTH, SSM_WIDTH, SSM_WIDTH), SSM_WIDTH ** -0.5),
        "w_out": n(ks[13], (DEPTH, D_MODEL, D_MODEL), D_MODEL ** -0.5),
        "norm_ffn_g": 1.0 + n(ks[14], (DEPTH, D_MODEL), 0.02),
        "w_router_group": n(ks[15], (DEPTH, D_MODEL, N_GROUPS), D_MODEL ** -0.5),
        "b_router_group": n(ks[16], (DEPTH, N_GROUPS), 0.01),
        "w_router_expert": n(ks[17], (DEPTH, N_GROUPS, D_MODEL, EXPERTS_PER_GROUP), D_MODEL ** -0.5),
        "b_router_expert": n(ks[18], (DEPTH, N_GROUPS, EXPERTS_PER_GROUP), 0.01),
        "w_gate": n(ks[19], (DEPTH, N_EXPERTS, D_MODEL, EXPERT_FF), D_MODEL ** -0.5),
        "w_up": n(ks[20], (DEPTH, N_EXPERTS, D_MODEL, EXPERT_FF), D_MODEL ** -0.5),
        "w_down": n(ks[21], (DEPTH, N_EXPERTS, EXPERT_FF, D_MODEL), EXPERT_FF ** -0.5),
        "norm_final_g": 1.0 + n(ks[22], (D_MODEL,), 0.02),
    }


def reference(x, meta_tokens, norm_mix_g, w_in, ssm_lambda_re, ssm_lambda_im, ssm_log_dt,
              ssm_b_re, ssm_b_im, ssm_c_re, ssm_c_im, ssm_d, w_glu, w_out, norm_ffn_g,
              w_router_group, b_router_group, w_router_expert, b_router_expert,
              w_gate, w_up, w_down, norm_final_g):
    bsz = x.shape[0]
    meta = jnp.broadcast_to(meta_tokens[None].astype(x.dtype), (bsz, N_META, D_MODEL))
    h = jnp.concatenate([meta, x], axis=1)
    splits = [SSM_WIDTH, SSM_WIDTH + RET_WIDTH, SSM_WIDTH + 2 * RET_WIDTH, SSM_WIDTH + 3 * RET_WIDTH]
    for layer in range(DEPTH):
        a = rms_norm(h, norm_mix_g[layer])
        z = a @ w_in[layer]
        u, q, k, v, g = jnp.split(z, splits, axis=-1)
        y_ssm = s5_mixer(u, ssm_lambda_re[layer], ssm_lambda_im[layer], ssm_log_dt[layer],
                         ssm_b_re[layer], ssm_b_im[layer], ssm_c_re[layer], ssm_c_im[layer],
                         ssm_d[layer], w_glu[layer])
        y_ret = retention_mixer(q, k, v, g)
        mixed = jnp.concatenate([y_ssm, y_ret], axis=-1)
        h = h + (mixed @ w_out[layer]).astype(h.dtype)
        h = h + hier_moe(rms_norm(h, norm_ffn_g[layer]), w_router_group[layer], b_router_group[layer],
                         w_router_expert[layer], b_router_expert[layer],
                         w_gate[layer], w_up[layer], w_down[layer])
    return rms_norm(h, norm_final_g)[:, N_META:]
```

```python
import numpy as np
from contextlib import ExitStack
import concourse.bass as bass
import concourse.mybir as mybir
from concourse.bass_utils import run_bass_kernel_spmd

F32 = mybir.dt.float32
BF16 = mybir.dt.bfloat16
AF = mybir.ActivationFunctionType
ALU = mybir.AluOpType
AX = mybir.AxisListType

D = 1024
SEQ = 4096
NT = 33
LP = NT * 128
EPS = 1e-6
NH = 6
NE = 16
INW = 3328
TB = 512
NB = (LP + TB - 1) // TB
TS = 256
NBS = (LP + TS - 1) // TS
GELU_C = 1.5957691216057308
SAME_ENG_WAR = True


class Buf:
    def __init__(self, name, t):
        self.name = name
        self.t = t
        self.lw = None
        self.rd = []
        self.sem = None
        self.nd = 0

    def __getitem__(self, k):
        return self.t[k]


class Op:
    __slots__ = ("eng", "fn", "deps", "sig", "cnt", "epoch", "is_dma", "dbuf", "dcnt")


class Prog:
    ENG = ["pe", "act", "dve", "pool", "sp"]

    NPROG = [0]
    TOT = [0]
    PH = {}

    def __init__(self, nc, es, semes):
        Prog.NPROG[0] += 1
        self.tag = "g%d" % Prog.NPROG[0]
        self.nc = nc
        self.es = es
        self.semes = semes
        self.ops = {e: [] for e in self.ENG}
        self.epoch = 0
        self.sems = {}
        self.out_dmas = []

    def buf(self, name, shape, dt, ps=False):
        if not ps:
            nb = int(np.prod(shape[1:])) * (2 if dt == BF16 else 4)
            Prog.TOT[0] += nb
            Prog.PH[self.tag] = Prog.PH.get(self.tag, 0) + nb
        if ps:
            t = self.es.enter_context(self.nc.psum_tensor(self.tag + name, shape, dt))
        else:
            t = self.es.enter_context(self.nc.sbuf_tensor(self.tag + name, shape, dt))
        return Buf(name, t)

    def new_epoch(self):
        self.epoch += 1

    def _mk(self, eng, fn, reads, writes):
        op = Op()
        op.eng = eng
        op.fn = fn
        op.deps = set()
        op.sig = False
        op.cnt = 0
        op.epoch = self.epoch
        op.is_dma = False
        op.dbuf = None
        op.dcnt = 0
        for b in reads:
            if b.lw is not None:
                op.deps.add(b.lw)
        for b in writes:
            if b.lw is not None:
                op.deps.add(b.lw)
            for r in b.rd:
                if SAME_ENG_WAR or r.is_dma or r.eng != eng:
                    op.deps.add(r)
        for b in reads:
            b.rd.append(op)
        for b in writes:
            b.lw = op
            b.rd = []
        self.ops[eng].append(op)
        return op

    def op(self, eng, fn, reads=(), writes=()):
        op = self._mk(eng, fn, reads, writes)
        if eng == "pe":
            op.deps = {d for d in op.deps if d.is_dma or d.eng != "pe"}
        return op

    def dma(self, eng, out, in_, reads=(), writes=(), sigbuf=None):
        def fn(e, out=out, in_=in_):
            return e.dma_start(out=out, in_=in_)
        op = self._mk(eng, fn, reads, writes)
        op.is_dma = True
        b = sigbuf if sigbuf is not None else (writes[0] if writes else reads[0])
        if b.sem is None:
            b.sem = self.semes.enter_context(self.nc.semaphore(self.tag + "d_" + b.name))
        b.nd += 1
        op.dbuf = b
        op.dcnt = b.nd
        return op

    def emit(self, final_waits=()):
        nc = self.nc
        allops = [o for e in self.ENG for o in self.ops[e]]
        for o in allops:
            for d in o.deps:
                if not d.is_dma:
                    d.sig = True
        for e in self.ENG:
            cnts = {}
            for o in self.ops[e]:
                if o.sig and not o.is_dma:
                    cnts[o.epoch] = cnts.get(o.epoch, 0) + 1
                    o.cnt = cnts[o.epoch]
                    key = (e, o.epoch)
                    if key not in self.sems:
                        self.sems[key] = self.semes.enter_context(nc.semaphore(self.tag + "s_%s_%d" % key))
        prog = self

        def run(eng_name, e):
            waited = {}
            for o in prog.ops[eng_name]:
                need = {}
                for d in o.deps:
                    if d.is_dma:
                        sem, val = d.dbuf.sem, 16 * d.dcnt
                    else:
                        sem, val = prog.sems[(d.eng, d.epoch)], d.cnt
                    k = id(sem)
                    if k not in need or need[k][1] < val:
                        need[k] = (sem, val)
                for k, (sem, val) in need.items():
                    if waited.get(k, 0) >= val:
                        continue
                    e.wait_ge(sem, val)
                    waited[k] = val
                ins = o.fn(e)
                if o.is_dma:
                    ins.then_inc(o.dbuf.sem, 16)
                elif o.sig:
                    ins.then_inc(prog.sems[(o.eng, o.epoch)], 1)
            if eng_name == "sp":
                for b in final_waits:
                    if b.sem is not None:
                        e.wait_ge(b.sem, 16 * b.nd)

        with nc.Block() as block:
            @block.tensor
            def _(e):
                run("pe", e)

            @block.scalar
            def _(e):
                run("act", e)

            @block.vector
            def _(e):
                run("dve", e)

            @block.gpsimd
            def _(e):
                run("pool", e)

            @block.sync
            def _(e):
                run("sp", e)


def bc(ap, shape):
    return ap.to_broadcast(shape)


def build_nc():
    nc = bass.Bass("TRN2", target_bir_lowering=False)

    def din(name, shape, dt=F32):
        return nc.dram_tensor(name, list(shape), dt, kind="ExternalInput").ap()

    x = din("x", [SEQ, D])
    meta = din("meta", [16, D])
    gmix = din("gmix", [128, 8])
    gffn = din("gffn", [128, 8])
    gfin = din("gfin", [128, D])
    w_in = din("w_in", [D, INW])
    w_out = din("w_out", [D, D])
    w_glu = din("w_glu", [256, 256])
    w_gate = din("w_gate", [NE, D, 256])
    w_up = din("w_up", [NE, D, 256])
    w_down = din("w_down", [NE, 256, D])
    wr = din("wr", [D, 20])
    br = din("br", [128, 20])
    lre = din("lre", [128, 8])
    lim = din("lim", [128, 8])
    ldt = din("ldt", [128, 8])
    Bl = din("Bl", [128, 2048])
    Cl = din("Cl", [128, 2048])
    Dl = din("Dl", [128, 256])
    cosT = din("cosT", [128, LP])
    sinT = din("sinT", [128, LP])
    maskT = din("maskT", [128, NH * 128])
    qdec = din("qdec", [128, NH])
    kdec = din("kdec", [128, NH])
    identf = din("identf", [128, 128])
    out = nc.dram_tensor("out", [SEQ, D], F32, kind="ExternalOutput").ap()
    sc_gate = nc.dram_tensor("sc_gate", [NE, 128, 2048], BF16).ap()
    sc_up = nc.dram_tensor("sc_up", [NE, 128, 2048], BF16).ap()
    sc_down = nc.dram_tensor("sc_down", [NE, 128, 2048], BF16).ap()

    gamma = [1.0 - 2.0 ** (-5.0 - h) for h in range(NH)]
    g128 = [float(g ** 128) for g in gamma]

    with ExitStack() as ges, ExitStack() as semes:
        G = Prog(nc, ges, semes)
        mixedT = G.buf("mixedT", [128, 8, LP], BF16)
        ident = G.buf("ident", [128, 128], BF16)
        gmix_s = G.buf("gmix_s", [128, 8], F32)
        gffn_s = G.buf("gffn_s", [128, 8], F32)
        rho = G.buf("rho", [128, 8], F32)
        qre = G.buf("qre", [128, 8], F32)
        qim = G.buf("qim", [128, 8], F32)
        ucs = G.buf("ucs", [128, 2, 8], F32)
        Bl_s = G.buf("Bl_s", [128, 2048], BF16)
        Cl_s = G.buf("Cl_s", [128, 2048], BF16)
        Dl_s = G.buf("Dl_s", [128, 256], BF16)
        wglu_s = G.buf("wglu_s", [128, 2, 256], BF16)
        mxb = [Buf("mx%d" % t, None) for t in range(NT)]

        with ExitStack() as es:
            P = Prog(nc, es, semes)
            idf = P.buf("idf", [128, 128], F32)
            P.dma("sp", idf[:], identf[:, :], writes=[idf])
            P.op("act", lambda e: e.activation(ident[:], idf[:], AF.Copy), [idf], [ident])
            P.dma("sp", gmix_s[:], gmix[:, :], writes=[gmix_s])
            P.dma("sp", gffn_s[:], gffn[:, :], writes=[gffn_s])
            stg = [P.buf("stg%d" % i, [128, 2048], F32) for i in range(2)]
            P.dma("sp", stg[0][:], Bl[:, :], writes=[stg[0]])
            P.op("act", lambda e: e.activation(Bl_s[:], stg[0][:], AF.Copy), [stg[0]], [Bl_s])
            P.dma("sp", stg[1][:], Cl[:, :], writes=[stg[1]])
            P.dma("sp", stg[0][:, 0:256], Dl[:, :], writes=[stg[0]])
            P.op("act", lambda e: e.activation(Dl_s[:], stg[0][:, 0:256], AF.Copy), [stg[0]], [Dl_s])
            P.dma("sp", stg[0][:, 512:1024].rearrange("p (c f) -> p c f", c=2),
                  w_glu.rearrange("(c p) f -> p c f", p=128), writes=[stg[0]])
            P.op("dve", lambda e: e.tensor_copy(wglu_s[:], stg[0][:, 512:1024].rearrange("p (c f) -> p c f", c=2)),
                 [stg[0]], [wglu_s])
            lre_s = P.buf("lre_s", [128, 8], F32)
            lim_s = P.buf("lim_s", [128, 8], F32)
            dt_s = P.buf("dt_s", [128, 8], F32)
            P.dma("sp", lre_s[:], lre[:, :], writes=[lre_s])
            P.dma("sp", lim_s[:], lim[:, :], writes=[lim_s])
            P.dma("sp", dt_s[:], ldt[:, :], writes=[dt_s])
            P.op("act", lambda e: e.activation(dt_s[:], dt_s[:], AF.Exp), [dt_s], [dt_s])
            tmpa = P.buf("tmpa", [128, 8], F32)
            tmpb = P.buf("tmpb", [128, 8], F32)
            th = P.buf("th", [128, 8], F32)
            P.op("dve", lambda e: e.tensor_tensor(tmpa[:], lre_s[:], dt_s[:], ALU.mult), [lre_s, dt_s], [tmpa])
            P.op("act", lambda e: e.activation(rho[:], tmpa[:], AF.Exp), [tmpa], [rho])
            P.op("dve", lambda e: e.tensor_tensor(th[:], lim_s[:], dt_s[:], ALU.mult), [lim_s, dt_s], [th])
            cc = P.buf("cc", [128, 8], F32)
            ss = P.buf("ss", [128, 8], F32)
            hp = P.buf("hp", [128, 1], F32)
            P.op("dve", lambda e: e.memset(hp[:], float(np.pi / 2)), [], [hp])
            P.op("act", lambda e: e.activation(ss[:], th[:], AF.Sin, scale=1.0 / 32), [th], [ss])
            P.op("act", lambda e: e.activation(cc[:], th[:], AF.Sin, scale=1.0 / 32, bias=hp[:, 0:1]), [th, hp], [cc])
            c2 = P.buf("c2", [128, 8], F32)
            s2 = P.buf("s2", [128, 8], F32)
            for it in range(5):
                P.op("dve", lambda e: e.tensor_tensor(c2[:], cc[:], cc[:], ALU.mult), [cc], [c2])
                P.op("dve", lambda e: e.tensor_tensor(s2[:], ss[:], ss[:], ALU.mult), [ss], [s2])
                P.op("dve", lambda e: e.tensor_tensor(tmpb[:], cc[:], ss[:], ALU.mult), [cc, ss], [tmpb])
                P.op("dve", lambda e: e.tensor_tensor(cc[:], c2[:], s2[:], ALU.subtract), [c2, s2], [cc])
                P.op("dve", lambda e: e.tensor_scalar(ss[:], tmpb[:], 2.0, None, ALU.mult), [tmpb], [ss])
            P.op("dve", lambda e: e.tensor_copy(ucs[:, 0, :], cc[:]), [cc], [ucs])
            P.op("dve", lambda e: e.tensor_copy(ucs[:, 1, :], ss[:]), [ss, ucs], [ucs])
            nre = P.buf("nre", [128, 8], F32)
            nim = P.buf("nim", [128, 8], F32)
            den = P.buf("den", [128, 8], F32)
            P.op("dve", lambda e: e.tensor_tensor(nre[:], rho[:], cc[:], ALU.mult), [rho, cc], [nre])
            P.op("dve", lambda e: e.tensor_scalar(nre[:], nre[:], -1.0, None, ALU.add), [nre], [nre])
            P.op("dve", lambda e: e.tensor_tensor(nim[:], rho[:], ss[:], ALU.mult), [rho, ss], [nim])
            P.op("dve", lambda e: e.tensor_tensor(den[:], lre_s[:], lre_s[:], ALU.mult), [lre_s], [den])
            P.op("dve", lambda e: e.tensor_tensor(tmpa[:], lim_s[:], lim_s[:], ALU.mult), [lim_s], [tmpa])
            P.op("dve", lambda e: e.tensor_tensor(den[:], den[:], tmpa[:], ALU.add), [den, tmpa], [den])
            P.op("dve", lambda e: e.reciprocal(den[:], den[:]), [den], [den])
            P.op("dve", lambda e: e.tensor_tensor(tmpa[:], nre[:], lre_s[:], ALU.mult), [nre, lre_s], [tmpa])
            P.op("dve", lambda e: e.tensor_tensor(tmpb[:], nim[:], lim_s[:], ALU.mult), [nim, lim_s], [tmpb])
            P.op("dve", lambda e: e.tensor_tensor(tmpa[:], tmpa[:], tmpb[:], ALU.add), [tmpa, tmpb], [tmpa])
            P.op("dve", lambda e: e.tensor_tensor(qre[:], tmpa[:], den[:], ALU.mult), [tmpa, den], [qre])
            P.op("dve", lambda e: e.tensor_tensor(tmpa[:], nim[:], lre_s[:], ALU.mult), [nim, lre_s], [tmpa])
            P.op("dve", lambda e: e.tensor_tensor(tmpb[:], nre[:], lim_s[:], ALU.mult), [nre, lim_s], [tmpb])
            P.op("dve", lambda e: e.tensor_tensor(tmpa[:], tmpa[:], tmpb[:], ALU.subtract), [tmpa, tmpb], [tmpa])
            P.op("dve", lambda e: e.tensor_tensor(qim[:], tmpa[:], den[:], ALU.mult), [tmpa, den], [qim])
            nqim = P.buf("nqim", [128, 8], F32)
            tq = P.buf("tq", [128, 128], F32)
            P.op("dve", lambda e: e.tensor_scalar(nqim[:], qim[:], -1.0, None, ALU.mult), [qim], [nqim])
            for k in range(8):
                cre_ = slice((2 * k) * 128, (2 * k + 1) * 128)
                cim_ = slice((2 * k + 1) * 128, (2 * k + 2) * 128)
                P.op("dve", lambda e, k=k, cre_=cre_: e.tensor_scalar(tq[:], stg[1][:, cre_], qre[:, k:k + 1], None, ALU.mult), [stg[1], qre], [tq])
                P.op("dve", lambda e, k=k, cre_=cre_, cim_=cim_: e.scalar_tensor_tensor(Cl_s[:, cre_], stg[1][:, cim_], nqim[:, k:k + 1], tq[:], ALU.mult, ALU.add), [stg[1], nqim, tq], [Cl_s])
                P.op("dve", lambda e, k=k, cre_=cre_: e.tensor_scalar(tq[:], stg[1][:, cre_], qim[:, k:k + 1], None, ALU.mult), [stg[1], qim], [tq])
                P.op("dve", lambda e, k=k, cim_=cim_: e.scalar_tensor_tensor(Cl_s[:, cim_], stg[1][:, cim_], qre[:, k:k + 1], tq[:], ALU.mult, ALU.add), [stg[1], qre, tq, Cl_s], [Cl_s])
            P.emit(final_waits=[gmix_s, gffn_s])
        nc.all_engine_barrier()
        for b in [mixedT, ident, gmix_s, gffn_s, rho, ucs, qre, qim, Bl_s, Cl_s, Dl_s, wglu_s]:
            b.lw = None
            b.rd = []

        with ExitStack() as es:
            P = Prog(nc, es, semes)
            Win = P.buf("Win", [128, 8, INW], BF16)
            xb = [P.buf("xb%d" % i, [128, D], F32) for i in range(2)]
            ab = [P.buf("ab%d" % i, [128, D], BF16) for i in range(2)]
            aTs = [P.buf("aT%d" % i, [128, 8, 256], BF16) for i in range(2)]
            qks = [P.buf("qk%d" % i, [128, 12, 256], BF16) for i in range(2)]
            qraw = [P.buf("qraw%d" % i, [128, 256], F32) for i in range(2)]
            rA = [P.buf("rA%d" % i, [128, 256], F32) for i in range(2)]
            rB = [P.buf("rB%d" % i, [128, 256], F32) for i in range(2)]
            cs = [P.buf("cs0", [128, 2, 256], F32)] * 2
            vg = [P.buf("vg%d" % i, [128, 1536], BF16) for i in range(4)]
            khat = P.buf("khat", [128, NH, 128], BF16)
            Sm = [P.buf("Sm%d" % i, [128, 3, 128], BF16) for i in range(2)]
            ctmp = P.buf("ctmp", [128, 3, 128], F32)
            o = P.buf("o", [128, NH, 128], F32)
            sg = P.buf("sg", [128, NH * 128], F32)
            yr = P.buf("yr", [128, NH * 128], BF16)
            R = P.buf("R", [128, NH, 128], F32)
            Rb = P.buf("Rb", [128, NH, 128], BF16)
            mask_s = P.buf("mask_s", [128, NH, 128], F32)
            qdec_s = P.buf("qdec_s", [128, NH], F32)
            kdec_s = P.buf("kdec_s", [128, NH], F32)
            st = [P.buf("st%d" % i, [128, 8], F32) for i in range(2)]
            gst = P.buf("gst", [128, 4, NH], F32)
            psT = P.buf("psT", [128, 8, 128], BF16, ps=True)
            psq = [P.buf("psq%d" % i, [128, 512], F32, ps=True) for i in range(2)]
            psB = P.buf("psB", [128, 8, 128], BF16, ps=True)
            psS = P.buf("psS", [128, 4, 128], F32, ps=True)
            psO = P.buf("psO", [128, 4, 128], F32, ps=True)
            psC = P.buf("psC", [128, 4, 128], F32, ps=True)
            psKV = P.buf("psKV", [128, 4, 128], F32, ps=True)

            P.dma("sp", mask_s[:], maskT.rearrange("p (h i) -> p h i", h=NH), writes=[mask_s])
            P.dma("sp", qdec_s[:], qdec[:, :], writes=[qdec_s])
            P.dma("sp", kdec_s[:], kdec[:, :], writes=[kdec_s])
            P.op("dve", lambda e: e.memset(R[:], 0.0), [], [R])
            P.op("pool", lambda e: e.memset(Rb[:], 0.0), [], [Rb])
            win_v = w_in.rearrange("(c p) n -> p c n", p=128)
            pieces = [(0, 1024), (1024, 1024), (2048, 1024), (3072, 256)]
            ci = 0
            pieces = [(0, 768), (768, 768), (1536, 768), (2304, 768), (3072, 256)]
            stg_bufs = [xb[0], xb[1], o, sg]
            stg_views = [xb[0][:], xb[1][:], o[:].rearrange("p h e -> p (h e)"), sg[:]]
            for c in range(8):
                for (c0, w) in pieces:
                    sb = stg_bufs[ci % 4]
                    sbv = stg_views[ci % 4]
                    P.dma("sp" if ci % 2 == 0 else "act", sbv[:, 0:w], win_v[:, c, c0:c0 + w], writes=[sb])
                    eng = ["act", "dve", "dve", "pool"][ci % 4]
                    if eng == "act":
                        P.op("act", lambda e, sbv=sbv, c=c, c0=c0, w=w: e.activation(Win[:, c, c0:c0 + w], sbv[:, 0:w], AF.Copy), [sb], [Win])
                    else:
                        P.op(eng, lambda e, sbv=sbv, c=c, c0=c0, w=w: e.tensor_copy(Win[:, c, c0:c0 + w], sbv[:, 0:w]), [sb], [Win])
                    ci += 1

            def load_x_tile(t, dst):
                if t == 0:
                    P.op("pool", lambda e: e.memset(dst[:], 0.0), [], [dst])
                    P.dma("sp", dst[48:64, :], meta[:, :], writes=[dst])
                    P.dma("sp", dst[64:128, :], x[0:64, :], writes=[dst])
                elif t == NT - 1:
                    P.op("pool", lambda e: e.memset(dst[:], 0.0), [], [dst])
                    P.dma("sp", dst[0:64, :], x[SEQ - 64:SEQ, :], writes=[dst])
                else:
                    P.dma("sp", dst[:], x[128 * t - 64:128 * t + 64, :], writes=[dst])

            def rms_to_bf(src, dst_bf, stt, scratch):
                P.op("act", lambda e: e.activation(scratch[:], src[:], AF.Square, accum_out=stt[:, 0:1]), [src], [scratch, stt])
                P.op("dve", lambda e: e.tensor_scalar(stt[:, 1:2], stt[:, 0:1], 1.0 / D, EPS, ALU.mult, ALU.add), [stt], [stt])
                P.op("act", lambda e: e.activation(stt[:, 2:3], stt[:, 1:2], AF.Sqrt), [stt], [stt])
                P.op("dve", lambda e: e.reciprocal(stt[:, 3:4], stt[:, 2:3]), [stt], [stt])
                P.op("act", lambda e: e.activation(dst_bf[:], src[:], AF.Copy, scale=stt[:, 3:4]), [src, stt], [dst_bf])

            NCB = 2
            cst = [P.buf("cst%d" % i, [128, 512], F32) for i in range(NCB)]
            cbf = [P.buf("cbf%d" % i, [128, 512], BF16) for i in range(NCB)]
            conv = []
            for e_ in range(NE):
                gv = w_gate[e_].rearrange("(c p) f -> p c f", p=128)
                uv = w_up[e_].rearrange("(c p) f -> p c f", p=128)
                dv = w_down[e_].rearrange("(c p) f -> p c f", p=128)
                for q4 in range(4):
                    conv.append((gv[:, 2 * q4:2 * q4 + 2, :], 2, sc_gate[e_][:, 512 * q4:512 * q4 + 512]))
                    conv.append((uv[:, 2 * q4:2 * q4 + 2, :], 2, sc_up[e_][:, 512 * q4:512 * q4 + 512]))
                    conv.append((dv[:, q4 // 2:q4 // 2 + 1, 512 * (q4 % 2):512 * (q4 % 2) + 512], 1, sc_down[e_][:, 512 * q4:512 * q4 + 512]))
            cvi = [0]
            LAG = 1

            def do_conv(kn):
                for _ in range(kn):
                    i = cvi[0]
                    cvi[0] += 1
                    if i < len(conv):
                        src, nc_, dst = conv[i]
                        bi = i % NCB
                        P.dma("sp", cst[bi][:].rearrange("p (c f) -> p c f", c=nc_), src, writes=[cst[bi]])
                    j = i - LAG
                    if 0 <= j < len(conv):
                        src, nc_, dst = conv[j]
                        bj = j % NCB
                        P.op("act", lambda e, bj=bj: e.activation(cbf[bj][:], cst[bj][:], AF.Copy), [cst[bj]], [cbf[bj]])
                        P.dma("sp", dst, cbf[bj][:], reads=[cbf[bj]], writes=[], sigbuf=cbf[bj])

            tcount = 0
            ngroups = (NT + 1) // 2

            def ret_pieces(tiles, qk, vgs):
                pcs = []
                for li, t in enumerate(tiles):
                    vt = vgs[li]
                    cols = slice(li * 128, (li + 1) * 128)

                    def p0(cols=cols):
                        def trk(e):
                            ins = None
                            for h in range(NH):
                                ins = e.transpose(psB[:, h, :], qk[:, 6 + h, cols], ident[:])
                            return ins
                        P.op("pe", trk, [qk, ident], [psB])
                        P.op("dve", lambda e: e.tensor_tensor(khat[:], psB[:, 0:NH, :], bc(kdec_s[:].unsqueeze(2), [128, NH, 128]), ALU.mult), [psB, kdec_s], [khat])
                    pcs.append(p0)
                    for half in range(2):
                        h0 = 3 * half
                        smb = Sm[half]

                        def p1(h0=h0, smb=smb, cols=cols):
                            def mmS(e):
                                ins = None
                                for hl in range(3):
                                    ins = e.matmul(psS[:, hl, :], qk[:, 6 + h0 + hl, cols], qk[:, h0 + hl, cols], start=True, stop=True)
                                return ins
                            P.op("pe", mmS, [qk], [psS])
                            P.op("dve", lambda e: e.tensor_tensor(smb[:], psS[:, 0:3, :], mask_s[:, h0:h0 + 3, :], ALU.mult), [psS, mask_s], [smb])

                            def mmC(e):
                                ins = None
                                for hl in range(3):
                                    ins = e.matmul(psC[:, hl, :], qk[:, h0 + hl, cols], Rb[:, h0 + hl, :], start=True, stop=True)
                                return ins
                            P.op("pe", mmC, [qk, Rb], [psC])
                            P.op("dve", lambda e: e.tensor_tensor(ctmp[:], psC[:, 0:3, :], bc(qdec_s[:, h0:h0 + 3].unsqueeze(2), [128, 3, 128]), ALU.mult), [psC, qdec_s], [ctmp])
                        pcs.append(p1)

                        def p2(h0=h0, smb=smb, vt=vt):
                            def mmO(e):
                                ins = None
                                for hl in range(3):
                                    h = h0 + hl
                                    ins = e.matmul(psO[:, hl, :], smb[:, hl, :], vt[:, h * 128:(h + 1) * 128], start=True, stop=True)
                                return ins
                            P.op("pe", mmO, [smb, vt], [psO])

                            def mmKV(e):
                                ins = None
                                for hl in range(3):
                                    h = h0 + hl
                                    ins = e.matmul(psKV[:, hl, :], khat[:, h, :], vt[:, h * 128:(h + 1) * 128], start=True, stop=True)
                                return ins
                            P.op("pe", mmKV, [khat, vt], [psKV])
                            P.op("dve", lambda e: e.tensor_tensor(o[:, h0:h0 + 3, :], psO[:, 0:3, :], ctmp[:], ALU.add), [psO, ctmp], [o])
                            for hl in range(3):
                                h = h0 + hl
                                P.op("dve", lambda e, h=h, hl=hl: e.scalar_tensor_tensor(R[:, h, :], R[:, h, :], g128[h], psKV[:, hl, :], ALU.mult, ALU.add), [R, psKV], [R])
                        pcs.append(p2)

                        def p2b(h0=h0):
                            P.op("act", lambda e: e.activation(Rb[:, h0:h0 + 3, :], R[:, h0:h0 + 3, :], AF.Copy), [R], [Rb])
                        pcs.append(p2b)

                    def p3a():
                        P.op("act", lambda e: e.activation(sg[:].rearrange("p (h e) -> p h e", h=NH), o[:], AF.Square), [o], [sg])
                    pcs.append(p3a)

                    def p3b():
                        P.op("dve", lambda e: e.tensor_reduce(gst[:, 0, :], o[:], AX.X, ALU.add), [o], [gst])
                        P.op("dve", lambda e: e.tensor_reduce(gst[:, 1, :], sg[:].rearrange("p (h e) -> p h e", h=NH), AX.X, ALU.add), [sg, gst], [gst])
                        P.op("dve", lambda e: e.tensor_scalar(gst[:, 0, :], gst[:, 0, :], 1.0 / 128, None, ALU.mult), [gst], [gst])
                        P.op("dve", lambda e: e.tensor_tensor(gst[:, 2, :], gst[:, 0, :], gst[:, 0, :], ALU.mult), [gst], [gst])
                        P.op("dve", lambda e: e.scalar_tensor_tensor(gst[:, 1, :], gst[:, 1, :], 1.0 / 128, gst[:, 2, :], ALU.mult, ALU.subtract), [gst], [gst])
                        P.op("dve", lambda e: e.tensor_scalar(gst[:, 1, :], gst[:, 1, :], EPS, None, ALU.add), [gst], [gst])
                    pcs.append(p3b)

                    def p3c(vt=vt):
                        P.op("act", lambda e: e.activation(gst[:, 2, :], gst[:, 1, :], AF.Sqrt), [gst], [gst])
                    pcs.append(p3c)

                    def p4a():
                        P.op("dve", lambda e: e.reciprocal(gst[:, 3, :], gst[:, 2, :]), [gst], [gst])
                        P.op("dve", lambda e: e.tensor_tensor(o[:], o[:], bc(gst[:, 0, :].unsqueeze(2), [128, NH, 128]), ALU.subtract), [o, gst], [o])
                        P.op("dve", lambda e: e.tensor_tensor(o[:], o[:], bc(gst[:, 3, :].unsqueeze(2), [128, NH, 128]), ALU.mult), [o, gst], [o])
                    pcs.append(p4a)

                    def p4b(vt=vt):
                        P.op("act", lambda e: e.activation(sg[:], vt[:, 768:1536], AF.Silu), [vt], [sg])
                    pcs.append(p4b)

                    def p5():
                        P.op("pool", lambda e: e.tensor_tensor(yr[:], o[:].rearrange("p h e -> p (h e)"), sg[:], ALU.mult), [o, sg], [yr])
                    pcs.append(p5)

                    def p6(t=t):
                        def try_(e):
                            ins = None
                            for h in range(NH):
                                ins = e.transpose(psB[:, h, :], yr[:, h * 128:(h + 1) * 128], ident[:])
                            return ins
                        P.op("pe", try_, [yr, ident], [psB])
                        P.op("act", lambda e: e.activation(mixedT[:, 2:8, 128 * t:128 * (t + 1)], psB[:, 0:NH, :], AF.Copy), [psB], [mxb[t]])
                    pcs.append(p6)
                return pcs

            def A_pieces(gi):
                nonlocal_t = []
                tiles_ = [t for t in (2 * gi, 2 * gi + 1) if t < NT]
                aTn = aTs[gi % 2]
                pcs = []
                for li, t in enumerate(tiles_):
                    k_ = tcnt[0]
                    tcnt[0] += 1
                    xs, a_, stt = xb[k_ % 2], ab[k_ % 2], st[k_ % 2]

                    def pa1(t=t, xs=xs):
                        load_x_tile(t, xs)
                    pcs.append(pa1)

                    def pa2(xs=xs, a_=a_, stt=stt):
                        P.op("act", lambda e: e.activation(a_[:], xs[:], AF.Square, accum_out=stt[:, 0:1]), [xs], [a_, stt])
                    pcs.append(pa2)

                    def pb1(stt=stt):
                        P.op("dve", lambda e: e.tensor_scalar(stt[:, 1:2], stt[:, 0:1], 1.0 / D, EPS, ALU.mult, ALU.add), [stt], [stt])
                        P.op("act", lambda e: e.activation(stt[:, 2:3], stt[:, 1:2], AF.Sqrt), [stt], [stt])
                    pcs.append(pb1)

                    def pb2(xs=xs, a_=a_, stt=stt):
                        P.op("dve", lambda e: e.reciprocal(stt[:, 3:4], stt[:, 2:3]), [stt], [stt])
                        P.op("act", lambda e: e.activation(a_[:], xs[:], AF.Copy, scale=stt[:, 3:4]), [xs, stt], [a_])
                    pcs.append(pb2)

                    def pc(li=li, a_=a_, aTn=aTn):
                        def tr(e):
                            ins = None
                            for c in range(8):
                                ins = e.transpose(psT[:, c, :], a_[:, c * 128:(c + 1) * 128], ident[:])
                            return ins
                        P.op("pe", tr, [a_, ident], [psT])
                        P.op("dve", lambda e: e.tensor_tensor(aTn[:, :, li * 128:(li + 1) * 128], psT[:], bc(gmix_s[:].unsqueeze(2), [128, 8, 128]), ALU.mult), [psT, gmix_s], [aTn])
                    pcs.append(pc)
                return pcs

            tcnt = [0]
            for fn in A_pieces(0):
                fn()
            pending = []
            for gi in range(ngroups):
                if gi % 4 == 0:
                    P.new_epoch()
                tiles = [t for t in (2 * gi, 2 * gi + 1) if t < NT]
                n = 128 * len(tiles)
                tok0 = 128 * tiles[0]
                mxs = [mxb[t] for t in tiles]
                csb = cs[gi % 2]
                qk = qks[gi % 2]
                aT = aTs[gi % 2]
                vgs = vg[2 * (gi % 2):2 * (gi % 2) + 2]
                nextA = A_pieces(gi + 1) if gi + 1 < ngroups else []
                P.dma("act", csb[:, 0, 0:n], cosT[:, tok0:tok0 + n], writes=[csb])
                P.dma("act", csb[:, 1, 0:n], sinT[:, tok0:tok0 + n], writes=[csb])
                pi = 0
                for blk in range(14):
                    ps = psq[pi % 2]
                    pi += 1
                    col0 = blk * 128

                    def mm(e, ps=ps, col0=col0, n=n, aT=aT):
                        ins = None
                        for c in range(8):
                            ins = e.matmul(ps[:, 0:n], Win[:, c, col0:col0 + 128], aT[:, c, 0:n], start=(c == 0), stop=(c == 7))
                        return ins
                    P.op("pe", mm, [Win, aT], [ps])
                    if blk < 2:
                        P.op("act", lambda e, ps=ps, blk=blk, n=n, tok0=tok0: e.activation(mixedT[:, blk, tok0:tok0 + n], ps[:, 0:n], AF.Copy), [ps], mxs)
                    else:
                        hh = blk - 2
                        qr = qraw[hh % 2]
                        A = rA[hh % 2]
                        B = rB[hh % 2]
                        P.op("act", lambda e, ps=ps, qr=qr, n=n: e.activation(qr[:, 0:n], ps[:, 0:n], AF.Copy), [ps], [qr])
                        P.op("dve", lambda e, qr=qr, A=A, n=n, csb=csb: e.tensor_tensor(A[:, 0:n], qr[:, 0:n], csb[:, 0, 0:n], ALU.mult), [qr, csb], [A])
                        P.op("dve", lambda e, qr=qr, B=B, n=n, csb=csb: e.tensor_tensor(B[0:64, 0:n], qr[64:128, 0:n], csb[64:128, 1, 0:n], ALU.mult), [qr, csb], [B])
                        P.op("dve", lambda e, qr=qr, B=B, n=n, csb=csb: e.tensor_tensor(B[64:128, 0:n], qr[0:64, 0:n], csb[0:64, 1, 0:n], ALU.mult), [qr, csb, B], [B])
                        P.op("pool", lambda e, A=A, B=B, hh=hh, n=n, qk=qk: e.tensor_tensor(qk[:, hh, 0:n], A[:, 0:n], B[:, 0:n], ALU.add), [A, B], [qk])
                        do_conv(1)
                    for _ in range(2 if blk % 2 == 1 else 1):
                        if pending:
                            pending.pop(0)()
                    if nextA and blk % 2 == 0:
                        nextA.pop(0)()
                for li, t in enumerate(tiles):
                    vt = vgs[li]
                    for cb in range(3):
                        ps = psq[pi % 2]
                        pi += 1

                        def mmv(e, ps=ps, li=li, cb=cb, aT=aT):
                            ins = None
                            for c in range(8):
                                ins = e.matmul(ps[:, :], aT[:, c, li * 128:(li + 1) * 128], Win[:, c, 1792 + cb * 512:1792 + (cb + 1) * 512], start=(c == 0), stop=(c == 7))
                            return ins
                        P.op("pe", mmv, [Win, aT], [ps])
                        P.op("act", lambda e, ps=ps, vt=vt, cb=cb: e.activation(vt[:, cb * 512:(cb + 1) * 512], ps[:, :], AF.Copy), [ps], [vt])
                        for _ in range(2):
                            if pending:
                                pending.pop(0)()
                        if nextA:
                            nextA.pop(0)()
                while pending:
                    pending.pop(0)()
                while nextA:
                    nextA.pop(0)()
                pending = ret_pieces(tiles, qk, vgs)
            while pending:
                pending.pop(0)()
            do_conv(len(conv) + LAG + 1 - cvi[0] if cvi[0] < len(conv) + LAG else 0)
            P.emit(final_waits=cbf)
        nc.all_engine_barrier()
        for b in [mixedT, ident, gmix_s, gffn_s, rho, ucs, qre, qim, Bl_s, Cl_s, Dl_s, wglu_s] + mxb:
            b.lw = None
            b.rd = []

        with ExitStack() as es:
            P = Prog(nc, es, semes)
            Wout = P.buf("Wout", [128, 8, D], BF16)
            wr_s = P.buf("wr_s", [128, 8, 20], BF16)
            br_s = P.buf("br_s", [128, 20], F32)
            gfin_s = P.buf("gfin_s", [128, D], F32)
            h2 = [P.buf("h2_%d" % i, [128, D], F32) for i in range(4)]
            tbf = [P.buf("tbf0", [128, D], BF16)] * 2
            tT = P.buf("tT", [128, 8, TB], BF16)
            comb = P.buf("comb", [128, 5, NE], F32)
            rt = P.buf("rt", [128, 5, 64], F32)
            st2 = P.buf("st2", [128, 8, 5], F32)
            st3 = P.buf("st3", [128, 8, 5], F32)
            wg = [P.buf("wg%d" % i, [128, 8, 256], BF16) for i in range(2)]
            wu = [P.buf("wu%d" % i, [128, 8, 256], BF16) for i in range(2)]
            wd = [P.buf("wd%d" % i, [128, 2, D], BF16) for i in range(2)]
            sil = [P.buf("sil%d" % i, [128, TB], BF16) for i in range(2)]
            actT = [P.buf("actT%d" % i, [128, 2, TB], BF16) for i in range(2)]
            psDs = [P.buf("psD%d" % i, [128, D], F32, ps=True) for i in range(2)]
            psGU = [P.buf("psGU%d" % i, [128, TB], F32, ps=True) for i in range(3)]
            psS5 = P.buf("psS5", [128, TB], F32, ps=True)
            psT2b = psS5
            psT2 = psS5[:].bitcast(BF16).rearrange("p (c t) -> p c t", c=8)
            psR = psS5
            dcount = [0]
            gucount = [0]
            Dre = P.buf("Dre", [128, 8, TS], F32)
            Dim = P.buf("Dim", [128, 8, TS], F32)
            bur = [P.buf("bur%d" % i, [128, TS], F32) for i in range(2)]
            bui = [P.buf("bui%d" % i, [128, TS], F32) for i in range(2)]
            mrs = [P.buf("mr%d" % i, [128, TS], F32) for i in range(2)]
            mis = [P.buf("mi%d" % i, [128, TS], F32) for i in range(2)]
            wrs = [P.buf("Wr%d" % i, [128, TS], F32) for i in range(2)]
            wis = [P.buf("Wi%d" % i, [128, TS], F32) for i in range(2)]
            ta = P.buf("ta", [128, TS], F32)
            tc = P.buf("tc", [128, TS], F32)
            tds = [[P.buf("td%d_%d" % (i, q), [128, TS], F32) for q in range(4)] for i in range(2)]
            Xb = P.buf("Xb", [128, 8, 2, TS], BF16)
            carry = P.buf("carry", [128, 8, 2], F32)
            w0 = P.buf("w0", [128, 8, 2], F32)
            w0s = P.buf("w0s", [128, 8, 2], F32)
            geT = P.buf("geT", [128, 2, TS], BF16)
            ysb = [P.buf("ysb%d" % i, [128, TS], F32) for i in range(2)]
            y2 = [P.buf("y2_%d" % i, [128, TS], F32) for i in range(2)]
            zz = y2
            sgm = [P.buf("sgm%d" % i, [128, TS], F32) for i in range(2)]
            _tv = tT[:].bitcast(F32)
            t1 = _tv[:, :, 0:TS // 2]
            t2 = _tv[:, :, TS // 2:TS]

            class VBuf(Buf):
                def __init__(self, name, ap):
                    Buf.__init__(self, name, None)
                    self.ap_ = ap

                def __getitem__(self, k):
                    return self.ap_[k]
            h2x = VBuf("h2x", Dre[:].rearrange("p k t -> p (k t)")[:, 0:D])
            dimv = Dim[:].bitcast(BF16).rearrange("p k t -> p (k t)")
            tTx_ap = dimv[:, 0:1024].rearrange("p (c t) -> p c t", c=8)
            actTx_ap = [dimv[:, 1024 + 256 * i:1024 + 256 * (i + 1)].rearrange("p (f t) -> p f t", f=2) for i in range(2)]
            silx_ap = [dimv[:, 1536 + 128 * i:1536 + 128 * (i + 1)] for i in range(2)]
            tTxb = Buf("tTxb", None)
            actTxb = [Buf("actTxb%d" % i, None) for i in range(2)]
            silxb = [Buf("silxb%d" % i, None) for i in range(2)]

            def tTa(c, li):
                return tT[:, c, li * 128:(li + 1) * 128] if li < 4 else tTx_ap[:, c, :]

            def tTbuf(li):
                return tT if li < 4 else tTxb

            wov = w_out.rearrange("(c p) n -> p c n", p=128)
            for c in range(8):
                sb = h2[2 + c % 2]
                P.dma("sp", sb[:], wov[:, c, :], writes=[sb])
                P.op("pool" if c % 2 else "act", (lambda e, sb=sb, c=c: e.tensor_copy(Wout[:, c, :], sb[:])) if c % 2 else (lambda e, sb=sb, c=c: e.activation(Wout[:, c, :], sb[:], AF.Copy)), [sb], [Wout])
            P.dma("sp", h2[2][:, 0:160].rearrange("p (c f) -> p c f", c=8), wr.rearrange("(c p) f -> p c f", p=128), writes=[h2[2]])
            P.op("act", lambda e: e.activation(wr_s[:], h2[2][:, 0:160].rearrange("p (c f) -> p c f", c=8), AF.Copy), [h2[2]], [wr_s])
            P.dma("sp", br_s[:], br[:, :], writes=[br_s])
            P.dma("sp", gfin_s[:], gfin[:, :], writes=[gfin_s])
            P.op("dve", lambda e: e.memset(carry[:], 0.0), [], [carry])
            P.op("dve", lambda e: e.memset(Dre[:, :, 0:1], 1.0), [], [Dre])
            P.op("dve", lambda e: e.memset(Dim[:, :, 0:1], 0.0), [], [Dim])
            P.op("dve", lambda e: e.tensor_copy(Dre[:, :, 1:2], ucs[:, 0, :].unsqueeze(2)), [ucs, Dre], [Dre])
            P.op("dve", lambda e: e.tensor_copy(Dim[:, :, 1:2], ucs[:, 1, :].unsqueeze(2)), [ucs, Dim], [Dim])
            n = 2
            while n < TS:
                h = n // 2
                P.op("dve", lambda e, h=h: e.tensor_tensor(t1[:, :, 0:1], Dre[:, :, h:h + 1], Dre[:, :, h:h + 1], ALU.mult), [Dre], [tT])
                P.op("dve", lambda e, h=h: e.tensor_tensor(t2[:, :, 0:1], Dim[:, :, h:h + 1], Dim[:, :, h:h + 1], ALU.mult), [Dim], [tT])
                P.op("dve", lambda e, n=n: e.tensor_tensor(Dre[:, :, n:n + 1], t1[:, :, 0:1], t2[:, :, 0:1], ALU.subtract), [tT, Dre], [Dre])
                P.op("dve", lambda e, h=h: e.tensor_tensor(t1[:, :, 0:1], Dre[:, :, h:h + 1], Dim[:, :, h:h + 1], ALU.mult), [Dre, Dim], [tT])
                P.op("dve", lambda e, n=n: e.tensor_scalar(Dim[:, :, n:n + 1], t1[:, :, 0:1], 2.0, None, ALU.mult), [tT, Dim], [Dim])
                m = n - 1
                P.op("dve", lambda e, n=n, m=m: e.tensor_tensor(t1[:, :, 0:m], Dre[:, :, 1:n], bc(Dre[:, :, n:n + 1], [128, 8, m]), ALU.mult), [Dre], [tT])
                P.op("dve", lambda e, n=n, m=m: e.tensor_tensor(t2[:, :, 0:m], Dim[:, :, 1:n], bc(Dim[:, :, n:n + 1], [128, 8, m]), ALU.mult), [Dim], [tT])
                P.op("dve", lambda e, n=n, m=m: e.tensor_tensor(Dre[:, :, n + 1:2 * n], t1[:, :, 0:m], t2[:, :, 0:m], ALU.subtract), [tT, Dre], [Dre])
                P.op("dve", lambda e, n=n, m=m: e.tensor_tensor(t1[:, :, 0:m], Dre[:, :, 1:n], bc(Dim[:, :, n:n + 1], [128, 8, m]), ALU.mult), [Dre, Dim], [tT])
                P.op("dve", lambda e, n=n, m=m: e.tensor_tensor(t2[:, :, 0:m], Dim[:, :, 1:n], bc(Dre[:, :, n:n + 1], [128, 8, m]), ALU.mult), [Dre, Dim], [tT])
                P.op("dve", lambda e, n=n, m=m: e.tensor_tensor(Dim[:, :, n + 1:2 * n], t1[:, :, 0:m], t2[:, :, 0:m], ALU.add), [tT, Dim], [Dim])
                n *= 2

            def s5_sched(bs, off, sched, fast=False):
                e1 = "dve" if fast else "pool"
                c0 = bs * TS
                n = min(TS, LP - c0)
                tl = [mxb[t] for t in range(c0 // 128, (c0 + n) // 128)]

                def at(slot, fn):
                    sched.setdefault(slot, []).append(fn)
                def pre():
                    P.op("dve", lambda e: e.tensor_tensor(w0[:, :, 0], carry[:, :, 0], ucs[:, 0, :], ALU.mult), [carry, ucs], [w0])
                    P.op("dve", lambda e: e.tensor_tensor(w0[:, :, 1], carry[:, :, 1], ucs[:, 1, :], ALU.mult), [carry, ucs, w0], [w0])
                    P.op("dve", lambda e: e.tensor_tensor(w0s[:, :, 0], w0[:, :, 0], w0[:, :, 1], ALU.subtract), [w0], [w0s])
                    P.op("dve", lambda e: e.tensor_tensor(w0[:, :, 0], carry[:, :, 0], ucs[:, 1, :], ALU.mult), [carry, ucs, w0], [w0])
                    P.op("dve", lambda e: e.tensor_tensor(w0[:, :, 1], carry[:, :, 1], ucs[:, 0, :], ALU.mult), [carry, ucs, w0], [w0])
                    P.op("dve", lambda e: e.tensor_tensor(w0s[:, :, 1], w0[:, :, 0], w0[:, :, 1], ALU.add), [w0], [w0s])
                at(off + 1, pre)
                hA, hB = slice(0, n), slice(256, 256 + n)
                for k in range(8):
                    par = k % 2
                    j, kk = k // 4, k % 4
                    br_, bi_, mr_, mi_, wr_, wi_ = bur[par], bui[par], mrs[par], mis[par], wrs[par], wis[par]
                    d0, d1, d2, d3 = tds[par]

                    def st0(k=k, j=j, kk=kk, br_=br_, bi_=bi_):
                        def mm(e):
                            e.matmul(psS5[:, hA], Bl_s[:, (j * 8 + kk * 2) * 128:(j * 8 + kk * 2 + 1) * 128], mixedT[:, j, c0:c0 + n], start=True, stop=True)
                            return e.matmul(psS5[:, hB], Bl_s[:, (j * 8 + kk * 2 + 1) * 128:(j * 8 + kk * 2 + 2) * 128], mixedT[:, j, c0:c0 + n], start=True, stop=True)
                        P.op("pe", mm, [Bl_s] + tl, [psS5])
                        P.op("act", lambda e: e.activation(br_[:, 0:n], psS5[:, hA], AF.Copy), [psS5], [br_])
                        P.op("act", lambda e: e.activation(bi_[:, 0:n], psS5[:, hB], AF.Copy), [psS5], [bi_])

                    def st1(k=k, br_=br_, bi_=bi_, mr_=mr_, mi_=mi_):
                        P.op(e1, lambda e: e.tensor_tensor(ta[:, 0:n], br_[:, 0:n], Dre[:, k, 0:n], ALU.mult), [br_, Dre], [ta])
                        P.op(e1, lambda e: e.tensor_tensor(mr_[:, 0:n], bi_[:, 0:n], Dim[:, k, 0:n], ALU.mult), [bi_, Dim], [mr_])
                        P.op(e1, lambda e: e.tensor_tensor(mr_[:, 0:n], ta[:, 0:n], mr_[:, 0:n], ALU.add), [ta, mr_], [mr_])
                        P.op(e1, lambda e: e.tensor_tensor(tc[:, 0:n], bi_[:, 0:n], Dre[:, k, 0:n], ALU.mult), [bi_, Dre], [tc])
                        P.op(e1, lambda e: e.tensor_tensor(mi_[:, 0:n], br_[:, 0:n], Dim[:, k, 0:n], ALU.mult), [br_, Dim], [mi_])
                        P.op(e1, lambda e: e.tensor_tensor(mi_[:, 0:n], tc[:, 0:n], mi_[:, 0:n], ALU.subtract), [tc, mi_], [mi_])

                    def st2(k=k, mr_=mr_, mi_=mi_, wr_=wr_, wi_=wi_):
                        P.op("dve", lambda e: e.tensor_tensor_scan(wr_[:, 0:n], bc(rho[:, k:k + 1], [128, n]), mr_[:, 0:n], w0s[:, k, 0:1], ALU.mult, ALU.add), [rho, mr_, w0s], [wr_])
                        P.op("dve", lambda e: e.tensor_tensor_scan(wi_[:, 0:n], bc(rho[:, k:k + 1], [128, n]), mi_[:, 0:n], w0s[:, k, 1:2], ALU.mult, ALU.add), [rho, mi_, w0s], [wi_])

                    def st3(k=k, wr_=wr_, wi_=wi_, d0=d0, d1=d1, d2=d2, d3=d3):
                        P.op("pool", lambda e: e.tensor_tensor(d0[:, 0:n], wr_[:, 0:n], Dre[:, k, 0:n], ALU.mult), [wr_, Dre], [d0])
                        P.op("pool", lambda e: e.tensor_tensor(d1[:, 0:n], wi_[:, 0:n], Dim[:, k, 0:n], ALU.mult), [wi_, Dim], [d1])
                        P.op("pool", lambda e: e.tensor_tensor(Xb[:, k, 0, 0:n], d0[:, 0:n], d1[:, 0:n], ALU.subtract), [d0, d1], [Xb])
                        P.op("pool", lambda e: e.tensor_tensor(d2[:, 0:n], wr_[:, 0:n], Dim[:, k, 0:n], ALU.mult), [wr_, Dim], [d2])
                        P.op("pool", lambda e: e.tensor_tensor(d3[:, 0:n], wi_[:, 0:n], Dre[:, k, 0:n], ALU.mult), [wi_, Dre], [d3])

                    def st4(k=k, d0=d0, d1=d1, d2=d2, d3=d3):
                        P.op("dve", lambda e: e.scalar_tensor_tensor(Xb[:, k, 1, 0:n], d2[:, 0:n], -1.0, d3[:, 0:n], ALU.mult, ALU.subtract), [d2, d3, Xb], [Xb])
                        P.op("dve", lambda e: e.tensor_tensor(carry[:, k, 0:1], d0[:, n - 1:n], d1[:, n - 1:n], ALU.subtract), [d0, d1, carry], [carry])
                        P.op("dve", lambda e: e.tensor_tensor(carry[:, k, 1:2], d2[:, n - 1:n], d3[:, n - 1:n], ALU.add), [d2, d3, carry], [carry])
                    for si, fn in enumerate((st0, st1, st2, st3, st4)):
                        at(off + k + si, fn)
                T = off + 12
                hs = [hA, hB]

                def T0():
                    for j in range(2):
                        def mmy(e, j=j):
                            first = True
                            for kk in range(4):
                                k = 4 * j + kk
                                for c in range(2):
                                    e.matmul(psS5[:, hs[j]], Cl_s[:, (k * 2 + c) * 128:(k * 2 + c + 1) * 128], Xb[:, k, c, 0:n], start=first, stop=False)
                                    first = False
                            return e.matmul(psS5[:, hs[j]], Dl_s[:, j * 128:(j + 1) * 128], mixedT[:, j, c0:c0 + n], start=False, stop=True)
                        P.op("pe", mmy, [Cl_s, Xb, Dl_s] + tl, [psS5])
                    for j in range(2):
                        P.op("act", lambda e, j=j: e.activation(ysb[j][:, 0:n], psS5[:, hs[j]], AF.Copy), [psS5], [ysb[j]])
                        P.op("act", lambda e, j=j: e.activation(y2[j][:, 0:n], psS5[:, hs[j]], AF.Square), [psS5], [y2[j]])

                def T1():
                    for j in range(2):
                        P.op("pool", lambda e, j=j: e.tensor_scalar(y2[j][:, 0:n], y2[j][:, 0:n], 0.044715, 1.0, ALU.mult, ALU.add), [y2[j]], [y2[j]])

                def T2():
                    for j in range(2):
                        P.op("dve", lambda e, j=j: e.tensor_tensor(zz[j][:, 0:n], y2[j][:, 0:n], ysb[j][:, 0:n], ALU.mult), [y2[j], ysb[j]], [zz[j]])

                def T3():
                    for j in range(2):
                        P.op("act", lambda e, j=j: e.activation(sgm[j][:, 0:n], zz[j][:, 0:n], AF.Sigmoid, scale=GELU_C), [zz[j]], [sgm[j]])

                def T4():
                    for j in range(2):
                        P.op("dve", lambda e, j=j: e.tensor_tensor(geT[:, j, 0:n], sgm[j][:, 0:n], ysb[j][:, 0:n], ALU.mult), [sgm[j], ysb[j]], [geT])

                def T5():
                    for jo in range(2):
                        def mmg(e, jo=jo):
                            e.matmul(psS5[:, hs[jo]], wglu_s[:, 0, jo * 128:(jo + 1) * 128], geT[:, 0, 0:n], start=True, stop=False)
                            return e.matmul(psS5[:, hs[jo]], wglu_s[:, 1, jo * 128:(jo + 1) * 128], geT[:, 1, 0:n], start=False, stop=True)
                        P.op("pe", mmg, [wglu_s, geT], [psS5])
                    for jo in range(2):
                        P.op("act", lambda e, jo=jo: e.activation(sgm[jo][:, 0:n], psS5[:, hs[jo]], AF.Sigmoid), [psS5], [sgm[jo]])

                def T6():
                    for jo in range(2):
                        P.op("pool", lambda e, jo=jo: e.tensor_tensor(mixedT[:, jo, c0:c0 + n], sgm[jo][:, 0:n], geT[:, jo, 0:n], ALU.mult), [sgm[jo], geT], tl)
                for si, fn in enumerate((T0, T1, T2, T3, T4, T5, T6)):
                    at(T + si, fn)

            def make_sched(bl, fast=False, step=None):
                sched = {}
                off = 0
                for bs in bl:
                    if bs < NBS:
                        s5_sched(bs, off, sched, fast)
                        off += step if step is not None else (10 if fast else 16)
                return sched

            def run_slot(sched, slot):
                for fn in sched.pop(slot, []):
                    fn()

            def run_rest(sched):
                for slot in sorted(sched.keys()):
                    for fn in sched[slot]:
                        fn()
                sched.clear()

            RATIO = TB // TS
            run_rest(make_sched(range(0, RATIO), fast=True))
            wcount = 0
            tcount = 0
            NBM = NB - 1
            for b in range(NBM):
                if b % 2 == 0:
                    P.new_epoch()
                if b < NBM - 2:
                    sched = make_sched(range(RATIO * (b + 1), RATIO * (b + 2)))
                elif b == NBM - 2:
                    sched = make_sched(range(RATIO * (b + 1), NBS), step=10)
                else:
                    sched = {}
                slot = [0]
                c0 = b * TB
                n = TB
                tiles = list(range(4 * b, 4 * b + 4)) + ([NT - 1] if b == NBM - 1 else [])
                nl = len(tiles)
                h2c = h2 + ([h2x] if nl == 5 else [])
                if nl == 5:
                    P.op("dve", lambda e: e.memset(w0[:, 0:1, 0:1], 0.0), [], [Dre, Dim, w0, h2x, tTxb] + actTxb + silxb)
                for li, t in enumerate(tiles):
                    hb = h2c[li]
                    if t == 0:
                        P.op("pool", lambda e, hb=hb: e.memset(hb[:], 0.0), [], [hb])
                        P.dma("sp", hb[48:64, :], meta[:, :], writes=[hb])
                        P.dma("sp", hb[64:128, :], x[0:64, :], writes=[hb])
                    elif t == NT - 1:
                        P.op("pool", lambda e, hb=hb: e.memset(hb[:], 0.0), [], [hb])
                        P.dma("sp", hb[0:64, :], x[SEQ - 64:SEQ, :], writes=[hb])
                    else:
                        P.dma("sp", hb[:], x[128 * t - 64:128 * t + 64, :], writes=[hb])
                def head_norm(lo, hi):
                    P.op("dve", lambda e: e.tensor_scalar(st2[:, 1, lo:hi], st2[:, 0, lo:hi], 1.0 / D, EPS, ALU.mult, ALU.add), [st2], [st2])
                    P.op("act", lambda e: e.activation(st2[:, 2, lo:hi], st2[:, 1, lo:hi], AF.Sqrt), [st2], [st2])
                    P.op("dve", lambda e: e.reciprocal(st2[:, 3, lo:hi], st2[:, 2, lo:hi]), [st2], [st2])
                for li, t in enumerate(tiles):
                    hb = h2c[li]
                    if li < 2:
                        pa_, pb_ = (psGU[1], psGU[2]) if li == 0 else (psGU[0], psS5)

                        def mmo0(e, t=t, pa_=pa_, pb_=pb_):
                            ins = None
                            for half, pp in enumerate((pa_, pb_)):
                                for c in range(8):
                                    ins = e.matmul(pp[:, 0:512], mixedT[:, c, 128 * t:128 * (t + 1)], Wout[:, c, half * 512:(half + 1) * 512], start=(c == 0), stop=(c == 7))
                            return ins
                        P.op("pe", mmo0, [mxb[t], Wout], [pa_, pb_])
                        P.op("dve", lambda e, hb=hb, pa_=pa_: e.tensor_tensor(hb[:, 0:512], hb[:, 0:512], pa_[:, 0:512], ALU.add), [hb, pa_], [hb])
                        P.op("dve", lambda e, hb=hb, pb_=pb_: e.tensor_tensor(hb[:, 512:1024], hb[:, 512:1024], pb_[:, 0:512], ALU.add), [hb, pb_], [hb])
                    else:
                        psD = psDs[dcount[0] % 2]
                        dcount[0] += 1

                        def mmo(e, t=t, psD=psD):
                            ins = None
                            for half in range(2):
                                for c in range(8):
                                    ins = e.matmul(psD[:, half * 512:(half + 1) * 512], mixedT[:, c, 128 * t:128 * (t + 1)], Wout[:, c, half * 512:(half + 1) * 512], start=(c == 0), stop=(c == 7))
                            return ins
                        P.op("pe", mmo, [mxb[t], Wout], [psD])
                        P.op("dve", lambda e, hb=hb, psD=psD: e.tensor_tensor(hb[:], hb[:], psD[:], ALU.add), [hb, psD], [hb])
                    P.op("act", lambda e, hb=hb, li=li: e.activation(actT[0][:].rearrange("p a b -> p (a b)"), hb[:], AF.Square, accum_out=st2[:, 0, li:li + 1]), [hb], [actT[0], st2])
                    if li == 1 and nl > 2:
                        head_norm(0, 2)
                head_norm(2 if nl > 2 else 0, nl)
                tb_aps = [tbf[0][:], actT[1][:].rearrange("p a b -> p (a b)")]
                tb_bufs = [tbf[0], actT[1]]
                pT_aps = [psT2, psGU[0][:].bitcast(BF16).rearrange("p (c t) -> p c t", c=8)]
                pT_bufs = [psS5, psGU[0]]
                for li, t in enumerate(tiles):
                    hb = h2c[li]
                    kq = li % 2
                    tb_ap, tb_b, pT_ap, pT_b = tb_aps[kq], tb_bufs[kq], pT_aps[kq], pT_bufs[kq]
                    P.op("act", lambda e, hb=hb, li=li, tb_ap=tb_ap: e.activation(tb_ap, hb[:], AF.Copy, scale=st2[:, 3, li:li + 1]), [hb, st2], [tb_b])

                    def tr2(e, tb_ap=tb_ap, pT_ap=pT_ap):
                        ins = None
                        for c in range(8):
                            ins = e.transpose(pT_ap[:, c, :], tb_ap[:, c * 128:(c + 1) * 128], ident[:])
                        return ins
                    P.op("pe", tr2, [tb_b, ident], [pT_b])

                    tT3 = tT[:, :, li * 128:(li + 1) * 128] if li < 4 else tTx_ap
                    P.op("dve", lambda e, tT3=tT3, pT_ap=pT_ap: e.tensor_tensor(tT3, pT_ap, bc(gffn_s[:].unsqueeze(2), [128, 8, 128]), ALU.mult), [pT_b, gffn_s], [tTbuf(li)])

                def head_router(nl):
                    def mmr(e):
                        ins = None
                        for li in range(nl):
                            for c in range(8):
                                ins = e.matmul(psR[:, 20 * li:20 * li + 20], tTa(c, li), wr_s[:, c, :], start=(c == 0), stop=(c == 7))
                        return ins
                    P.op("pe", mmr, [tT, wr_s] + ([tTxb] if nl == 5 else []), [psR])
                    R_ = lambda a, b_: rt[:, 0:nl, a:b_]
                    cmv = comb[:, 0:nl, :]
                    P.op("dve", lambda e: e.tensor_tensor(R_(0, 20), psR[:, 0:20 * nl].rearrange("p (t f) -> p t f", f=20), bc(br_s[:].unsqueeze(1), [128, nl, 20]), ALU.add), [psR, br_s], [rt])
                    P.op("dve", lambda e: e.tensor_reduce(rt[:, 0:nl, 20], R_(0, 4), AX.X, ALU.max), [rt], [rt])
                    P.op("dve", lambda e: e.tensor_tensor(R_(21, 25), R_(0, 4), bc(R_(20, 21), [128, nl, 4]), ALU.is_ge), [rt], [rt])
                    P.op("dve", lambda e: e.tensor_tensor(R_(25, 29), R_(0, 4), bc(R_(20, 21), [128, nl, 4]), ALU.subtract), [rt], [rt])
                    P.op("act", lambda e: e.activation(R_(25, 29), R_(25, 29), AF.Exp), [rt], [rt])
                    P.op("dve", lambda e: e.tensor_reduce(rt[:, 0:nl, 29], R_(25, 29), AX.X, ALU.add), [rt], [rt])
                    P.op("dve", lambda e: e.reciprocal(R_(30, 31), R_(29, 30)), [rt], [rt])
                    P.op("dve", lambda e: e.tensor_tensor(cmv.rearrange("p t (e g) -> p t e g", g=4), R_(4, 20).rearrange("p t (g e) -> p t e g", e=4), bc(R_(21, 25).unsqueeze(2), [128, nl, 4, 4]), ALU.mult), [rt], [comb])
                    P.op("dve", lambda e: e.tensor_reduce(R_(31, 35), cmv.rearrange("p t (e g) -> p t e g", g=4), AX.X, ALU.add), [comb, rt], [rt])
                    P.op("dve", lambda e: e.tensor_reduce(rt[:, 0:nl, 35], R_(31, 35), AX.X, ALU.max), [rt], [rt])
                    P.op("dve", lambda e: e.tensor_tensor(R_(36, 40), R_(31, 35), bc(R_(35, 36), [128, nl, 4]), ALU.is_ge), [rt], [rt])
                    P.op("dve", lambda e: e.scalar_tensor_tensor(R_(40, 44), R_(36, 40), -1e30, R_(31, 35), ALU.mult, ALU.add), [rt], [rt])
                    P.op("dve", lambda e: e.tensor_reduce(rt[:, 0:nl, 44], R_(40, 44), AX.X, ALU.max), [rt], [rt])
                    P.op("dve", lambda e: e.tensor_tensor(R_(45, 49), R_(40, 44), bc(R_(44, 45), [128, nl, 4]), ALU.is_ge), [rt], [rt])
                    P.op("dve", lambda e: e.tensor_tensor(R_(49, 50), R_(44, 45), R_(35, 36), ALU.subtract), [rt], [rt])
                    P.op("act", lambda e: e.activation(R_(50, 51), R_(49, 50), AF.Exp), [rt], [rt])
                    P.op("dve", lambda e: e.tensor_scalar(R_(51, 52), R_(50, 51), 1.0, None, ALU.add), [rt], [rt])
                    P.op("dve", lambda e: e.reciprocal(R_(52, 53), R_(51, 52)), [rt], [rt])
                    P.op("dve", lambda e: e.tensor_tensor(R_(51, 52), R_(50, 51), R_(52, 53), ALU.mult), [rt], [rt])
                    P.op("dve", lambda e: e.tensor_tensor(R_(53, 57), R_(36, 40), bc(R_(52, 53), [128, nl, 4]), ALU.mult), [rt], [rt])
                    P.op("dve", lambda e: e.tensor_tensor(R_(57, 61), R_(45, 49), bc(R_(51, 52), [128, nl, 4]), ALU.mult), [rt], [rt])
                    P.op("dve", lambda e: e.tensor_tensor(R_(53, 57), R_(53, 57), R_(57, 61), ALU.add), [rt], [rt])
                    P.op("dve", lambda e: e.tensor_tensor(R_(57, 61), R_(21, 25), bc(R_(30, 31), [128, nl, 4]), ALU.mult), [rt], [rt])
                    P.op("dve", lambda e: e.tensor_tensor(cmv.rearrange("p t (g e) -> p t g e", e=4), bc(R_(57, 61).unsqueeze(3), [128, nl, 4, 4]), bc(R_(53, 57).unsqueeze(2), [128, nl, 4, 4]), ALU.mult), [rt], [comb])
                head_router(nl)

                def down(ex, lis, wi, at, h2c=h2c):
                    for li in lis:
                        psD = psDs[dcount[0] % 2]
                        dcount[0] += 1
                        atx = actTx_ap[ex % 2]

                        def mmd(e, at=at, atx=atx, wi=wi, li=li, psD=psD):
                            ins = None
                            for half in range(2):
                                for f in range(2):
                                    src = at[:, f, li * 128:(li + 1) * 128] if li < 4 else atx[:, f, :]
                                    ins = e.matmul(psD[:, half * 512:(half + 1) * 512], src, wd[wi][:, f, half * 512:(half + 1) * 512], start=(f == 0), stop=(f == 1))
                            return ins
                        P.op("pe", mmd, [at if li < 4 else actTxb[ex % 2], wd[wi]], [psD])
                        P.op("dve", lambda e, li=li, ex=ex, psD=psD, hb=h2c[li]: e.scalar_tensor_tensor(hb[:], psD[:], comb[:, li, ex:ex + 1], hb[:], ALU.mult, ALU.add), [psD, comb, h2c[li]], [h2c[li]])

                prev = None
                for ex in range(NE):
                    wi = wcount % 2
                    wcount += 1
                    P.dma("sp", wg[wi][:].rearrange("p c f -> p (c f)"), sc_gate[ex], writes=[wg[wi]])
                    P.dma("sp", wu[wi][:].rearrange("p c f -> p (c f)"), sc_up[ex], writes=[wu[wi]])
                    P.dma("sp", wd[wi][:].rearrange("p c f -> p (c f)"), sc_down[ex], writes=[wd[wi]])
                    at = actT[ex % 2]
                    for f in range(2):
                        pg_, pu_ = psGU[gucount[0] % 3], psGU[(gucount[0] + 1) % 3]
                        gucount[0] += 2

                        def mmg2(e, pg_=pg_, wi=wi, f=f, n=n):
                            ins = None
                            for c in range(8):
                                ins = e.matmul(pg_[:, 0:n], wg[wi][:, c, f * 128:(f + 1) * 128], tT[:, c, 0:n], start=(c == 0), stop=(c == 7))
                            return ins
                        P.op("pe", mmg2, [wg[wi], tT], [pg_])

                        def mmu2(e, pu_=pu_, wi=wi, f=f, n=n):
                            ins = None
                            for c in range(8):
                                ins = e.matmul(pu_[:, 0:n], wu[wi][:, c, f * 128:(f + 1) * 128], tT[:, c, 0:n], start=(c == 0), stop=(c == 7))
                            return ins
                        P.op("pe", mmu2, [wu[wi], tT], [pu_])
                        sl_ = sil[f]
                        P.op("act", lambda e, pg_=pg_, sl_=sl_, n=n: e.activation(sl_[:, 0:n], pg_[:, 0:n], AF.Silu), [pg_], [sl_])
                        P.op("dve", lambda e, pu_=pu_, sl_=sl_, at=at, f=f, n=n: e.tensor_tensor(at[:, f, 0:n], pu_[:, 0:n], sl_[:, 0:n], ALU.mult), [pu_, sl_], [at])
                        if nl == 5:
                            def mmx(e, wi=wi, f=f):
                                ins = None
                                for c in range(8):
                                    e.matmul(psS5[:, 0:128], wg[wi][:, c, f * 128:(f + 1) * 128], tTx_ap[:, c, :], start=(c == 0), stop=(c == 7))
                                for c in range(8):
                                    ins = e.matmul(psS5[:, 128:256], wu[wi][:, c, f * 128:(f + 1) * 128], tTx_ap[:, c, :], start=(c == 0), stop=(c == 7))
                                return ins
                            P.op("pe", mmx, [wg[wi], wu[wi], tTxb], [psS5])
                            P.op("act", lambda e, f=f: e.activation(silx_ap[f], psS5[:, 0:128], AF.Silu), [psS5], [silxb[f]])
                            P.op("dve", lambda e, f=f, ex=ex: e.tensor_tensor(actTx_ap[ex % 2][:, f, :], psS5[:, 128:256], silx_ap[f], ALU.mult), [psS5, silxb[f]], [actTxb[ex % 2]])
                        run_slot(sched, slot[0])
                        slot[0] += 1
                        if prev is not None:
                            lis = list(range(nl))[(f * ((nl + 1) // 2)):((f + 1) * ((nl + 1) // 2))] if nl > 1 else ([0] if f == 0 else [])
                            down(prev[0], lis, prev[1], prev[2])
                    prev = (ex, wi, at)
                down(prev[0], list(range(nl)), prev[1], prev[2])
                run_rest(sched)
                for li, t in enumerate(tiles):
                    hb = h2c[li]
                    P.op("act", lambda e, hb=hb, li=li: e.activation(actT[0][:].rearrange("p a b -> p (a b)"), hb[:], AF.Square, accum_out=st3[:, 0, li:li + 1]), [hb], [actT[0], st3])
                def fin_norm(nl):
                    P.op("dve", lambda e: e.tensor_scalar(st3[:, 1, 0:nl], st3[:, 0, 0:nl], 1.0 / D, EPS, ALU.mult, ALU.add), [st3], [st3])
                    P.op("act", lambda e: e.activation(st3[:, 2, 0:nl], st3[:, 1, 0:nl], AF.Sqrt), [st3], [st3])
                    P.op("dve", lambda e: e.reciprocal(st3[:, 3, 0:nl], st3[:, 2, 0:nl]), [st3], [st3])
                fin_norm(nl)
                for li, t in enumerate(tiles):
                    hb = h2c[li]
                    P.op("dve", lambda e, hb=hb, li=li: e.scalar_tensor_tensor(hb[:], hb[:], st3[:, 3, li:li + 1], gfin_s[:], ALU.mult, ALU.mult), [hb, st3, gfin_s], [hb])
                    if t == 0:
                        P.dma("act", out[0:64, :], hb[64:128, :], reads=[hb], sigbuf=hb)
                    elif t == NT - 1:
                        P.dma("act", out[SEQ - 64:SEQ, :], hb[0:64, :], reads=[hb], sigbuf=hb)
                    else:
                        P.dma("act", out[128 * t - 64:128 * t + 64, :], hb[:], reads=[hb], sigbuf=hb)
            P.emit(final_waits=h2 + [h2x])
    return nc


def _host_consts():
    c = {}
    d = np.arange(128)
    inv_freq = (10000.0 ** (-(np.arange(0, 128, 2, dtype=np.float32)) / np.float32(128))).astype(np.float32)
    pos = (np.arange(LP, dtype=np.float32) - np.float32(48.0)).astype(np.float32)
    ang = (pos[None, :] * inv_freq[d % 64][:, None]).astype(np.float32)
    c["cosT"] = np.cos(ang).astype(np.float32)
    sn = np.sin(ang).astype(np.float32)
    sn[64:] = -sn[64:]
    c["sinT"] = sn
    gamma = 1.0 - 2.0 ** (-5.0 - np.arange(NH, dtype=np.float64))
    j = np.arange(128)[:, None]
    i = np.arange(128)[None, :]
    mask = np.zeros((128, NH, 128), np.float64)
    same = (j // 64) == (i // 64)
    causal_ab = (j < 64) & (i >= 64)
    for h in range(NH):
        m = np.where(same, gamma[h] ** np.abs(i - j), 0.0)
        m = np.where(causal_ab, gamma[h] ** (i - j), m)
        mask[:, h, :] = m * (128.0 ** -0.5)
    c["maskT"] = mask.reshape(128, NH * 128).astype(np.float32)
    r = np.arange(128)[:, None]
    c["qdec"] = (gamma[None, :] ** (r + 1)).astype(np.float32)
    c["kdec"] = ((gamma[None, :] ** (127 - r)) * (128.0 ** -0.5)).astype(np.float32)
    c["identf"] = np.eye(128, dtype=np.float32)
    return c


_NC_CACHE = {}


def kernel(x, meta_tokens, norm_mix_g, w_in, ssm_lambda_re, ssm_lambda_im, ssm_log_dt,
           ssm_b_re, ssm_b_im, ssm_c_re, ssm_c_im, ssm_d, w_glu, w_out, norm_ffn_g,
           w_router_group, b_router_group, w_router_expert, b_router_expert,
           w_gate, w_up, w_down, norm_final_g):
    f = lambda a: np.ascontiguousarray(np.asarray(a, dtype=np.float32))
    x = f(x)
    shared = dict(_host_consts())
    shared["meta"] = f(meta_tokens)
    shared["gmix"] = f(np.asarray(norm_mix_g)[0].reshape(8, 128).T)
    shared["gffn"] = f(np.asarray(norm_ffn_g)[0].reshape(8, 128).T)
    shared["gfin"] = f(np.broadcast_to(np.asarray(norm_final_g)[None, :], (128, D)))
    shared["w_in"] = f(np.asarray(w_in)[0])
    shared["w_out"] = f(np.asarray(w_out)[0])
    shared["w_glu"] = f(np.asarray(w_glu)[0])
    shared["w_gate"] = f(np.asarray(w_gate)[0])
    shared["w_up"] = f(np.asarray(w_up)[0])
    shared["w_down"] = f(np.asarray(w_down)[0])
    wre = np.asarray(w_router_expert)[0]
    shared["wr"] = f(np.concatenate([np.asarray(w_router_group)[0], wre.transpose(1, 0, 2).reshape(D, 16)], axis=1))
    brv = np.concatenate([np.asarray(b_router_group)[0], np.asarray(b_router_expert)[0].reshape(16)])
    shared["br"] = f(np.broadcast_to(brv[None, :], (128, 20)))
    def srow(a):
        return f(np.asarray(a).reshape(8, 2, 64).transpose(1, 2, 0).reshape(128, 8))
    shared["lre"] = srow(np.asarray(ssm_lambda_re)[0])
    shared["lim"] = srow(np.asarray(ssm_lambda_im)[0])
    shared["ldt"] = f(np.broadcast_to(np.asarray(ssm_log_dt)[0].reshape(8, 2)[None, :, :], (64, 8, 2)).transpose(2, 0, 1).reshape(128, 8))
    bre = np.asarray(ssm_b_re)[0]
    bim = np.asarray(ssm_b_im)[0]
    cre = np.asarray(ssm_c_re)[0]
    cim = np.asarray(ssm_c_im)[0]
    dsk = np.asarray(ssm_d)[0]
    Blh = np.zeros((128, 2, 4, 2, 128), np.float32)
    Clh = np.zeros((128, 8, 2, 128), np.float32)
    Dlh = np.zeros((128, 2, 128), np.float32)
    for g in range(16):
        k, q = g // 2, g % 2
        j, kk = k // 4, k % 4
        gl = g - 8 * j
        rows = slice(q * 64, q * 64 + 64)
        cls = slice(gl * 16, gl * 16 + 16)
        Blh[cls, j, kk, 0, rows] = bre[g].T
        Blh[cls, j, kk, 1, rows] = bim[g].T
        Clh[rows, k, 0, cls] = cre[g].T
        Clh[rows, k, 1, cls] = cim[g].T
        for h in range(16):
            Dlh[gl * 16 + h, j, gl * 16 + h] = dsk[g, h]
    shared["Bl"] = Blh.reshape(128, 2048)
    shared["Cl"] = Clh.reshape(128, 2048)
    shared["Dl"] = Dlh.reshape(128, 256)
    if "nc" not in _NC_CACHE:
        _NC_CACHE["nc"] = build_nc()
    nc = _NC_CACHE["nc"]
    in_maps = []
    for b in range(8):
        m = dict(shared)
        m["x"] = np.ascontiguousarray(x[b])
        in_maps.append(m)
    res = run_bass_kernel_spmd(nc, in_maps, core_ids=list(range(8)))
    return np.stack([r["out"] for r in res.results], axis=0).astype(np.float32)
```

```python
import numpy as np
from contextlib import ExitStack
import concourse.bass as bass
import concourse.mybir as mybir
from concourse.bass_utils import run_bass_kernel_spmd

F32 = mybir.dt.float32
BF16 = mybir.dt.bfloat16
AF = mybir.ActivationFunctionType
ALU = mybir.AluOpType
AX = mybir.AxisListType

D = 1024
SEQ = 4096
NT = 33
LP = NT * 128
EPS = 1e-6
NH = 6
NE = 16
INW = 3328
TB = 512
NB = (LP + TB - 1) // TB
TS = 256
NBS = (LP + TS - 1) // TS
GELU_C = 1.5957691216057308
SAME_ENG_WAR = True


class Buf:
    def __init__(self, name, t):
        self.name = name
        self.t = t
        self.lw = None
        self.rd = []
        self.sem = None
        self.nd = 0

    def __getitem__(self, k):
        return self.t[k]


class Op:
    __slots__ = ("eng", "fn", "deps", "sig", "cnt", "epoch", "is_dma", "dbuf", "dcnt")


class Prog:
    ENG = ["pe", "act", "dve", "pool", "sp"]

    NPROG = [0]
    TOT = [0]
    PH = {}

    def __init__(self, nc, es, semes):
        Prog.NPROG[0] += 1
        self.tag = "g%d" % Prog.NPROG[0]
        self.nc = nc
        self.es = es
        self.semes = semes
        self.ops = {e: [] for e in self.ENG}
        self.epoch = 0
        self.sems = {}
        self.out_dmas = []

    def buf(self, name, shape, dt, ps=False):
        if not ps:
            nb = int(np.prod(shape[1:])) * (2 if dt == BF16 else 4)
            Prog.TOT[0] += nb
            Prog.PH[self.tag] = Prog.PH.get(self.tag, 0) + nb
        if ps:
            t = self.es.enter_context(self.nc.psum_tensor(self.tag + name, shape, dt))
        else:
            t = self.es.enter_context(self.nc.sbuf_tensor(self.tag + name, shape, dt))
        return Buf(name, t)

    def new_epoch(self):
        self.epoch += 1

    def _mk(self, eng, fn, reads, writes):
        op = Op()
        op.eng = eng
        op.fn = fn
        op.deps = set()
        op.sig = False
        op.cnt = 0
        op.epoch = self.epoch
        op.is_dma = False
        op.dbuf = None
        op.dcnt = 0
        for b in reads:
            if b.lw is not None:
                op.deps.add(b.lw)
        for b in writes:
            if b.lw is not None:
                op.deps.add(b.lw)
            for r in b.rd:
                if SAME_ENG_WAR or r.is_dma or r.eng != eng:
                    op.deps.add(r)
        for b in reads:
            b.rd.append(op)
        for b in writes:
            b.lw = op
            b.rd = []
        self.ops[eng].append(op)
        return op

    def op(self, eng, fn, reads=(), writes=()):
        op = self._mk(eng, fn, reads, writes)
        if eng == "pe":
            op.deps = {d for d in op.deps if d.is_dma or d.eng != "pe"}
        return op

    def dma(self, eng, out, in_, reads=(), writes=(), sigbuf=None):
        def fn(e, out=out, in_=in_):
            return e.dma_start(out=out, in_=in_)
        op = self._mk(eng, fn, reads, writes)
        op.is_dma = True
        b = sigbuf if sigbuf is not None else (writes[0] if writes else reads[0])
        if b.sem is None:
            b.sem = self.semes.enter_context(self.nc.semaphore(self.tag + "d_" + b.name))
        b.nd += 1
        op.dbuf = b
        op.dcnt = b.nd
        return op

    def emit(self, final_waits=()):
        nc = self.nc
        allops = [o for e in self.ENG for o in self.ops[e]]
        for o in allops:
            for d in o.deps:
                if not d.is_dma:
                    d.sig = True
        for e in self.ENG:
            cnts = {}
            for o in self.ops[e]:
                if o.sig and not o.is_dma:
                    cnts[o.epoch] = cnts.get(o.epoch, 0) + 1
                    o.cnt = cnts[o.epoch]
                    key = (e, o.epoch)
                    if key not in self.sems:
                        self.sems[key] = self.semes.enter_context(nc.semaphore(self.tag + "s_%s_%d" % key))
        prog = self

        def run(eng_name, e):
            waited = {}
            for o in prog.ops[eng_name]:
                need = {}
                for d in o.deps:
                    if d.is_dma:
                        sem, val = d.dbuf.sem, 16 * d.dcnt
                    else:
                        sem, val = prog.sems[(d.eng, d.epoch)], d.cnt
                    k = id(sem)
                    if k not in need or need[k][1] < val:
                        need[k] = (sem, val)
                for k, (sem, val) in need.items():
                    if waited.get(k, 0) >= val:
                        continue
                    e.wait_ge(sem, val)
                    waited[k] = val
                ins = o.fn(e)
                if o.is_dma:
                    ins.then_inc(o.dbuf.sem, 16)
                elif o.sig:
                    ins.then_inc(prog.sems[(o.eng, o.epoch)], 1)
            if eng_name == "sp":
                for b in final_waits:
                    if b.sem is not None:
                        e.wait_ge(b.sem, 16 * b.nd)

        with nc.Block() as block:
            @block.tensor
            def _(e):
                run("pe", e)

            @block.scalar
            def _(e):
                run("act", e)

            @block.vector
            def _(e):
                run("dve", e)

            @block.gpsimd
            def _(e):
                run("pool", e)

            @block.sync
            def _(e):
                run("sp", e)


def bc(ap, shape):
    return ap.to_broadcast(shape)


def build_nc():
    nc = bass.Bass("TRN2", target_bir_lowering=False)

    def din(name, shape, dt=F32):
        return nc.dram_tensor(name, list(shape), dt, kind="ExternalInput").ap()

    x = din("x", [SEQ, D])
    meta = din("meta", [16, D])
    gmix = din("gmix", [128, 8])
    gffn = din("gffn", [128, 8])
    gfin = din("gfin", [128, D])
    w_in = din("w_in", [D, INW])
    w_out = din("w_out", [D, D])
    w_glu = din("w_glu", [256, 256])
    w_gate = din("w_gate", [NE, D, 256])
    w_up = din("w_up", [NE, D, 256])
    w_down = din("w_down", [NE, 256, D])
    wr = din("wr", [D, 20])
    br = din("br", [128, 20])
    lre = din("lre", [128, 8])
    lim = din("lim", [128, 8])
    ldt = din("ldt", [128, 8])
    Bl = din("Bl", [128, 2048])
    Cl = din("Cl", [128, 2048])
    Dl = din("Dl", [128, 256])
    cosT = din("cosT", [128, LP])
    sinT = din("sinT", [128, LP])
    maskT = din("maskT", [128, NH * 128])
    qdec = din("qdec", [128, NH])
    kdec = din("kdec", [128, NH])
    identf = din("identf", [128, 128])
    out = nc.dram_tensor("out", [SEQ, D], F32, kind="ExternalOutput").ap()
    sc_gate = nc.dram_tensor("sc_gate", [NE, 128, 2048], BF16).ap()
    sc_up = nc.dram_tensor("sc_up", [NE, 128, 2048], BF16).ap()
    sc_down = nc.dram_tensor("sc_down", [NE, 128, 2048], BF16).ap()

    gamma = [1.0 - 2.0 ** (-5.0 - h) for h in range(NH)]
    g128 = [float(g ** 128) for g in gamma]

    with ExitStack() as ges, ExitStack() as semes:
        G = Prog(nc, ges, semes)
        mixedT = G.buf("mixedT", [128, 8, LP], BF16)
        ident = G.buf("ident", [128, 128], BF16)
        gmix_s = G.buf("gmix_s", [128, 8], F32)
        gffn_s = G.buf("gffn_s", [128, 8], F32)
        rho = G.buf("rho", [128, 8], F32)
        qre = G.buf("qre", [128, 8], F32)
        qim = G.buf("qim", [128, 8], F32)
        ucs = G.buf("ucs", [128, 2, 8], F32)
        Bl_s = G.buf("Bl_s", [128, 2048], BF16)
        Cl_s = G.buf("Cl_s", [128, 2048], BF16)
        Dl_s = G.buf("Dl_s", [128, 256], BF16)
        wglu_s = G.buf("wglu_s", [128, 2, 256], BF16)
        mxb = [Buf("mx%d" % t, None) for t in range(NT)]

        with ExitStack() as es:
            P = Prog(nc, es, semes)
            idf = P.buf("idf", [128, 128], F32)
            P.dma("sp", idf[:], identf[:, :], writes=[idf])
            P.op("act", lambda e: e.activation(ident[:], idf[:], AF.Copy), [idf], [ident])
            P.dma("sp", gmix_s[:], gmix[:, :], writes=[gmix_s])
            P.dma("sp", gffn_s[:], gffn[:, :], writes=[gffn_s])
            stg = [P.buf("stg%d" % i, [128, 2048], F32) for i in range(2)]
            P.dma("sp", stg[0][:], Bl[:, :], writes=[stg[0]])
            P.op("act", lambda e: e.activation(Bl_s[:], stg[0][:], AF.Copy), [stg[0]], [Bl_s])
            P.dma("sp", stg[1][:], Cl[:, :], writes=[stg[1]])
            P.dma("sp", stg[0][:, 0:256], Dl[:, :], writes=[stg[0]])
            P.op("act", lambda e: e.activation(Dl_s[:], stg[0][:, 0:256], AF.Copy), [stg[0]], [Dl_s])
            P.dma("sp", stg[0][:, 512:1024].rearrange("p (c f) -> p c f", c=2),
                  w_glu.rearrange("(c p) f -> p c f", p=128), writes=[stg[0]])
            P.op("dve", lambda e: e.tensor_copy(wglu_s[:], stg[0][:, 512:1024].rearrange("p (c f) -> p c f", c=2)),
                 [stg[0]], [wglu_s])
            lre_s = P.buf("lre_s", [128, 8], F32)
            lim_s = P.buf("lim_s", [128, 8], F32)
            dt_s = P.buf("dt_s", [128, 8], F32)
            P.dma("sp", lre_s[:], lre[:, :], writes=[lre_s])
            P.dma("sp", lim_s[:], lim[:, :], writes=[lim_s])
            P.dma("sp", dt_s[:], ldt[:, :], writes=[dt_s])
            P.op("act", lambda e: e.activation(dt_s[:], dt_s[:], AF.Exp), [dt_s], [dt_s])
            tmpa = P.buf("tmpa", [128, 8], F32)
            tmpb = P.buf("tmpb", [128, 8], F32)
            th = P.buf("th", [128, 8], F32)
            P.op("dve", lambda e: e.tensor_tensor(tmpa[:], lre_s[:], dt_s[:], ALU.mult), [lre_s, dt_s], [tmpa])
            P.op("act", lambda e: e.activation(rho[:], tmpa[:], AF.Exp), [tmpa], [rho])
            P.op("dve", lambda e: e.tensor_tensor(th[:], lim_s[:], dt_s[:], ALU.mult), [lim_s, dt_s], [th])
            cc = P.buf("cc", [128, 8], F32)
            ss = P.buf("ss", [128, 8], F32)
            hp = P.buf("hp", [128, 1], F32)
            P.op("dve", lambda e: e.memset(hp[:], float(np.pi / 2)), [], [hp])
            P.op("act", lambda e: e.activation(ss[:], th[:], AF.Sin, scale=1.0 / 32), [th], [ss])
            P.op("act", lambda e: e.activation(cc[:], th[:], AF.Sin, scale=1.0 / 32, bias=hp[:, 0:1]), [th, hp], [cc])
            c2 = P.buf("c2", [128, 8], F32)
            s2 = P.buf("s2", [128, 8], F32)
            for it in range(5):
                P.op("dve", lambda e: e.tensor_tensor(c2[:], cc[:], cc[:], ALU.mult), [cc], [c2])
                P.op("dve", lambda e: e.tensor_tensor(s2[:], ss[:], ss[:], ALU.mult), [ss], [s2])
                P.op("dve", lambda e: e.tensor_tensor(tmpb[:], cc[:], ss[:], ALU.mult), [cc, ss], [tmpb])
                P.op("dve", lambda e: e.tensor_tensor(cc[:], c2[:], s2[:], ALU.subtract), [c2, s2], [cc])
                P.op("dve", lambda e: e.tensor_scalar(ss[:], tmpb[:], 2.0, None, ALU.mult), [tmpb], [ss])
            P.op("dve", lambda e: e.tensor_copy(ucs[:, 0, :], cc[:]), [cc], [ucs])
            P.op("dve", lambda e: e.tensor_copy(ucs[:, 1, :], ss[:]), [ss, ucs], [ucs])
            nre = P.buf("nre", [128, 8], F32)
            nim = P.buf("nim", [128, 8], F32)
            den = P.buf("den", [128, 8], F32)
            P.op("dve", lambda e: e.tensor_tensor(nre[:], rho[:], cc[:], ALU.mult), [rho, cc], [nre])
            P.op("dve", lambda e: e.tensor_scalar(nre[:], nre[:], -1.0, None, ALU.add), [nre], [nre])
            P.op("dve", lambda e: e.tensor_tensor(nim[:], rho[:], ss[:], ALU.mult), [rho, ss], [nim])
            P.op("dve", lambda e: e.tensor_tensor(den[:], lre_s[:], lre_s[:], ALU.mult), [lre_s], [den])
            P.op("dve", lambda e: e.tensor_tensor(tmpa[:], lim_s[:], lim_s[:], ALU.mult), [lim_s], [tmpa])
            P.op("dve", lambda e: e.tensor_tensor(den[:], den[:], tmpa[:], ALU.add), [den, tmpa], [den])
            P.op("dve", lambda e: e.reciprocal(den[:], den[:]), [den], [den])
            P.op("dve", lambda e: e.tensor_tensor(tmpa[:], nre[:], lre_s[:], ALU.mult), [nre, lre_s], [tmpa])
            P.op("dve", lambda e: e.tensor_tensor(tmpb[:], nim[:], lim_s[:], ALU.mult), [nim, lim_s], [tmpb])
            P.op("dve", lambda e: e.tensor_tensor(tmpa[:], tmpa[:], tmpb[:], ALU.add), [tmpa, tmpb], [tmpa])
            P.op("dve", lambda e: e.tensor_tensor(qre[:], tmpa[:], den[:], ALU.mult), [tmpa, den], [qre])
            P.op("dve", lambda e: e.tensor_tensor(tmpa[:], nim[:], lre_s[:], ALU.mult), [nim, lre_s], [tmpa])
            P.op("dve", lambda e: e.tensor_tensor(tmpb[:], nre[:], lim_s[:], ALU.mult), [nre, lim_s], [tmpb])
            P.op("dve", lambda e: e.tensor_tensor(tmpa[:], tmpa[:], tmpb[:], ALU.subtract), [tmpa, tmpb], [tmpa])
            P.op("dve", lambda e: e.tensor_tensor(qim[:], tmpa[:], den[:], ALU.mult), [tmpa, den], [qim])
            nqim = P.buf("nqim", [128, 8], F32)
            tq = P.buf("tq", [128, 128], F32)
            P.op("dve", lambda e: e.tensor_scalar(nqim[:], qim[:], -1.0, None, ALU.mult), [qim], [nqim])
            for k in range(8):
                cre_ = slice((2 * k) * 128, (2 * k + 1) * 128)
                cim_ = slice((2 * k + 1) * 128, (2 * k + 2) * 128)
                P.op("dve", lambda e, k=k, cre_=cre_: e.tensor_scalar(tq[:], stg[1][:, cre_], qre[:, k:k + 1], None, ALU.mult), [stg[1], qre], [tq])
                P.op("dve", lambda e, k=k, cre_=cre_, cim_=cim_: e.scalar_tensor_tensor(Cl_s[:, cre_], stg[1][:, cim_], nqim[:, k:k + 1], tq[:], ALU.mult, ALU.add), [stg[1], nqim, tq], [Cl_s])
                P.op("dve", lambda e, k=k, cre_=cre_: e.tensor_scalar(tq[:], stg[1][:, cre_], qim[:, k:k + 1], None, ALU.mult), [stg[1], qim], [tq])
                P.op("dve", lambda e, k=k, cim_=cim_: e.scalar_tensor_tensor(Cl_s[:, cim_], stg[1][:, cim_], qre[:, k:k + 1], tq[:], ALU.mult, ALU.add), [stg[1], qre, tq, Cl_s], [Cl_s])
            P.emit(final_waits=[gmix_s, gffn_s])
        nc.all_engine_barrier()
        for b in [mixedT, ident, gmix_s, gffn_s, rho, ucs, qre, qim, Bl_s, Cl_s, Dl_s, wglu_s]:
            b.lw = None
            b.rd = []

        with ExitStack() as es:
            P = Prog(nc, es, semes)
            Win = P.buf("Win", [128, 8, INW], BF16)
            xb = [P.buf("xb%d" % i, [128, D], F32) for i in range(2)]
            ab = [P.buf("ab%d" % i, [128, D], BF16) for i in range(2)]
            aTs = [P.buf("aT%d" % i, [128, 8, 256], BF16) for i in range(2)]
            qks = [P.buf("qk%d" % i, [128, 12, 256], BF16) for i in range(2)]
            qraw = [P.buf("qraw%d" % i, [128, 256], F32) for i in range(2)]
            rA = [P.buf("rA%d" % i, [128, 256], F32) for i in range(2)]
            rB = [P.buf("rB%d" % i, [128, 256], F32) for i in range(2)]
            cs = [P.buf("cs0", [128, 2, 256], F32)] * 2
            vg = [P.buf("vg%d" % i, [128, 1536], BF16) for i in range(4)]
            khat = P.buf("khat", [128, NH, 128], BF16)
            Sm = [P.buf("Sm%d" % i, [128, 3, 128], BF16) for i in range(2)]
            ctmp = P.buf("ctmp", [128, 3, 128], F32)
            o = P.buf("o", [128, NH, 128], F32)
            sg = P.buf("sg", [128, NH * 128], F32)
            yr = P.buf("yr", [128, NH * 128], BF16)
            R = P.buf("R", [128, NH, 128], F32)
            Rb = P.buf("Rb", [128, NH, 128], BF16)
            mask_s = P.buf("mask_s", [128, NH, 128], F32)
            qdec_s = P.buf("qdec_s", [128, NH], F32)
            kdec_s = P.buf("kdec_s", [128, NH], F32)
            st = [P.buf("st%d" % i, [128, 8], F32) for i in range(2)]
            gst = P.buf("gst", [128, 4, NH], F32)
            psT = P.buf("psT", [128, 8, 128], BF16, ps=True)
            psq = [P.buf("psq%d" % i, [128, 512], F32, ps=True) for i in range(2)]
            psB = P.buf("psB", [128, 8, 128], BF16, ps=True)
            psS = P.buf("psS", [128, 4, 128], F32, ps=True)
            psO = P.buf("psO", [128, 4, 128], F32, ps=True)
            psC = P.buf("psC", [128, 4, 128], F32, ps=True)
            psKV = P.buf("psKV", [128, 4, 128], F32, ps=True)

            P.dma("sp", mask_s[:], maskT.rearrange("p (h i) -> p h i", h=NH), writes=[mask_s])
            P.dma("sp", qdec_s[:], qdec[:, :], writes=[qdec_s])
            P.dma("sp", kdec_s[:], kdec[:, :], writes=[kdec_s])
            P.op("dve", lambda e: e.memset(R[:], 0.0), [], [R])
            P.op("pool", lambda e: e.memset(Rb[:], 0.0), [], [Rb])
            win_v = w_in.rearrange("(c p) n -> p c n", p=128)
            pieces = [(0, 1024), (1024, 1024), (2048, 1024), (3072, 256)]
            ci = 0
            pieces = [(0, 768), (768, 768), (1536, 768), (2304, 768), (3072, 256)]
            stg_bufs = [xb[0], xb[1], o, sg]
            stg_views = [xb[0][:], xb[1][:], o[:].rearrange("p h e -> p (h e)"), sg[:]]
            for c in range(8):
                for (c0, w) in pieces:
                    sb = stg_bufs[ci % 4]
                    sbv = stg_views[ci % 4]
                    P.dma("sp" if ci % 2 == 0 else "act", sbv[:, 0:w], win_v[:, c, c0:c0 + w], writes=[sb])
                    eng = ["act", "dve", "dve", "pool"][ci % 4]
                    if eng == "act":
                        P.op("act", lambda e, sbv=sbv, c=c, c0=c0, w=w: e.activation(Win[:, c, c0:c0 + w], sbv[:, 0:w], AF.Copy), [sb], [Win])
                    else:
                        P.op(eng, lambda e, sbv=sbv, c=c, c0=c0, w=w: e.tensor_copy(Win[:, c, c0:c0 + w], sbv[:, 0:w]), [sb], [Win])
                    ci += 1

            def load_x_tile(t, dst):
                if t == 0:
                    P.op("pool", lambda e: e.memset(dst[:], 0.0), [], [dst])
                    P.dma("sp", dst[48:64, :], meta[:, :], writes=[dst])
                    P.dma("sp", dst[64:128, :], x[0:64, :], writes=[dst])
                elif t == NT - 1:
                    P.op("pool", lambda e: e.memset(dst[:], 0.0), [], [dst])
                    P.dma("sp", dst[0:64, :], x[SEQ - 64:SEQ, :], writes=[dst])
                else:
                    P.dma("sp", dst[:], x[128 * t - 64:128 * t + 64, :], writes=[dst])

            def rms_to_bf(src, dst_bf, stt, scratch):
                P.op("act", lambda e: e.activation(scratch[:], src[:], AF.Square, accum_out=stt[:, 0:1]), [src], [scratch, stt])
                P.op("dve", lambda e: e.tensor_scalar(stt[:, 1:2], stt[:, 0:1], 1.0 / D, EPS, ALU.mult, ALU.add), [stt], [stt])
                P.op("act", lambda e: e.activation(stt[:, 2:3], stt[:, 1:2], AF.Sqrt), [stt], [stt])
                P.op("dve", lambda e: e.reciprocal(stt[:, 3:4], stt[:, 2:3]), [stt], [stt])
                P.op("act", lambda e: e.activation(dst_bf[:], src[:], AF.Copy, scale=stt[:, 3:4]), [src, stt], [dst_bf])

            NCB = 2
            cst = [P.buf("cst%d" % i, [128, 512], F32) for i in range(NCB)]
            cbf = [P.buf("cbf%d" % i, [128, 512], BF16) for i in range(NCB)]
            conv = []
            for e_ in range(NE):
                gv = w_gate[e_].rearrange("(c p) f -> p c f", p=128)
                uv = w_up[e_].rearrange("(c p) f -> p c f", p=128)
                dv = w_down[e_].rearrange("(c p) f -> p c f", p=128)
                for q4 in range(4):
                    conv.append((gv[:, 2 * q4:2 * q4 + 2, :], 2, sc_gate[e_][:, 512 * q4:512 * q4 + 512]))
                    conv.append((uv[:, 2 * q4:2 * q4 + 2, :], 2, sc_up[e_][:, 512 * q4:512 * q4 + 512]))
                    conv.append((dv[:, q4 // 2:q4 // 2 + 1, 512 * (q4 % 2):512 * (q4 % 2) + 512], 1, sc_down[e_][:, 512 * q4:512 * q4 + 512]))
            cvi = [0]
            LAG = 1

            def do_conv(kn):
                for _ in range(kn):
                    i = cvi[0]
                    cvi[0] += 1
                    if i < len(conv):
                        src, nc_, dst = conv[i]
                        bi = i % NCB
                        P.dma("sp", cst[bi][:].rearrange("p (c f) -> p c f", c=nc_), src, writes=[cst[bi]])
                    j = i - LAG
                    if 0 <= j < len(conv):
                        src, nc_, dst = conv[j]
                        bj = j % NCB
                        P.op("act", lambda e, bj=bj: e.activation(cbf[bj][:], cst[bj][:], AF.Copy), [cst[bj]], [cbf[bj]])
                        P.dma("sp", dst, cbf[bj][:], reads=[cbf[bj]], writes=[], sigbuf=cbf[bj])

            tcount = 0
            ngroups = (NT + 1) // 2

            def ret_pieces(tiles, qk, vgs):
                pcs = []
                for li, t in enumerate(tiles):
                    vt = vgs[li]
                    cols = slice(li * 128, (li + 1) * 128)

                    def p0(cols=cols):
                        def trk(e):
                            ins = None
                            for h in range(NH):
                                ins = e.transpose(psB[:, h, :], qk[:, 6 + h, cols], ident[:])
                            return ins
                        P.op("pe", trk, [qk, ident], [psB])
                        P.op("dve", lambda e: e.tensor_tensor(khat[:], psB[:, 0:NH, :], bc(kdec_s[:].unsqueeze(2), [128, NH, 128]), ALU.mult), [psB, kdec_s], [khat])
                    pcs.append(p0)
                    for half in range(2):
                        h0 = 3 * half
                        smb = Sm[half]

                        def p1(h0=h0, smb=smb, cols=cols):
                            def mmS(e):
                                ins = None
                                for hl in range(3):
                                    ins = e.matmul(psS[:, hl, :], qk[:, 6 + h0 + hl, cols], qk[:, h0 + hl, cols], start=True, stop=True)
                                return ins
                            P.op("pe", mmS, [qk], [psS])
                            P.op("dve", lambda e: e.tensor_tensor(smb[:], psS[:, 0:3, :], mask_s[:, h0:h0 + 3, :], ALU.mult), [psS, mask_s], [smb])

                            def mmC(e):
                                ins = None
                                for hl in range(3):
                                    ins = e.matmul(psC[:, hl, :], qk[:, h0 + hl, cols], Rb[:, h0 + hl, :], start=True, stop=True)
                                return ins
                            P.op("pe", mmC, [qk, Rb], [psC])
                            P.op("dve", lambda e: e.tensor_tensor(ctmp[:], psC[:, 0:3, :], bc(qdec_s[:, h0:h0 + 3].unsqueeze(2), [128, 3, 128]), ALU.mult), [psC, qdec_s], [ctmp])
                        pcs.append(p1)

                        def p2(h0=h0, smb=smb, vt=vt):
                            def mmO(e):
                                ins = None
                                for hl in range(3):
                                    h = h0 + hl
                                    ins = e.matmul(psO[:, hl, :], smb[:, hl, :], vt[:, h * 128:(h + 1) * 128], start=True, stop=True)
                                return ins
                            P.op("pe", mmO, [smb, vt], [psO])

                            def mmKV(e):
                                ins = None
                                for hl in range(3):
                                    h = h0 + hl
                                    ins = e.matmul(psKV[:, hl, :], khat[:, h, :], vt[:, h * 128:(h + 1) * 128], start=True, stop=True)
                                return ins
                            P.op("pe", mmKV, [khat, vt], [psKV])
                            P.op("dve", lambda e: e.tensor_tensor(o[:, h0:h0 + 3, :], psO[:, 0:3, :], ctmp[:], ALU.add), [psO, ctmp], [o])
                            for hl in range(3):
                                h = h0 + hl
                                P.op("dve", lambda e, h=h, hl=hl: e.scalar_tensor_tensor(R[:, h, :], R[:, h, :], g128[h], psKV[:, hl, :], ALU.mult, ALU.add), [R, psKV], [R])
                        pcs.append(p2)

                        def p2b(h0=h0):
                            P.op("act", lambda e: e.activation(Rb[:, h0:h0 + 3, :], R[:, h0:h0 + 3, :], AF.Copy), [R], [Rb])
                        pcs.append(p2b)

                    def p3a():
                        P.op("act", lambda e: e.activation(sg[:].rearrange("p (h e) -> p h e", h=NH), o[:], AF.Square), [o], [sg])
                    pcs.append(p3a)

                    def p3b():
                        P.op("dve", lambda e: e.tensor_reduce(gst[:, 0, :], o[:], AX.X, ALU.add), [o], [gst])
                        P.op("dve", lambda e: e.tensor_reduce(gst[:, 1, :], sg[:].rearrange("p (h e) -> p h e", h=NH), AX.X, ALU.add), [sg, gst], [gst])
                        P.op("dve", lambda e: e.tensor_scalar(gst[:, 0, :], gst[:, 0, :], 1.0 / 128, None, ALU.mult), [gst], [gst])
                        P.op("dve", lambda e: e.tensor_tensor(gst[:, 2, :], gst[:, 0, :], gst[:, 0, :], ALU.mult), [gst], [gst])
                        P.op("dve", lambda e: e.scalar_tensor_tensor(gst[:, 1, :], gst[:, 1, :], 1.0 / 128, gst[:, 2, :], ALU.mult, ALU.subtract), [gst], [gst])
                        P.op("dve", lambda e: e.tensor_scalar(gst[:, 1, :], gst[:, 1, :], EPS, None, ALU.add), [gst], [gst])
                    pcs.append(p3b)

                    def p3c(vt=vt):
                        P.op("act", lambda e: e.activation(gst[:, 2, :], gst[:, 1, :], AF.Sqrt), [gst], [gst])
                    pcs.append(p3c)

                    def p4a():
                        P.op("dve", lambda e: e.reciprocal(gst[:, 3, :], gst[:, 2, :]), [gst], [gst])
                        P.op("dve", lambda e: e.tensor_tensor(o[:], o[:], bc(gst[:, 0, :].unsqueeze(2), [128, NH, 128]), ALU.subtract), [o, gst], [o])
                        P.op("dve", lambda e: e.tensor_tensor(o[:], o[:], bc(gst[:, 3, :].unsqueeze(2), [128, NH, 128]), ALU.mult), [o, gst], [o])
                    pcs.append(p4a)

                    def p4b(vt=vt):
                        P.op("act", lambda e: e.activation(sg[:], vt[:, 768:1536], AF.Silu), [vt], [sg])
                    pcs.append(p4b)

                    def p5():
                        P.op("pool", lambda e: e.tensor_tensor(yr[:], o[:].rearrange("p h e -> p (h e)"), sg[:], ALU.mult), [o, sg], [yr])
                    pcs.append(p5)

                    def p6(t=t):
                        def try_(e):
                            ins = None
                            for h in range(NH):
                                ins = e.transpose(psB[:, h, :], yr[:, h * 128:(h + 1) * 128], ident[:])
                            return ins
                        P.op("pe", try_, [yr, ident], [psB])
                        P.op("act", lambda e: e.activation(mixedT[:, 2:8, 128 * t:128 * (t + 1)], psB[:, 0:NH, :], AF.Copy), [psB], [mxb[t]])
                    pcs.append(p6)
                return pcs

            def A_pieces(gi):
                nonlocal_t = []
                tiles_ = [t for t in (2 * gi, 2 * gi + 1) if t < NT]
                aTn = aTs[gi % 2]
                pcs = []
                for li, t in enumerate(tiles_):
                    k_ = tcnt[0]
                    tcnt[0] += 1
                    xs, a_, stt = xb[k_ % 2], ab[k_ % 2], st[k_ % 2]

                    def pa1(t=t, xs=xs):
                        load_x_tile(t, xs)
                    pcs.append(pa1)

                    def pa2(xs=xs, a_=a_, stt=stt):
                        P.op("act", lambda e: e.activation(a_[:], xs[:], AF.Square, accum_out=stt[:, 0:1]), [xs], [a_, stt])
                    pcs.append(pa2)

                    def pb1(stt=stt):
                        P.op("dve", lambda e: e.tensor_scalar(stt[:, 1:2], stt[:, 0:1], 1.0 / D, EPS, ALU.mult, ALU.add), [stt], [stt])
                        P.op("act", lambda e: e.activation(stt[:, 2:3], stt[:, 1:2], AF.Sqrt), [stt], [stt])
                    pcs.append(pb1)

                    def pb2(xs=xs, a_=a_, stt=stt):
                        P.op("dve", lambda e: e.reciprocal(stt[:, 3:4], stt[:, 2:3]), [stt], [stt])
                        P.op("act", lambda e: e.activation(a_[:], xs[:], AF.Copy, scale=stt[:, 3:4]), [xs, stt], [a_])
                    pcs.append(pb2)

                    def pc(li=li, a_=a_, aTn=aTn):
                        def tr(e):
                            ins = None
                            for c in range(8):
                                ins = e.transpose(psT[:, c, :], a_[:, c * 128:(c + 1) * 128], ident[:])
                            return ins
                        P.op("pe", tr, [a_, ident], [psT])
                        P.op("dve", lambda e: e.tensor_tensor(aTn[:, :, li * 128:(li + 1) * 128], psT[:], bc(gmix_s[:].unsqueeze(2), [128, 8, 128]), ALU.mult), [psT, gmix_s], [aTn])
                    pcs.append(pc)
                return pcs

            tcnt = [0]
            for fn in A_pieces(0):
                fn()
            pending = []
            for gi in range(ngroups):
                if gi % 4 == 0:
                    P.new_epoch()
                tiles = [t for t in (2 * gi, 2 * gi + 1) if t < NT]
                n = 128 * len(tiles)
                tok0 = 128 * tiles[0]
                mxs = [mxb[t] for t in tiles]
                csb = cs[gi % 2]
                qk = qks[gi % 2]
                aT = aTs[gi % 2]
                vgs = vg[2 * (gi % 2):2 * (gi % 2) + 2]
                nextA = A_pieces(gi + 1) if gi + 1 < ngroups else []
                P.dma("act", csb[:, 0, 0:n], cosT[:, tok0:tok0 + n], writes=[csb])
                P.dma("act", csb[:, 1, 0:n], sinT[:, tok0:tok0 + n], writes=[csb])
                pi = 0
                for blk in range(14):
                    ps = psq[pi % 2]
                    pi += 1
                    col0 = blk * 128

                    def mm(e, ps=ps, col0=col0, n=n, aT=aT):
                        ins = None
                        for c in range(8):
                            ins = e.matmul(ps[:, 0:n], Win[:, c, col0:col0 + 128], aT[:, c, 0:n], start=(c == 0), stop=(c == 7))
                        return ins
                    P.op("pe", mm, [Win, aT], [ps])
                    if blk < 2:
                        P.op("act", lambda e, ps=ps, blk=blk, n=n, tok0=tok0: e.activation(mixedT[:, blk, tok0:tok0 + n], ps[:, 0:n], AF.Copy), [ps], mxs)
                    else:
                        hh = blk - 2
                        qr = qraw[hh % 2]
                        A = rA[hh % 2]
                        B = rB[hh % 2]
                        P.op("act", lambda e, ps=ps, qr=qr, n=n: e.activation(qr[:, 0:n], ps[:, 0:n], AF.Copy), [ps], [qr])
                        P.op("dve", lambda e, qr=qr, A=A, n=n, csb=csb: e.tensor_tensor(A[:, 0:n], qr[:, 0:n], csb[:, 0, 0:n], ALU.mult), [qr, csb], [A])
                        P.op("dve", lambda e, qr=qr, B=B, n=n, csb=csb: e.tensor_tensor(B[0:64, 0:n], qr[64:128, 0:n], csb[64:128, 1, 0:n], ALU.mult), [qr, csb], [B])
                        P.op("dve", lambda e, qr=qr, B=B, n=n, csb=csb: e.tensor_tensor(B[64:128, 0:n], qr[0:64, 0:n], csb[0:64, 1, 0:n], ALU.mult), [qr, csb, B], [B])
                        P.op("pool", lambda e, A=A, B=B, hh=hh, n=n, qk=qk: e.tensor_tensor(qk[:, hh, 0:n], A[:, 0:n], B[:, 0:n], ALU.add), [A, B], [qk])
                        do_conv(1)
                    for _ in range(2 if blk % 2 == 1 else 1):
                        if pending:
                            pending.pop(0)()
                    if nextA and blk % 2 == 0:
                        nextA.pop(0)()
                for li, t in enumerate(tiles):
                    vt = vgs[li]
                    for cb in range(3):
                        ps = psq[pi % 2]
                        pi += 1

                        def mmv(e, ps=ps, li=li, cb=cb, aT=aT):
                            ins = None
                            for c in range(8):
                                ins = e.matmul(ps[:, :], aT[:, c, li * 128:(li + 1) * 128], Win[:, c, 1792 + cb * 512:1792 + (cb + 1) * 512], start=(c == 0), stop=(c == 7))
                            return ins
                        P.op("pe", mmv, [Win, aT], [ps])
                        P.op("act", lambda e, ps=ps, vt=vt, cb=cb: e.activation(vt[:, cb * 512:(cb + 1) * 512], ps[:, :], AF.Copy), [ps], [vt])
                        for _ in range(2):
                            if pending:
                                pending.pop(0)()
                        if nextA:
                            nextA.pop(0)()
                while pending:
                    pending.pop(0)()
                while nextA:
                    nextA.pop(0)()
                pending = ret_pieces(tiles, qk, vgs)
            while pending:
                pending.pop(0)()
            do_conv(len(conv) + LAG + 1 - cvi[0] if cvi[0] < len(conv) + LAG else 0)
            P.emit(final_waits=cbf)
        nc.all_engine_barrier()
        for b in [mixedT, ident, gmix_s, gffn_s, rho, ucs, qre, qim, Bl_s, Cl_s, Dl_s, wglu_s] + mxb:
            b.lw = None
            b.rd = []

        with ExitStack() as es:
            P = Prog(nc, es, semes)
            Wout = P.buf("Wout", [128, 8, D], BF16)
            wr_s = P.buf("wr_s", [128, 8, 20], BF16)
            br_s = P.buf("br_s", [128, 20], F32)
            gfin_s = P.buf("gfin_s", [128, D], F32)
            h2 = [P.buf("h2_%d" % i, [128, D], F32) for i in range(4)]
            tbf = [P.buf("tbf0", [128, D], BF16)] * 2
            tT = P.buf("tT", [128, 8, TB], BF16)
            comb = P.buf("comb", [128, 5, NE], F32)
            rt = P.buf("rt", [128, 5, 64], F32)
            st2 = P.buf("st2", [128, 8, 5], F32)
            st3 = P.buf("st3", [128, 8, 5], F32)
            wg = [P.buf("wg%d" % i, [128, 8, 256], BF16) for i in range(2)]
            wu = [P.buf("wu%d" % i, [128, 8, 256], BF16) for i in range(2)]
            wd = [P.buf("wd%d" % i, [128, 2, D], BF16) for i in range(2)]
            sil = [P.buf("sil%d" % i, [128, TB], BF16) for i in range(2)]
            actT = [P.buf("actT%d" % i, [128, 2, TB], BF16) for i in range(2)]
            psDs = [P.buf("psD%d" % i, [128, D], F32, ps=True) for i in range(2)]
            psGU = [P.buf("psGU%d" % i, [128, TB], F32, ps=True) for i in range(3)]
            psS5 = P.buf("psS5", [128, TB], F32, ps=True)
            psT2b = psS5
            psT2 = psS5[:].bitcast(BF16).rearrange("p (c t) -> p c t", c=8)
            psR = psS5
            dcount = [0]
            gucount = [0]
            Dre = P.buf("Dre", [128, 8, TS], F32)
            Dim = P.buf("Dim", [128, 8, TS], F32)
            bur = [P.buf("bur%d" % i, [128, TS], F32) for i in range(2)]
            bui = [P.buf("bui%d" % i, [128, TS], F32) for i in range(2)]
            mrs = [P.buf("mr%d" % i, [128, TS], F32) for i in range(2)]
            mis = [P.buf("mi%d" % i, [128, TS], F32) for i in range(2)]
            wrs = [P.buf("Wr%d" % i, [128, TS], F32) for i in range(2)]
            wis = [P.buf("Wi%d" % i, [128, TS], F32) for i in range(2)]
            ta = P.buf("ta", [128, TS], F32)
            tc = P.buf("tc", [128, TS], F32)
            tds = [[P.buf("td%d_%d" % (i, q), [128, TS], F32) for q in range(4)] for i in range(2)]
            Xb = P.buf("Xb", [128, 8, 2, TS], BF16)
            carry = P.buf("carry", [128, 8, 2], F32)
            w0 = P.buf("w0", [128, 8, 2], F32)
            w0s = P.buf("w0s", [128, 8, 2], F32)
            geT = P.buf("geT", [128, 2, TS], BF16)
            ysb = [P.buf("ysb%d" % i, [128, TS], F32) for i in range(2)]
            y2 = [P.buf("y2_%d" % i, [128, TS], F32) for i in range(2)]
            zz = y2
            sgm = [P.buf("sgm%d" % i, [128, TS], F32) for i in range(2)]
            _tv = tT[:].bitcast(F32)
            t1 = _tv[:, :, 0:TS // 2]
            t2 = _tv[:, :, TS // 2:TS]

            class VBuf(Buf):
                def __init__(self, name, ap):
                    Buf.__init__(self, name, None)
                    self.ap_ = ap

                def __getitem__(self, k):
                    return self.ap_[k]
            h2x = VBuf("h2x", Dre[:].rearrange("p k t -> p (k t)")[:, 0:D])
            dimv = Dim[:].bitcast(BF16).rearrange("p k t -> p (k t)")
            tTx_ap = dimv[:, 0:1024].rearrange("p (c t) -> p c t", c=8)
            actTx_ap = [dimv[:, 1024 + 256 * i:1024 + 256 * (i + 1)].rearrange("p (f t) -> p f t", f=2) for i in range(2)]
            silx_ap = [dimv[:, 1536 + 128 * i:1536 + 128 * (i + 1)] for i in range(2)]
            tTxb = Buf("tTxb", None)
            actTxb = [Buf("actTxb%d" % i, None) for i in range(2)]
            silxb = [Buf("silxb%d" % i, None) for i in range(2)]

            def tTa(c, li):
                return tT[:, c, li * 128:(li + 1) * 128] if li < 4 else tTx_ap[:, c, :]

            def tTbuf(li):
                return tT if li < 4 else tTxb

            wov = w_out.rearrange("(c p) n -> p c n", p=128)
            for c in range(8):
                sb = h2[2 + c % 2]
                P.dma("sp", sb[:], wov[:, c, :], writes=[sb])
                P.op("pool" if c % 2 else "act", (lambda e, sb=sb, c=c: e.tensor_copy(Wout[:, c, :], sb[:])) if c % 2 else (lambda e, sb=sb, c=c: e.activation(Wout[:, c, :], sb[:], AF.Copy)), [sb], [Wout])
            P.dma("sp", h2[2][:, 0:160].rearrange("p (c f) -> p c f", c=8), wr.rearrange("(c p) f -> p c f", p=128), writes=[h2[2]])
            P.op("act", lambda e: e.activation(wr_s[:], h2[2][:, 0:160].rearrange("p (c f) -> p c f", c=8), AF.Copy), [h2[2]], [wr_s])
            P.dma("sp", br_s[:], br[:, :], writes=[br_s])
            P.dma("sp", gfin_s[:], gfin[:, :], writes=[gfin_s])
            P.op("dve", lambda e: e.memset(carry[:], 0.0), [], [carry])
            P.op("dve", lambda e: e.memset(Dre[:, :, 0:1], 1.0), [], [Dre])
            P.op("dve", lambda e: e.memset(Dim[:, :, 0:1], 0.0), [], [Dim])
            P.op("dve", lambda e: e.tensor_copy(Dre[:, :, 1:2], ucs[:, 0, :].unsqueeze(2)), [ucs, Dre], [Dre])
            P.op("dve", lambda e: e.tensor_copy(Dim[:, :, 1:2], ucs[:, 1, :].unsqueeze(2)), [ucs, Dim], [Dim])
            n = 2
            while n < TS:
                h = n // 2
                P.op("dve", lambda e, h=h: e.tensor_tensor(t1[:, :, 0:1], Dre[:, :, h:h + 1], Dre[:, :, h:h + 1], ALU.mult), [Dre], [tT])
                P.op("dve", lambda e, h=h: e.tensor_tensor(t2[:, :, 0:1], Dim[:, :, h:h + 1], Dim[:, :, h:h + 1], ALU.mult), [Dim], [tT])
                P.op("dve", lambda e, n=n: e.tensor_tensor(Dre[:, :, n:n + 1], t1[:, :, 0:1], t2[:, :, 0:1], ALU.subtract), [tT, Dre], [Dre])
                P.op("dve", lambda e, h=h: e.tensor_tensor(t1[:, :, 0:1], Dre[:, :, h:h + 1], Dim[:, :, h:h + 1], ALU.mult), [Dre, Dim], [tT])
                P.op("dve", lambda e, n=n: e.tensor_scalar(Dim[:, :, n:n + 1], t1[:, :, 0:1], 2.0, None, ALU.mult), [tT, Dim], [Dim])
                m = n - 1
                P.op("dve", lambda e, n=n, m=m: e.tensor_tensor(t1[:, :, 0:m], Dre[:, :, 1:n], bc(Dre[:, :, n:n + 1], [128, 8, m]), ALU.mult), [Dre], [tT])
                P.op("dve", lambda e, n=n, m=m: e.tensor_tensor(t2[:, :, 0:m], Dim[:, :, 1:n], bc(Dim[:, :, n:n + 1], [128, 8, m]), ALU.mult), [Dim], [tT])
                P.op("dve", lambda e, n=n, m=m: e.tensor_tensor(Dre[:, :, n + 1:2 * n], t1[:, :, 0:m], t2[:, :, 0:m], ALU.subtract), [tT, Dre], [Dre])
                P.op("dve", lambda e, n=n, m=m: e.tensor_tensor(t1[:, :, 0:m], Dre[:, :, 1:n], bc(Dim[:, :, n:n + 1], [128, 8, m]), ALU.mult), [Dre, Dim], [tT])
                P.op("dve", lambda e, n=n, m=m: e.tensor_tensor(t2[:, :, 0:m], Dim[:, :, 1:n], bc(Dre[:, :, n:n + 1], [128, 8, m]), ALU.mult), [Dre, Dim], [tT])
                P.op("dve", lambda e, n=n, m=m: e.tensor_tensor(Dim[:, :, n + 1:2 * n], t1[:, :, 0:m], t2[:, :, 0:m], ALU.add), [tT, Dim], [Dim])
                n *= 2

            def s5_sched(bs, off, sched, fast=False):
                e1 = "dve" if fast else "pool"
                c0 = bs * TS
                n = min(TS, LP - c0)
                tl = [mxb[t] for t in range(c0 // 128, (c0 + n) // 128)]

                def at(slot, fn):
                    sched.setdefault(slot, []).append(fn)
                def pre():
                    P.op("dve", lambda e: e.tensor_tensor(w0[:, :, 0], carry[:, :, 0], ucs[:, 0, :], ALU.mult), [carry, ucs], [w0])
                    P.op("dve", lambda e: e.tensor_tensor(w0[:, :, 1], carry[:, :, 1], ucs[:, 1, :], ALU.mult), [carry, ucs, w0], [w0])
                    P.op("dve", lambda e: e.tensor_tensor(w0s[:, :, 0], w0[:, :, 0], w0[:, :, 1], ALU.subtract), [w0], [w0s])
                    P.op("dve", lambda e: e.tensor_tensor(w0[:, :, 0], carry[:, :, 0], ucs[:, 1, :], ALU.mult), [carry, ucs, w0], [w0])
                    P.op("dve", lambda e: e.tensor_tensor(w0[:, :, 1], carry[:, :, 1], ucs[:, 0, :], ALU.mult), [carry, ucs, w0], [w0])
                    P.op("dve", lambda e: e.tensor_tensor(w0s[:, :, 1], w0[:, :, 0], w0[:, :, 1], ALU.add), [w0], [w0s])
                at(off + 1, pre)
                hA, hB = slice(0, n), slice(256, 256 + n)
                for k in range(8):
                    par = k % 2
                    j, kk = k // 4, k % 4
                    br_, bi_, mr_, mi_, wr_, wi_ = bur[par], bui[par], mrs[par], mis[par], wrs[par], wis[par]
                    d0, d1, d2, d3 = tds[par]

                    def st0(k=k, j=j, kk=kk, br_=br_, bi_=bi_):
                        def mm(e):
                            e.matmul(psS5[:, hA], Bl_s[:, (j * 8 + kk * 2) * 128:(j * 8 + kk * 2 + 1) * 128], mixedT[:, j, c0:c0 + n], start=True, stop=True)
                            return e.matmul(psS5[:, hB], Bl_s[:, (j * 8 + kk * 2 + 1) * 128:(j * 8 + kk * 2 + 2) * 128], mixedT[:, j, c0:c0 + n], start=True, stop=True)
                        P.op("pe", mm, [Bl_s] + tl, [psS5])
                        P.op("act", lambda e: e.activation(br_[:, 0:n], psS5[:, hA], AF.Copy), [psS5], [br_])
                        P.op("act", lambda e: e.activation(bi_[:, 0:n], psS5[:, hB], AF.Copy), [psS5], [bi_])

                    def st1(k=k, br_=br_, bi_=bi_, mr_=mr_, mi_=mi_):
                        P.op(e1, lambda e: e.tensor_tensor(ta[:, 0:n], br_[:, 0:n], Dre[:, k, 0:n], ALU.mult), [br_, Dre], [ta])
                        P.op(e1, lambda e: e.tensor_tensor(mr_[:, 0:n], bi_[:, 0:n], Dim[:, k, 0:n], ALU.mult), [bi_, Dim], [mr_])
                        P.op(e1, lambda e: e.tensor_tensor(mr_[:, 0:n], ta[:, 0:n], mr_[:, 0:n], ALU.add), [ta, mr_], [mr_])
                        P.op(e1, lambda e: e.tensor_tensor(tc[:, 0:n], bi_[:, 0:n], Dre[:, k, 0:n], ALU.mult), [bi_, Dre], [tc])
                        P.op(e1, lambda e: e.tensor_tensor(mi_[:, 0:n], br_[:, 0:n], Dim[:, k, 0:n], ALU.mult), [br_, Dim], [mi_])
                        P.op(e1, lambda e: e.tensor_tensor(mi_[:, 0:n], tc[:, 0:n], mi_[:, 0:n], ALU.subtract), [tc, mi_], [mi_])

                    def st2(k=k, mr_=mr_, mi_=mi_, wr_=wr_, wi_=wi_):
                        P.op("dve", lambda e: e.tensor_tensor_scan(wr_[:, 0:n], bc(rho[:, k:k + 1], [128, n]), mr_[:, 0:n], w0s[:, k, 0:1], ALU.mult, ALU.add), [rho, mr_, w0s], [wr_])
                        P.op("dve", lambda e: e.tensor_tensor_scan(wi_[:, 0:n], bc(rho[:, k:k + 1], [128, n]), mi_[:, 0:n], w0s[:, k, 1:2], ALU.mult, ALU.add), [rho, mi_, w0s], [wi_])

                    def st3(k=k, wr_=wr_, wi_=wi_, d0=d0, d1=d1, d2=d2, d3=d3):
                        P.op("pool", lambda e: e.tensor_tensor(d0[:, 0:n], wr_[:, 0:n], Dre[:, k, 0:n], ALU.mult), [wr_, Dre], [d0])
                        P.op("pool", lambda e: e.tensor_tensor(d1[:, 0:n], wi_[:, 0:n], Dim[:, k, 0:n], ALU.mult), [wi_, Dim], [d1])
                        P.op("pool", lambda e: e.tensor_tensor(Xb[:, k, 0, 0:n], d0[:, 0:n], d1[:, 0:n], ALU.subtract), [d0, d1], [Xb])
                        P.op("pool", lambda e: e.tensor_tensor(d2[:, 0:n], wr_[:, 0:n], Dim[:, k, 0:n], ALU.mult), [wr_, Dim], [d2])
                        P.op("pool", lambda e: e.tensor_tensor(d3[:, 0:n], wi_[:, 0:n], Dre[:, k, 0:n], ALU.mult), [wi_, Dre], [d3])

                    def st4(k=k, d0=d0, d1=d1, d2=d2, d3=d3):
                        P.op("dve", lambda e: e.scalar_tensor_tensor(Xb[:, k, 1, 0:n], d2[:, 0:n], -1.0, d3[:, 0:n], ALU.mult, ALU.subtract), [d2, d3, Xb], [Xb])
                        P.op("dve", lambda e: e.tensor_tensor(carry[:, k, 0:1], d0[:, n - 1:n], d1[:, n - 1:n], ALU.subtract), [d0, d1, carry], [carry])
                        P.op("dve", lambda e: e.tensor_tensor(carry[:, k, 1:2], d2[:, n - 1:n], d3[:, n - 1:n], ALU.add), [d2, d3, carry], [carry])
                    for si, fn in enumerate((st0, st1, st2, st3, st4)):
                        at(off + k + si, fn)
                T = off + 12
                hs = [hA, hB]

                def T0():
                    for j in range(2):
                        def mmy(e, j=j):
                            first = True
                            for kk in range(4):
                                k = 4 * j + kk
                                for c in range(2):
                                    e.matmul(psS5[:, hs[j]], Cl_s[:, (k * 2 + c) * 128:(k * 2 + c + 1) * 128], Xb[:, k, c, 0:n], start=first, stop=False)
                                    first = False
                            return e.matmul(psS5[:, hs[j]], Dl_s[:, j * 128:(j + 1) * 128], mixedT[:, j, c0:c0 + n], start=False, stop=True)
                        P.op("pe", mmy, [Cl_s, Xb, Dl_s] + tl, [psS5])
                    for j in range(2):
                        P.op("act", lambda e, j=j: e.activation(ysb[j][:, 0:n], psS5[:, hs[j]], AF.Copy), [psS5], [ysb[j]])
                        P.op("act", lambda e, j=j: e.activation(y2[j][:, 0:n], psS5[:, hs[j]], AF.Square), [psS5], [y2[j]])

                def T1():
                    for j in range(2):
                        P.op("pool", lambda e, j=j: e.tensor_scalar(y2[j][:, 0:n], y2[j][:, 0:n], 0.044715, 1.0, ALU.mult, ALU.add), [y2[j]], [y2[j]])

                def T2():
                    for j in range(2):
                        P.op("dve", lambda e, j=j: e.tensor_tensor(zz[j][:, 0:n], y2[j][:, 0:n], ysb[j][:, 0:n], ALU.mult), [y2[j], ysb[j]], [zz[j]])

                def T3():
                    for j in range(2):
                        P.op("act", lambda e, j=j: e.activation(sgm[j][:, 0:n], zz[j][:, 0:n], AF.Sigmoid, scale=GELU_C), [zz[j]], [sgm[j]])

                def T4():
                    for j in range(2):
                        P.op("dve", lambda e, j=j: e.tensor_tensor(geT[:, j, 0:n], sgm[j][:, 0:n], ysb[j][:, 0:n], ALU.mult), [sgm[j], ysb[j]], [geT])

                def T5():
                    for jo in range(2):
                        def mmg(e, jo=jo):
                            e.matmul(psS5[:, hs[jo]], wglu_s[:, 0, jo * 128:(jo + 1) * 128], geT[:, 0, 0:n], start=True, stop=False)
                            return e.matmul(psS5[:, hs[jo]], wglu_s[:, 1, jo * 128:(jo + 1) * 128], geT[:, 1, 0:n], start=False, stop=True)
                        P.op("pe", mmg, [wglu_s, geT], [psS5])
                    for jo in range(2):
                        P.op("act", lambda e, jo=jo: e.activation(sgm[jo][:, 0:n], psS5[:, hs[jo]], AF.Sigmoid), [psS5], [sgm[jo]])

                def T6():
                    for jo in range(2):
                        P.op("pool", lambda e, jo=jo: e.tensor_tensor(mixedT[:, jo, c0:c0 + n], sgm[jo][:, 0:n], geT[:, jo, 0:n], ALU.mult), [sgm[jo], geT], tl)
                for si, fn in enumerate((T0, T1, T2, T3, T4, T5, T6)):
                    at(T + si, fn)

            def make_sched(bl, fast=False, step=None):
                sched = {}
                off = 0
                for bs in bl:
                    if bs < NBS:
                        s5_sched(bs, off, sched, fast)
                        off += step if step is not None else (10 if fast else 12)
                return sched

            def run_slot(sched, slot):
                for fn in sched.pop(slot, []):
                    fn()

            def run_rest(sched):
                for slot in sorted(sched.keys()):
                    for fn in sched[slot]:
                        fn()
                sched.clear()

            RATIO = TB // TS
            run_rest(make_sched(range(0, RATIO), fast=True))
            wcount = 0
            tcount = 0
            NBM = NB - 1
            for b in range(NBM):
                if b % 2 == 0:
                    P.new_epoch()
                if b < NBM - 2:
                    sched = make_sched(range(RATIO * (b + 1), RATIO * (b + 2)))
                elif b == NBM - 2:
                    sched = make_sched(range(RATIO * (b + 1), NBS), step=10)
                else:
                    sched = {}
                slot = [0]
                c0 = b * TB
                n = TB
                tiles = list(range(4 * b, 4 * b + 4)) + ([NT - 1] if b == NBM - 1 else [])
                nl = len(tiles)
                h2c = h2 + ([h2x] if nl == 5 else [])
                if nl == 5:
                    P.op("dve", lambda e: e.memset(w0[:, 0:1, 0:1], 0.0), [], [Dre, Dim, w0, h2x, tTxb] + actTxb + silxb)
                for li, t in enumerate(tiles):
                    hb = h2c[li]
                    if t == 0:
                        P.op("pool", lambda e, hb=hb: e.memset(hb[:], 0.0), [], [hb])
                        P.dma("sp", hb[48:64, :], meta[:, :], writes=[hb])
                        P.dma("sp", hb[64:128, :], x[0:64, :], writes=[hb])
                    elif t == NT - 1:
                        P.op("pool", lambda e, hb=hb: e.memset(hb[:], 0.0), [], [hb])
                        P.dma("sp", hb[0:64, :], x[SEQ - 64:SEQ, :], writes=[hb])
                    else:
                        P.dma("sp", hb[:], x[128 * t - 64:128 * t + 64, :], writes=[hb])
                def head_norm(lo, hi):
                    P.op("dve", lambda e: e.tensor_scalar(st2[:, 1, lo:hi], st2[:, 0, lo:hi], 1.0 / D, EPS, ALU.mult, ALU.add), [st2], [st2])
                    P.op("act", lambda e: e.activation(st2[:, 2, lo:hi], st2[:, 1, lo:hi], AF.Sqrt), [st2], [st2])
                    P.op("dve", lambda e: e.reciprocal(st2[:, 3, lo:hi], st2[:, 2, lo:hi]), [st2], [st2])
                for li, t in enumerate(tiles):
                    hb = h2c[li]
                    if li < 2:
                        pa_, pb_ = (psGU[1], psGU[2]) if li == 0 else (psGU[0], psS5)

                        def mmo0(e, t=t, pa_=pa_, pb_=pb_):
                            ins = None
                            for half, pp in enumerate((pa_, pb_)):
                                for c in range(8):
                                    ins = e.matmul(pp[:, 0:512], mixedT[:, c, 128 * t:128 * (t + 1)], Wout[:, c, half * 512:(half + 1) * 512], start=(c == 0), stop=(c == 7))
                            return ins
                        P.op("pe", mmo0, [mxb[t], Wout], [pa_, pb_])
                        P.op("dve", lambda e, hb=hb, pa_=pa_: e.tensor_tensor(hb[:, 0:512], hb[:, 0:512], pa_[:, 0:512], ALU.add), [hb, pa_], [hb])
                        P.op("dve", lambda e, hb=hb, pb_=pb_: e.tensor_tensor(hb[:, 512:1024], hb[:, 512:1024], pb_[:, 0:512], ALU.add), [hb, pb_], [hb])
                    else:
                        psD = psDs[dcount[0] % 2]
                        dcount[0] += 1

                        def mmo(e, t=t, psD=psD):
                            ins = None
                            for half in range(2):
                                for c in range(8):
                                    ins = e.matmul(psD[:, half * 512:(half + 1) * 512], mixedT[:, c, 128 * t:128 * (t + 1)], Wout[:, c, half * 512:(half + 1) * 512], start=(c == 0), stop=(c == 7))
                            return ins
                        P.op("pe", mmo, [mxb[t], Wout], [psD])
                        P.op("dve", lambda e, hb=hb, psD=psD: e.tensor_tensor(hb[:], hb[:], psD[:], ALU.add), [hb, psD], [hb])
                    P.op("act", lambda e, hb=hb, li=li: e.activation(actT[0][:].rearrange("p a b -> p (a b)"), hb[:], AF.Square, accum_out=st2[:, 0, li:li + 1]), [hb], [actT[0], st2])
                    if li == 1 and nl > 2:
                        head_norm(0, 2)
                head_norm(2 if nl > 2 else 0, nl)
                tb_aps = [tbf[0][:], actT[1][:].rearrange("p a b -> p (a b)")]
                tb_bufs = [tbf[0], actT[1]]
                pT_aps = [psT2, psGU[0][:].bitcast(BF16).rearrange("p (c t) -> p c t", c=8)]
                pT_bufs = [psS5, psGU[0]]
                for li, t in enumerate(tiles):
                    hb = h2c[li]
                    kq = li % 2
                    tb_ap, tb_b, pT_ap, pT_b = tb_aps[kq], tb_bufs[kq], pT_aps[kq], pT_bufs[kq]
                    P.op("act", lambda e, hb=hb, li=li, tb_ap=tb_ap: e.activation(tb_ap, hb[:], AF.Copy, scale=st2[:, 3, li:li + 1]), [hb, st2], [tb_b])

                    def tr2(e, tb_ap=tb_ap, pT_ap=pT_ap):
                        ins = None
                        for c in range(8):
                            ins = e.transpose(pT_ap[:, c, :], tb_ap[:, c * 128:(c + 1) * 128], ident[:])
                        return ins
                    P.op("pe", tr2, [tb_b, ident], [pT_b])

                    tT3 = tT[:, :, li * 128:(li + 1) * 128] if li < 4 else tTx_ap
                    P.op("dve", lambda e, tT3=tT3, pT_ap=pT_ap: e.tensor_tensor(tT3, pT_ap, bc(gffn_s[:].unsqueeze(2), [128, 8, 128]), ALU.mult), [pT_b, gffn_s], [tTbuf(li)])

                def head_router(nl):
                    def mmr(e):
                        ins = None
                        for li in range(nl):
                            for c in range(8):
                                ins = e.matmul(psR[:, 20 * li:20 * li + 20], tTa(c, li), wr_s[:, c, :], start=(c == 0), stop=(c == 7))
                        return ins
                    P.op("pe", mmr, [tT, wr_s] + ([tTxb] if nl == 5 else []), [psR])
                    R_ = lambda a, b_: rt[:, 0:nl, a:b_]
                    cmv = comb[:, 0:nl, :]
                    P.op("dve", lambda e: e.tensor_tensor(R_(0, 20), psR[:, 0:20 * nl].rearrange("p (t f) -> p t f", f=20), bc(br_s[:].unsqueeze(1), [128, nl, 20]), ALU.add), [psR, br_s], [rt])
                    P.op("dve", lambda e: e.tensor_reduce(rt[:, 0:nl, 20], R_(0, 4), AX.X, ALU.max), [rt], [rt])
                    P.op("dve", lambda e: e.tensor_tensor(R_(21, 25), R_(0, 4), bc(R_(20, 21), [128, nl, 4]), ALU.is_ge), [rt], [rt])
                    P.op("dve", lambda e: e.tensor_tensor(R_(25, 29), R_(0, 4), bc(R_(20, 21), [128, nl, 4]), ALU.subtract), [rt], [rt])
                    P.op("act", lambda e: e.activation(R_(25, 29), R_(25, 29), AF.Exp), [rt], [rt])
                    P.op("dve", lambda e: e.tensor_reduce(rt[:, 0:nl, 29], R_(25, 29), AX.X, ALU.add), [rt], [rt])
                    P.op("dve", lambda e: e.reciprocal(R_(30, 31), R_(29, 30)), [rt], [rt])
                    P.op("dve", lambda e: e.tensor_tensor(cmv.rearrange("p t (e g) -> p t e g", g=4), R_(4, 20).rearrange("p t (g e) -> p t e g", e=4), bc(R_(21, 25).unsqueeze(2), [128, nl, 4, 4]), ALU.mult), [rt], [comb])
                    P.op("dve", lambda e: e.tensor_reduce(R_(31, 35), cmv.rearrange("p t (e g) -> p t e g", g=4), AX.X, ALU.add), [comb, rt], [rt])
                    P.op("dve", lambda e: e.tensor_reduce(rt[:, 0:nl, 35], R_(31, 35), AX.X, ALU.max), [rt], [rt])
                    P.op("dve", lambda e: e.tensor_tensor(R_(36, 40), R_(31, 35), bc(R_(35, 36), [128, nl, 4]), ALU.is_ge), [rt], [rt])
                    P.op("dve", lambda e: e.scalar_tensor_tensor(R_(40, 44), R_(36, 40), -1e30, R_(31, 35), ALU.mult, ALU.add), [rt], [rt])
                    P.op("dve", lambda e: e.tensor_reduce(rt[:, 0:nl, 44], R_(40, 44), AX.X, ALU.max), [rt], [rt])
                    P.op("dve", lambda e: e.tensor_tensor(R_(45, 49), R_(40, 44), bc(R_(44, 45), [128, nl, 4]), ALU.is_ge), [rt], [rt])
                    P.op("dve", lambda e: e.tensor_tensor(R_(49, 50), R_(44, 45), R_(35, 36), ALU.subtract), [rt], [rt])
                    P.op("act", lambda e: e.activation(R_(50, 51), R_(49, 50), AF.Exp), [rt], [rt])
                    P.op("dve", lambda e: e.tensor_scalar(R_(51, 52), R_(50, 51), 1.0, None, ALU.add), [rt], [rt])
                    P.op("dve", lambda e: e.reciprocal(R_(52, 53), R_(51, 52)), [rt], [rt])
                    P.op("dve", lambda e: e.tensor_tensor(R_(51, 52), R_(50, 51), R_(52, 53), ALU.mult), [rt], [rt])
                    P.op("dve", lambda e: e.tensor_tensor(R_(53, 57), R_(36, 40), bc(R_(52, 53), [128, nl, 4]), ALU.mult), [rt], [rt])
                    P.op("dve", lambda e: e.tensor_tensor(R_(57, 61), R_(45, 49), bc(R_(51, 52), [128, nl, 4]), ALU.mult), [rt], [rt])
                    P.op("dve", lambda e: e.tensor_tensor(R_(53, 57), R_(53, 57), R_(57, 61), ALU.add), [rt], [rt])
                    P.op("dve", lambda e: e.tensor_tensor(R_(57, 61), R_(21, 25), bc(R_(30, 31), [128, nl, 4]), ALU.mult), [rt], [rt])
                    P.op("dve", lambda e: e.tensor_tensor(cmv.rearrange("p t (g e) -> p t g e", e=4), bc(R_(57, 61).unsqueeze(3), [128, nl, 4, 4]), bc(R_(53, 57).unsqueeze(2), [128, nl, 4, 4]), ALU.mult), [rt], [comb])
                head_router(nl)

                def down(ex, lis, wi, at, h2c=h2c):
                    for li in lis:
                        psD = psDs[dcount[0] % 2]
                        dcount[0] += 1
                        atx = actTx_ap[ex % 2]

                        def mmd(e, at=at, atx=atx, wi=wi, li=li, psD=psD):
                            ins = None
                            for half in range(2):
                                for f in range(2):
                                    src = at[:, f, li * 128:(li + 1) * 128] if li < 4 else atx[:, f, :]
                                    ins = e.matmul(psD[:, half * 512:(half + 1) * 512], src, wd[wi][:, f, half * 512:(half + 1) * 512], start=(f == 0), stop=(f == 1))
                            return ins
                        P.op("pe", mmd, [at if li < 4 else actTxb[ex % 2], wd[wi]], [psD])
                        P.op("dve", lambda e, li=li, ex=ex, psD=psD, hb=h2c[li]: e.scalar_tensor_tensor(hb[:], psD[:], comb[:, li, ex:ex + 1], hb[:], ALU.mult, ALU.add), [psD, comb, h2c[li]], [h2c[li]])

                prev = None
                for ex in range(NE):
                    wi = wcount % 2
                    wcount += 1
                    P.dma("sp", wg[wi][:].rearrange("p c f -> p (c f)"), sc_gate[ex], writes=[wg[wi]])
                    P.dma("sp", wu[wi][:].rearrange("p c f -> p (c f)"), sc_up[ex], writes=[wu[wi]])
                    P.dma("sp", wd[wi][:].rearrange("p c f -> p (c f)"), sc_down[ex], writes=[wd[wi]])
                    at = actT[ex % 2]
                    for f in range(2):
                        pg_, pu_ = psGU[gucount[0] % 3], psGU[(gucount[0] + 1) % 3]
                        gucount[0] += 2

                        def mmg2(e, pg_=pg_, wi=wi, f=f, n=n):
                            ins = None
                            for c in range(8):
                                ins = e.matmul(pg_[:, 0:n], wg[wi][:, c, f * 128:(f + 1) * 128], tT[:, c, 0:n], start=(c == 0), stop=(c == 7))
                            return ins
                        P.op("pe", mmg2, [wg[wi], tT], [pg_])

                        def mmu2(e, pu_=pu_, wi=wi, f=f, n=n):
                            ins = None
                            for c in range(8):
                                ins = e.matmul(pu_[:, 0:n], wu[wi][:, c, f * 128:(f + 1) * 128], tT[:, c, 0:n], start=(c == 0), stop=(c == 7))
                            return ins
                        P.op("pe", mmu2, [wu[wi], tT], [pu_])
                        sl_ = sil[f]
                        P.op("act", lambda e, pg_=pg_, sl_=sl_, n=n: e.activation(sl_[:, 0:n], pg_[:, 0:n], AF.Silu), [pg_], [sl_])
                        P.op("dve", lambda e, pu_=pu_, sl_=sl_, at=at, f=f, n=n: e.tensor_tensor(at[:, f, 0:n], pu_[:, 0:n], sl_[:, 0:n], ALU.mult), [pu_, sl_], [at])
                        if nl == 5:
                            def mmx(e, wi=wi, f=f):
                                ins = None
                                for c in range(8):
                                    e.matmul(psS5[:, 0:128], wg[wi][:, c, f * 128:(f + 1) * 128], tTx_ap[:, c, :], start=(c == 0), stop=(c == 7))
                                for c in range(8):
                                    ins = e.matmul(psS5[:, 128:256], wu[wi][:, c, f * 128:(f + 1) * 128], tTx_ap[:, c, :], start=(c == 0), stop=(c == 7))
                                return ins
                            P.op("pe", mmx, [wg[wi], wu[wi], tTxb], [psS5])
                            P.op("act", lambda e, f=f: e.activation(silx_ap[f], psS5[:, 0:128], AF.Silu), [psS5], [silxb[f]])
                            P.op("dve", lambda e, f=f, ex=ex: e.tensor_tensor(actTx_ap[ex % 2][:, f, :], psS5[:, 128:256], silx_ap[f], ALU.mult), [psS5, silxb[f]], [actTxb[ex % 2]])
                        run_slot(sched, slot[0])
                        slot[0] += 1
                        if prev is not None:
                            lis = list(range(nl))[(f * ((nl + 1) // 2)):((f + 1) * ((nl + 1) // 2))] if nl > 1 else ([0] if f == 0 else [])
                            down(prev[0], lis, prev[1], prev[2])
                    prev = (ex, wi, at)
                down(prev[0], list(range(nl)), prev[1], prev[2])
                run_rest(sched)
                for li, t in enumerate(tiles):
                    hb = h2c[li]
                    P.op("act", lambda e, hb=hb, li=li: e.activation(actT[0][:].rearrange("p a b -> p (a b)"), hb[:], AF.Square, accum_out=st3[:, 0, li:li + 1]), [hb], [actT[0], st3])
                def fin_norm(nl):
                    P.op("dve", lambda e: e.tensor_scalar(st3[:, 1, 0:nl], st3[:, 0, 0:nl], 1.0 / D, EPS, ALU.mult, ALU.add), [st3], [st3])
                    P.op("act", lambda e: e.activation(st3[:, 2, 0:nl], st3[:, 1, 0:nl], AF.Sqrt), [st3], [st3])
                    P.op("dve", lambda e: e.reciprocal(st3[:, 3, 0:nl], st3[:, 2, 0:nl]), [st3], [st3])
                fin_norm(nl)
                for li, t in enumerate(tiles):
                    hb = h2c[li]
                    P.op("dve", lambda e, hb=hb, li=li: e.scalar_tensor_tensor(hb[:], hb[:], st3[:, 3, li:li + 1], gfin_s[:], ALU.mult, ALU.mult), [hb, st3, gfin_s], [hb])
                    if t == 0:
                        P.dma("act", out[0:64, :], hb[64:128, :], reads=[hb], sigbuf=hb)
                    elif t == NT - 1:
                        P.dma("act", out[SEQ - 64:SEQ, :], hb[0:64, :], reads=[hb], sigbuf=hb)
                    else:
                        P.dma("act", out[128 * t - 64:128 * t + 64, :], hb[:], reads=[hb], sigbuf=hb)
            P.emit(final_waits=h2 + [h2x])
    return nc


def _host_consts():
    c = {}
    d = np.arange(128)
    inv_freq = (10000.0 ** (-(np.arange(0, 128, 2, dtype=np.float32)) / np.float32(128))).astype(np.float32)
    pos = (np.arange(LP, dtype=np.float32) - np.float32(48.0)).astype(np.float32)
    ang = (pos[None, :] * inv_freq[d % 64][:, None]).astype(np.float32)
    c["cosT"] = np.cos(ang).astype(np.float32)
    sn = np.sin(ang).astype(np.float32)
    sn[64:] = -sn[64:]
    c["sinT"] = sn
    gamma = 1.0 - 2.0 ** (-5.0 - np.arange(NH, dtype=np.float64))
    j = np.arange(128)[:, None]
    i = np.arange(128)[None, :]
    mask = np.zeros((128, NH, 128), np.float64)
    same = (j // 64) == (i // 64)
    causal_ab = (j < 64) & (i >= 64)
    for h in range(NH):
        m = np.where(same, gamma[h] ** np.abs(i - j), 0.0)
        m = np.where(causal_ab, gamma[h] ** (i - j), m)
        mask[:, h, :] = m * (128.0 ** -0.5)
    c["maskT"] = mask.reshape(128, NH * 128).astype(np.float32)
    r = np.arange(128)[:, None]
    c["qdec"] = (gamma[None, :] ** (r + 1)).astype(np.float32)
    c["kdec"] = ((gamma[None, :] ** (127 - r)) * (128.0 ** -0.5)).astype(np.float32)
    c["identf"] = np.eye(128, dtype=np.float32)
    return c


_NC_CACHE = {}


def kernel(x, meta_tokens, norm_mix_g, w_in, ssm_lambda_re, ssm_lambda_im, ssm_log_dt,
           ssm_b_re, ssm_b_im, ssm_c_re, ssm_c_im, ssm_d, w_glu, w_out, norm_ffn_g,
           w_router_group, b_router_group, w_router_expert, b_router_expert,
           w_gate, w_up, w_down, norm_final_g):
    f = lambda a: np.ascontiguousarray(np.asarray(a, dtype=np.float32))
    x = f(x)
    shared = dict(_host_consts())
    shared["meta"] = f(meta_tokens)
    shared["gmix"] = f(np.asarray(norm_mix_g)[0].reshape(8, 128).T)
    shared["gffn"] = f(np.asarray(norm_ffn_g)[0].reshape(8, 128).T)
    shared["gfin"] = f(np.broadcast_to(np.asarray(norm_final_g)[None, :], (128, D)))
    shared["w_in"] = f(np.asarray(w_in)[0])
    shared["w_out"] = f(np.asarray(w_out)[0])
    shared["w_glu"] = f(np.asarray(w_glu)[0])
    shared["w_gate"] = f(np.asarray(w_gate)[0])
    shared["w_up"] = f(np.asarray(w_up)[0])
    shared["w_down"] = f(np.asarray(w_down)[0])
    wre = np.asarray(w_router_expert)[0]
    shared["wr"] = f(np.concatenate([np.asarray(w_router_group)[0], wre.transpose(1, 0, 2).reshape(D, 16)], axis=1))
    brv = np.concatenate([np.asarray(b_router_group)[0], np.asarray(b_router_expert)[0].reshape(16)])
    shared["br"] = f(np.broadcast_to(brv[None, :], (128, 20)))
    def srow(a):
        return f(np.asarray(a).reshape(8, 2, 64).transpose(1, 2, 0).reshape(128, 8))
    shared["lre"] = srow(np.asarray(ssm_lambda_re)[0])
    shared["lim"] = srow(np.asarray(ssm_lambda_im)[0])
    shared["ldt"] = f(np.broadcast_to(np.asarray(ssm_log_dt)[0].reshape(8, 2)[None, :, :], (64, 8, 2)).transpose(2, 0, 1).reshape(128, 8))
    bre = np.asarray(ssm_b_re)[0]
    bim = np.asarray(ssm_b_im)[0]
    cre = np.asarray(ssm_c_re)[0]
    cim = np.asarray(ssm_c_im)[0]
    dsk = np.asarray(ssm_d)[0]
    Blh = np.zeros((128, 2, 4, 2, 128), np.float32)
    Clh = np.zeros((128, 8, 2, 128), np.float32)
    Dlh = np.zeros((128, 2, 128), np.float32)
    for g in range(16):
        k, q = g // 2, g % 2
        j, kk = k // 4, k % 4
        gl = g - 8 * j
        rows = slice(q * 64, q * 64 + 64)
        cls = slice(gl * 16, gl * 16 + 16)
        Blh[cls, j, kk, 0, rows] = bre[g].T
        Blh[cls, j, kk, 1, rows] = bim[g].T
        Clh[rows, k, 0, cls] = cre[g].T
        Clh[rows, k, 1, cls] = cim[g].T
        for h in range(16):
            Dlh[gl * 16 + h, j, gl * 16 + h] = dsk[g, h]
    shared["Bl"] = Blh.reshape(128, 2048)
    shared["Cl"] = Clh.reshape(128, 2048)
    shared["Dl"] = Dlh.reshape(128, 256)
    if "nc" not in _NC_CACHE:
        _NC_CACHE["nc"] = build_nc()
    nc = _NC_CACHE["nc"]
    in_maps = []
    for b in range(8):
        m = dict(shared)
        m["x"] = np.ascontiguousarray(x[b])
        in_maps.append(m)
    res = run_bass_kernel_spmd(nc, in_maps, core_ids=list(range(8)))
    return np.stack([r["out"] for r in res.results], axis=0).astype(np.float32)
```

```python
import numpy as np
from contextlib import ExitStack
import concourse.bass as bass
import concourse.mybir as mybir
from concourse.bass_utils import run_bass_kernel_spmd

F32 = mybir.dt.float32
BF16 = mybir.dt.bfloat16
AF = mybir.ActivationFunctionType
ALU = mybir.AluOpType
AX = mybir.AxisListType

D = 1024
SEQ = 4096
NT = 33
LP = NT * 128
EPS = 1e-6
NH = 6
NE = 16
INW = 3328
TB = 512
NB = (LP + TB - 1) // TB
TS = 256
NBS = (LP + TS - 1) // TS
GELU_C = 1.5957691216057308
SAME_ENG_WAR = False


class Buf:
    def __init__(self, name, t):
        self.name = name
        self.t = t
        self.lw = None
        self.rd = []
        self.sem = None
        self.nd = 0

    def __getitem__(self, k):
        return self.t[k]


class Op:
    __slots__ = ("eng", "fn", "deps", "sig", "cnt", "epoch", "is_dma", "dbuf", "dcnt")


class Prog:
    ENG = ["pe", "act", "dve", "pool", "sp"]

    NPROG = [0]
    TOT = [0]
    PH = {}

    def __init__(self, nc, es, semes):
        Prog.NPROG[0] += 1
        self.tag = "g%d" % Prog.NPROG[0]
        self.nc = nc
        self.es = es
        self.semes = semes
        self.ops = {e: [] for e in self.ENG}
        self.epoch = 0
        self.sems = {}
        self.out_dmas = []

    def buf(self, name, shape, dt, ps=False):
        if not ps:
            nb = int(np.prod(shape[1:])) * (2 if dt == BF16 else 4)
            Prog.TOT[0] += nb
            Prog.PH[self.tag] = Prog.PH.get(self.tag, 0) + nb
        if ps:
            t = self.es.enter_context(self.nc.psum_tensor(self.tag + name, shape, dt))
        else:
            t = self.es.enter_context(self.nc.sbuf_tensor(self.tag + name, shape, dt))
        return Buf(name, t)

    def new_epoch(self):
        self.epoch += 1

    def _mk(self, eng, fn, reads, writes):
        op = Op()
        op.eng = eng
        op.fn = fn
        op.deps = set()
        op.sig = False
        op.cnt = 0
        op.epoch = self.epoch
        op.is_dma = False
        op.dbuf = None
        op.dcnt = 0
        for b in reads:
            if b.lw is not None:
                op.deps.add(b.lw)
        for b in writes:
            if b.lw is not None:
                op.deps.add(b.lw)
            for r in b.rd:
                if SAME_ENG_WAR or r.is_dma or r.eng != eng:
                    op.deps.add(r)
        for b in reads:
            b.rd.append(op)
        for b in writes:
            b.lw = op
            b.rd = []
        self.ops[eng].append(op)
        return op

    def op(self, eng, fn, reads=(), writes=()):
        op = self._mk(eng, fn, reads, writes)
        if eng == "pe":
            op.deps = {d for d in op.deps if d.is_dma or d.eng != "pe"}
        return op

    def dma(self, eng, out, in_, reads=(), writes=(), sigbuf=None):
        def fn(e, out=out, in_=in_):
            return e.dma_start(out=out, in_=in_)
        op = self._mk(eng, fn, reads, writes)
        op.is_dma = True
        b = sigbuf if sigbuf is not None else (writes[0] if writes else reads[0])
        if b.sem is None:
            b.sem = self.semes.enter_context(self.nc.semaphore(self.tag + "d_" + b.name))
        b.nd += 1
        op.dbuf = b
        op.dcnt = b.nd
        return op

    def emit(self, final_waits=()):
        nc = self.nc
        allops = [o for e in self.ENG for o in self.ops[e]]
        for o in allops:
            for d in o.deps:
                if not d.is_dma:
                    d.sig = True
        for e in self.ENG:
            cnts = {}
            for o in self.ops[e]:
                if o.sig and not o.is_dma:
                    cnts[o.epoch] = cnts.get(o.epoch, 0) + 1
                    o.cnt = cnts[o.epoch]
                    key = (e, o.epoch)
                    if key not in self.sems:
                        self.sems[key] = self.semes.enter_context(nc.semaphore(self.tag + "s_%s_%d" % key))
        prog = self

        def run(eng_name, e):
            waited = {}
            for o in prog.ops[eng_name]:
                need = {}
                for d in o.deps:
                    if d.is_dma:
                        sem, val = d.dbuf.sem, 16 * d.dcnt
                    else:
                        sem, val = prog.sems[(d.eng, d.epoch)], d.cnt
                    k = id(sem)
                    if k not in need or need[k][1] < val:
                        need[k] = (sem, val)
                for k, (sem, val) in need.items():
                    if waited.get(k, 0) >= val:
                        continue
                    e.wait_ge(sem, val)
                    waited[k] = val
                ins = o.fn(e)
                if o.is_dma:
                    ins.then_inc(o.dbuf.sem, 16)
                elif o.sig:
                    ins.then_inc(prog.sems[(o.eng, o.epoch)], 1)
            if eng_name == "sp":
                for b in final_waits:
                    if b.sem is not None:
                        e.wait_ge(b.sem, 16 * b.nd)

        with nc.Block() as block:
            @block.tensor
            def _(e):
                run("pe", e)

            @block.scalar
            def _(e):
                run("act", e)

            @block.vector
            def _(e):
                run("dve", e)

            @block.gpsimd
            def _(e):
                run("pool", e)

            @block.sync
            def _(e):
                run("sp", e)


def bc(ap, shape):
    return ap.to_broadcast(shape)


def build_nc():
    nc = bass.Bass("TRN2", target_bir_lowering=False)

    def din(name, shape, dt=F32):
        return nc.dram_tensor(name, list(shape), dt, kind="ExternalInput").ap()

    x = din("x", [SEQ, D])
    meta = din("meta", [16, D])
    gmix = din("gmix", [128, 8])
    gffn = din("gffn", [128, 8])
    gfin = din("gfin", [128, D])
    w_in = din("w_in", [D, INW])
    w_out = din("w_out", [D, D])
    w_glu = din("w_glu", [256, 256])
    w_gate = din("w_gate", [NE, D, 256])
    w_up = din("w_up", [NE, D, 256])
    w_down = din("w_down", [NE, 256, D])
    wr = din("wr", [D, 20])
    br = din("br", [128, 20])
    lre = din("lre", [128, 8])
    lim = din("lim", [128, 8])
    ldt = din("ldt", [128, 8])
    Bl = din("Bl", [128, 2048])
    Cl = din("Cl", [128, 2048])
    Dl = din("Dl", [128, 256])
    cosT = din("cosT", [128, LP])
    sinT = din("sinT", [128, LP])
    maskT = din("maskT", [128, NH * 128])
    qdec = din("qdec", [128, NH])
    kdec = din("kdec", [128, NH])
    identf = din("identf", [128, 128])
    out = nc.dram_tensor("out", [SEQ, D], F32, kind="ExternalOutput").ap()
    sc_gate = nc.dram_tensor("sc_gate", [NE, 128, 2048], BF16).ap()
    sc_up = nc.dram_tensor("sc_up", [NE, 128, 2048], BF16).ap()
    sc_down = nc.dram_tensor("sc_down", [NE, 128, 2048], BF16).ap()

    gamma = [1.0 - 2.0 ** (-5.0 - h) for h in range(NH)]
    g128 = [float(g ** 128) for g in gamma]

    with ExitStack() as ges, ExitStack() as semes:
        G = Prog(nc, ges, semes)
        mixedT = G.buf("mixedT", [128, 8, LP], BF16)
        ident = G.buf("ident", [128, 128], BF16)
        gmix_s = G.buf("gmix_s", [128, 8], F32)
        gffn_s = G.buf("gffn_s", [128, 8], F32)
        rho = G.buf("rho", [128, 8], F32)
        qre = G.buf("qre", [128, 8], F32)
        qim = G.buf("qim", [128, 8], F32)
        ucs = G.buf("ucs", [128, 2, 8], F32)
        Bl_s = G.buf("Bl_s", [128, 2048], BF16)
        Cl_s = G.buf("Cl_s", [128, 2048], BF16)
        Dl_s = G.buf("Dl_s", [128, 256], BF16)
        wglu_s = G.buf("wglu_s", [128, 2, 256], BF16)
        mxb = [Buf("mx%d" % t, None) for t in range(NT)]

        with ExitStack() as es:
            P = Prog(nc, es, semes)
            idf = P.buf("idf", [128, 128], F32)
            P.dma("sp", idf[:], identf[:, :], writes=[idf])
            P.op("act", lambda e: e.activation(ident[:], idf[:], AF.Copy), [idf], [ident])
            P.dma("sp", gmix_s[:], gmix[:, :], writes=[gmix_s])
            P.dma("sp", gffn_s[:], gffn[:, :], writes=[gffn_s])
            stg = [P.buf("stg%d" % i, [128, 2048], F32) for i in range(2)]
            P.dma("sp", stg[0][:], Bl[:, :], writes=[stg[0]])
            P.op("act", lambda e: e.activation(Bl_s[:], stg[0][:], AF.Copy), [stg[0]], [Bl_s])
            P.dma("sp", stg[1][:], Cl[:, :], writes=[stg[1]])
            P.dma("sp", stg[0][:, 0:256], Dl[:, :], writes=[stg[0]])
            P.op("act", lambda e: e.activation(Dl_s[:], stg[0][:, 0:256], AF.Copy), [stg[0]], [Dl_s])
            P.dma("sp", stg[0][:, 512:1024].rearrange("p (c f) -> p c f", c=2),
                  w_glu.rearrange("(c p) f -> p c f", p=128), writes=[stg[0]])
            P.op("dve", lambda e: e.tensor_copy(wglu_s[:], stg[0][:, 512:1024].rearrange("p (c f) -> p c f", c=2)),
                 [stg[0]], [wglu_s])
            lre_s = P.buf("lre_s", [128, 8], F32)
            lim_s = P.buf("lim_s", [128, 8], F32)
            dt_s = P.buf("dt_s", [128, 8], F32)
            P.dma("sp", lre_s[:], lre[:, :], writes=[lre_s])
            P.dma("sp", lim_s[:], lim[:, :], writes=[lim_s])
            P.dma("sp", dt_s[:], ldt[:, :], writes=[dt_s])
            P.op("act", lambda e: e.activation(dt_s[:], dt_s[:], AF.Exp), [dt_s], [dt_s])
            tmpa = P.buf("tmpa", [128, 8], F32)
            tmpb = P.buf("tmpb", [128, 8], F32)
            th = P.buf("th", [128, 8], F32)
            P.op("dve", lambda e: e.tensor_tensor(tmpa[:], lre_s[:], dt_s[:], ALU.mult), [lre_s, dt_s], [tmpa])
            P.op("act", lambda e: e.activation(rho[:], tmpa[:], AF.Exp), [tmpa], [rho])
            P.op("dve", lambda e: e.tensor_tensor(th[:], lim_s[:], dt_s[:], ALU.mult), [lim_s, dt_s], [th])
            cc = P.buf("cc", [128, 8], F32)
            ss = P.buf("ss", [128, 8], F32)
            hp = P.buf("hp", [128, 1], F32)
            P.op("dve", lambda e: e.memset(hp[:], float(np.pi / 2)), [], [hp])
            P.op("act", lambda e: e.activation(ss[:], th[:], AF.Sin, scale=1.0 / 32), [th], [ss])
            P.op("act", lambda e: e.activation(cc[:], th[:], AF.Sin, scale=1.0 / 32, bias=hp[:, 0:1]), [th, hp], [cc])
            c2 = P.buf("c2", [128, 8], F32)
            s2 = P.buf("s2", [128, 8], F32)
            for it in range(5):
                P.op("dve", lambda e: e.tensor_tensor(c2[:], cc[:], cc[:], ALU.mult), [cc], [c2])
                P.op("dve", lambda e: e.tensor_tensor(s2[:], ss[:], ss[:], ALU.mult), [ss], [s2])
                P.op("dve", lambda e: e.tensor_tensor(tmpb[:], cc[:], ss[:], ALU.mult), [cc, ss], [tmpb])
                P.op("dve", lambda e: e.tensor_tensor(cc[:], c2[:], s2[:], ALU.subtract), [c2, s2], [cc])
                P.op("dve", lambda e: e.tensor_scalar(ss[:], tmpb[:], 2.0, None, ALU.mult), [tmpb], [ss])
            P.op("dve", lambda e: e.tensor_copy(ucs[:, 0, :], cc[:]), [cc], [ucs])
            P.op("dve", lambda e: e.tensor_copy(ucs[:, 1, :], ss[:]), [ss, ucs], [ucs])
            nre = P.buf("nre", [128, 8], F32)
            nim = P.buf("nim", [128, 8], F32)
            den = P.buf("den", [128, 8], F32)
            P.op("dve", lambda e: e.tensor_tensor(nre[:], rho[:], cc[:], ALU.mult), [rho, cc], [nre])
            P.op("dve", lambda e: e.tensor_scalar(nre[:], nre[:], -1.0, None, ALU.add), [nre], [nre])
            P.op("dve", lambda e: e.tensor_tensor(nim[:], rho[:], ss[:], ALU.mult), [rho, ss], [nim])
            P.op("dve", lambda e: e.tensor_tensor(den[:], lre_s[:], lre_s[:], ALU.mult), [lre_s], [den])
            P.op("dve", lambda e: e.tensor_tensor(tmpa[:], lim_s[:], lim_s[:], ALU.mult), [lim_s], [tmpa])
            P.op("dve", lambda e: e.tensor_tensor(den[:], den[:], tmpa[:], ALU.add), [den, tmpa], [den])
            P.op("dve", lambda e: e.reciprocal(den[:], den[:]), [den], [den])
            P.op("dve", lambda e: e.tensor_tensor(tmpa[:], nre[:], lre_s[:], ALU.mult), [nre, lre_s], [tmpa])
            P.op("dve", lambda e: e.tensor_tensor(tmpb[:], nim[:], lim_s[:], ALU.mult), [nim, lim_s], [tmpb])
            P.op("dve", lambda e: e.tensor_tensor(tmpa[:], tmpa[:], tmpb[:], ALU.add), [tmpa, tmpb], [tmpa])
            P.op("dve", lambda e: e.tensor_tensor(qre[:], tmpa[:], den[:], ALU.mult), [tmpa, den], [qre])
            P.op("dve", lambda e: e.tensor_tensor(tmpa[:], nim[:], lre_s[:], ALU.mult), [nim, lre_s], [tmpa])
            P.op("dve", lambda e: e.tensor_tensor(tmpb[:], nre[:], lim_s[:], ALU.mult), [nre, lim_s], [tmpb])
            P.op("dve", lambda e: e.tensor_tensor(tmpa[:], tmpa[:], tmpb[:], ALU.subtract), [tmpa, tmpb], [tmpa])
            P.op("dve", lambda e: e.tensor_tensor(qim[:], tmpa[:], den[:], ALU.mult), [tmpa, den], [qim])
            nqim = P.buf("nqim", [128, 8], F32)
            tq = P.buf("tq", [128, 128], F32)
            P.op("dve", lambda e: e.tensor_scalar(nqim[:], qim[:], -1.0, None, ALU.mult), [qim], [nqim])
            for k in range(8):
                cre_ = slice((2 * k) * 128, (2 * k + 1) * 128)
                cim_ = slice((2 * k + 1) * 128, (2 * k + 2) * 128)
                P.op("dve", lambda e, k=k, cre_=cre_: e.tensor_scalar(tq[:], stg[1][:, cre_], qre[:, k:k + 1], None, ALU.mult), [stg[1], qre], [tq])
                P.op("dve", lambda e, k=k, cre_=cre_, cim_=cim_: e.scalar_tensor_tensor(Cl_s[:, cre_], stg[1][:, cim_], nqim[:, k:k + 1], tq[:], ALU.mult, ALU.add), [stg[1], nqim, tq], [Cl_s])
                P.op("dve", lambda e, k=k, cre_=cre_: e.tensor_scalar(tq[:], stg[1][:, cre_], qim[:, k:k + 1], None, ALU.mult), [stg[1], qim], [tq])
                P.op("dve", lambda e, k=k, cim_=cim_: e.scalar_tensor_tensor(Cl_s[:, cim_], stg[1][:, cim_], qre[:, k:k + 1], tq[:], ALU.mult, ALU.add), [stg[1], qre, tq, Cl_s], [Cl_s])
            P.emit(final_waits=[gmix_s, gffn_s])
        nc.all_engine_barrier()
        for b in [mixedT, ident, gmix_s, gffn_s, rho, ucs, qre, qim, Bl_s, Cl_s, Dl_s, wglu_s]:
            b.lw = None
            b.rd = []

        with ExitStack() as es:
            P = Prog(nc, es, semes)
            Win = P.buf("Win", [128, 8, INW], BF16)
            xb = [P.buf("xb%d" % i, [128, D], F32) for i in range(2)]
            ab = [P.buf("ab%d" % i, [128, D], BF16) for i in range(2)]
            aTs = [P.buf("aT%d" % i, [128, 8, 256], BF16) for i in range(2)]
            qks = [P.buf("qk%d" % i, [128, 12, 256], BF16) for i in range(2)]
            qraw = [P.buf("qraw%d" % i, [128, 256], F32) for i in range(2)]
            rA = [P.buf("rA%d" % i, [128, 256], F32) for i in range(2)]
            rB = [P.buf("rB%d" % i, [128, 256], F32) for i in range(2)]
            cs = [P.buf("cs0", [128, 2, 256], F32)] * 2
            vg = [P.buf("vg%d" % i, [128, 1536], BF16) for i in range(4)]
            khat = P.buf("khat", [128, NH, 128], BF16)
            Sm = [P.buf("Sm%d" % i, [128, 3, 128], BF16) for i in range(2)]
            ctmp = P.buf("ctmp", [128, 3, 128], F32)
            o = P.buf("o", [128, NH, 128], F32)
            sg = P.buf("sg", [128, NH * 128], F32)
            yr = P.buf("yr", [128, NH * 128], BF16)
            R = P.buf("R", [128, NH, 128], F32)
            Rb = P.buf("Rb", [128, NH, 128], BF16)
            mask_s = P.buf("mask_s", [128, NH, 128], F32)
            qdec_s = P.buf("qdec_s", [128, NH], F32)
            kdec_s = P.buf("kdec_s", [128, NH], F32)
            st = [P.buf("st%d" % i, [128, 8], F32) for i in range(2)]
            gst = P.buf("gst", [128, 4, NH], F32)
            psT = P.buf("psT", [128, 8, 128], BF16, ps=True)
            psq = [P.buf("psq%d" % i, [128, 512], F32, ps=True) for i in range(2)]
            psB = P.buf("psB", [128, 8, 128], BF16, ps=True)
            psS = P.buf("psS", [128, 4, 128], F32, ps=True)
            psO = P.buf("psO", [128, 4, 128], F32, ps=True)
            psC = P.buf("psC", [128, 4, 128], F32, ps=True)
            psKV = P.buf("psKV", [128, 4, 128], F32, ps=True)

            P.dma("sp", mask_s[:], maskT.rearrange("p (h i) -> p h i", h=NH), writes=[mask_s])
            P.dma("sp", qdec_s[:], qdec[:, :], writes=[qdec_s])
            P.dma("sp", kdec_s[:], kdec[:, :], writes=[kdec_s])
            P.op("dve", lambda e: e.memset(R[:], 0.0), [], [R])
            P.op("pool", lambda e: e.memset(Rb[:], 0.0), [], [Rb])
            win_v = w_in.rearrange("(c p) n -> p c n", p=128)
            pieces = [(0, 1024), (1024, 1024), (2048, 1024), (3072, 256)]
            ci = 0
            pieces = [(0, 768), (768, 768), (1536, 768), (2304, 768), (3072, 256)]
            stg_bufs = [xb[0], xb[1], o, sg]
            stg_views = [xb[0][:], xb[1][:], o[:].rearrange("p h e -> p (h e)"), sg[:]]
            for c in range(8):
                for (c0, w) in pieces:
                    sb = stg_bufs[ci % 4]
                    sbv = stg_views[ci % 4]
                    P.dma("sp" if ci % 2 == 0 else "act", sbv[:, 0:w], win_v[:, c, c0:c0 + w], writes=[sb])
                    eng = ["act", "dve", "dve", "pool"][ci % 4]
                    if eng == "act":
                        P.op("act", lambda e, sbv=sbv, c=c, c0=c0, w=w: e.activation(Win[:, c, c0:c0 + w], sbv[:, 0:w], AF.Copy), [sb], [Win])
                    else:
                        P.op(eng, lambda e, sbv=sbv, c=c, c0=c0, w=w: e.tensor_copy(Win[:, c, c0:c0 + w], sbv[:, 0:w]), [sb], [Win])
                    ci += 1

            def load_x_tile(t, dst):
                if t == 0:
                    P.op("pool", lambda e: e.memset(dst[:], 0.0), [], [dst])
                    P.dma("sp", dst[48:64, :], meta[:, :], writes=[dst])
                    P.dma("sp", dst[64:128, :], x[0:64, :], writes=[dst])
                elif t == NT - 1:
                    P.op("pool", lambda e: e.memset(dst[:], 0.0), [], [dst])
                    P.dma("sp", dst[0:64, :], x[SEQ - 64:SEQ, :], writes=[dst])
                else:
                    P.dma("sp", dst[:], x[128 * t - 64:128 * t + 64, :], writes=[dst])

            def rms_to_bf(src, dst_bf, stt, scratch):
                P.op("act", lambda e: e.activation(scratch[:], src[:], AF.Square, accum_out=stt[:, 0:1]), [src], [scratch, stt])
                P.op("dve", lambda e: e.tensor_scalar(stt[:, 1:2], stt[:, 0:1], 1.0 / D, EPS, ALU.mult, ALU.add), [stt], [stt])
                P.op("act", lambda e: e.activation(stt[:, 2:3], stt[:, 1:2], AF.Sqrt), [stt], [stt])
                P.op("dve", lambda e: e.reciprocal(stt[:, 3:4], stt[:, 2:3]), [stt], [stt])
                P.op("act", lambda e: e.activation(dst_bf[:], src[:], AF.Copy, scale=stt[:, 3:4]), [src, stt], [dst_bf])

            NCB = 2
            cst = [P.buf("cst%d" % i, [128, 512], F32) for i in range(NCB)]
            cbf = [P.buf("cbf%d" % i, [128, 512], BF16) for i in range(NCB)]
            conv = []
            for e_ in range(NE):
                gv = w_gate[e_].rearrange("(c p) f -> p c f", p=128)
                uv = w_up[e_].rearrange("(c p) f -> p c f", p=128)
                dv = w_down[e_].rearrange("(c p) f -> p c f", p=128)
                for q4 in range(4):
                    conv.append((gv[:, 2 * q4:2 * q4 + 2, :], 2, sc_gate[e_][:, 512 * q4:512 * q4 + 512]))
                    conv.append((uv[:, 2 * q4:2 * q4 + 2, :], 2, sc_up[e_][:, 512 * q4:512 * q4 + 512]))
                    conv.append((dv[:, q4 // 2:q4 // 2 + 1, 512 * (q4 % 2):512 * (q4 % 2) + 512], 1, sc_down[e_][:, 512 * q4:512 * q4 + 512]))
            cvi = [0]
            LAG = 1

            def do_conv(kn):
                for _ in range(kn):
                    i = cvi[0]
                    cvi[0] += 1
                    if i < len(conv):
                        src, nc_, dst = conv[i]
                        bi = i % NCB
                        P.dma("sp", cst[bi][:].rearrange("p (c f) -> p c f", c=nc_), src, writes=[cst[bi]])
                    j = i - LAG
                    if 0 <= j < len(conv):
                        src, nc_, dst = conv[j]
                        bj = j % NCB
                        P.op("act", lambda e, bj=bj: e.activation(cbf[bj][:], cst[bj][:], AF.Copy), [cst[bj]], [cbf[bj]])
                        P.dma("sp", dst, cbf[bj][:], reads=[cbf[bj]], writes=[], sigbuf=cbf[bj])

            tcount = 0
            ngroups = (NT + 1) // 2

            def ret_pieces(tiles, qk, vgs):
                pcs = []
                for li, t in enumerate(tiles):
                    vt = vgs[li]
                    cols = slice(li * 128, (li + 1) * 128)

                    def p0(cols=cols):
                        def trk(e):
                            ins = None
                            for h in range(NH):
                                ins = e.transpose(psB[:, h, :], qk[:, 6 + h, cols], ident[:])
                            return ins
                        P.op("pe", trk, [qk, ident], [psB])
                        P.op("dve", lambda e: e.tensor_tensor(khat[:], psB[:, 0:NH, :], bc(kdec_s[:].unsqueeze(2), [128, NH, 128]), ALU.mult), [psB, kdec_s], [khat])
                    pcs.append(p0)
                    for half in range(2):
                        h0 = 3 * half
                        smb = Sm[half]

                        def p1(h0=h0, smb=smb, cols=cols):
                            def mmS(e):
                                ins = None
                                for hl in range(3):
                                    ins = e.matmul(psS[:, hl, :], qk[:, 6 + h0 + hl, cols], qk[:, h0 + hl, cols], start=True, stop=True)
                                return ins
                            P.op("pe", mmS, [qk], [psS])
                            P.op("dve", lambda e: e.tensor_tensor(smb[:], psS[:, 0:3, :], mask_s[:, h0:h0 + 3, :], ALU.mult), [psS, mask_s], [smb])

                            def mmC(e):
                                ins = None
                                for hl in range(3):
                                    ins = e.matmul(psC[:, hl, :], qk[:, h0 + hl, cols], Rb[:, h0 + hl, :], start=True, stop=True)
                                return ins
                            P.op("pe", mmC, [qk, Rb], [psC])
                            P.op("dve", lambda e: e.tensor_tensor(ctmp[:], psC[:, 0:3, :], bc(qdec_s[:, h0:h0 + 3].unsqueeze(2), [128, 3, 128]), ALU.mult), [psC, qdec_s], [ctmp])
                        pcs.append(p1)

                        def p2(h0=h0, smb=smb, vt=vt):
                            def mmO(e):
                                ins = None
                                for hl in range(3):
                                    h = h0 + hl
                                    ins = e.matmul(psO[:, hl, :], smb[:, hl, :], vt[:, h * 128:(h + 1) * 128], start=True, stop=True)
                                return ins
                            P.op("pe", mmO, [smb, vt], [psO])

                            def mmKV(e):
                                ins = None
                                for hl in range(3):
                                    h = h0 + hl
                                    ins = e.matmul(psKV[:, hl, :], khat[:, h, :], vt[:, h * 128:(h + 1) * 128], start=True, stop=True)
                                return ins
                            P.op("pe", mmKV, [khat, vt], [psKV])
                            P.op("dve", lambda e: e.tensor_tensor(o[:, h0:h0 + 3, :], psO[:, 0:3, :], ctmp[:], ALU.add), [psO, ctmp], [o])
                            for hl in range(3):
                                h = h0 + hl
                                P.op("dve", lambda e, h=h, hl=hl: e.scalar_tensor_tensor(R[:, h, :], R[:, h, :], g128[h], psKV[:, hl, :], ALU.mult, ALU.add), [R, psKV], [R])
                        pcs.append(p2)

                        def p2b(h0=h0):
                            P.op("act", lambda e: e.activation(Rb[:, h0:h0 + 3, :], R[:, h0:h0 + 3, :], AF.Copy), [R], [Rb])
                        pcs.append(p2b)

                    def p3a():
                        P.op("act", lambda e: e.activation(sg[:].rearrange("p (h e) -> p h e", h=NH), o[:], AF.Square), [o], [sg])
                    pcs.append(p3a)

                    def p3b():
                        P.op("dve", lambda e: e.tensor_reduce(gst[:, 0, :], o[:], AX.X, ALU.add), [o], [gst])
                        P.op("dve", lambda e: e.tensor_reduce(gst[:, 1, :], sg[:].rearrange("p (h e) -> p h e", h=NH), AX.X, ALU.add), [sg, gst], [gst])
                        P.op("dve", lambda e: e.tensor_scalar(gst[:, 0, :], gst[:, 0, :], 1.0 / 128, None, ALU.mult), [gst], [gst])
                        P.op("dve", lambda e: e.tensor_tensor(gst[:, 2, :], gst[:, 0, :], gst[:, 0, :], ALU.mult), [gst], [gst])
                        P.op("dve", lambda e: e.scalar_tensor_tensor(gst[:, 1, :], gst[:, 1, :], 1.0 / 128, gst[:, 2, :], ALU.mult, ALU.subtract), [gst], [gst])
                        P.op("dve", lambda e: e.tensor_scalar(gst[:, 1, :], gst[:, 1, :], EPS, None, ALU.add), [gst], [gst])
                    pcs.append(p3b)

                    def p3c(vt=vt):
                        P.op("act", lambda e: e.activation(gst[:, 2, :], gst[:, 1, :], AF.Sqrt), [gst], [gst])
                    pcs.append(p3c)

                    def p4a():
                        P.op("dve", lambda e: e.reciprocal(gst[:, 3, :], gst[:, 2, :]), [gst], [gst])
                        P.op("dve", lambda e: e.tensor_tensor(o[:], o[:], bc(gst[:, 0, :].unsqueeze(2), [128, NH, 128]), ALU.subtract), [o, gst], [o])
                        P.op("dve", lambda e: e.tensor_tensor(o[:], o[:], bc(gst[:, 3, :].unsqueeze(2), [128, NH, 128]), ALU.mult), [o, gst], [o])
                    pcs.append(p4a)

                    def p4b(vt=vt):
                        P.op("act", lambda e: e.activation(sg[:], vt[:, 768:1536], AF.Silu), [vt], [sg])
                    pcs.append(p4b)

                    def p5():
                        P.op("pool", lambda e: e.tensor_tensor(yr[:], o[:].rearrange("p h e -> p (h e)"), sg[:], ALU.mult), [o, sg], [yr])
                    pcs.append(p5)

                    def p6(t=t):
                        def try_(e):
                            ins = None
                            for h in range(NH):
                                ins = e.transpose(psB[:, h, :], yr[:, h * 128:(h + 1) * 128], ident[:])
                            return ins
                        P.op("pe", try_, [yr, ident], [psB])
                        P.op("act", lambda e: e.activation(mixedT[:, 2:8, 128 * t:128 * (t + 1)], psB[:, 0:NH, :], AF.Copy), [psB], [mxb[t]])
                    pcs.append(p6)
                return pcs

            def A_pieces(gi):
                nonlocal_t = []
                tiles_ = [t for t in (2 * gi, 2 * gi + 1) if t < NT]
                aTn = aTs[gi % 2]
                pcs = []
                for li, t in enumerate(tiles_):
                    k_ = tcnt[0]
                    tcnt[0] += 1
                    xs, a_, stt = xb[k_ % 2], ab[k_ % 2], st[k_ % 2]

                    def pa1(t=t, xs=xs):
                        load_x_tile(t, xs)
                    pcs.append(pa1)

                    def pa2(xs=xs, a_=a_, stt=stt):
                        P.op("act", lambda e: e.activation(a_[:], xs[:], AF.Square, accum_out=stt[:, 0:1]), [xs], [a_, stt])
                    pcs.append(pa2)

                    def pb1(stt=stt):
                        P.op("dve", lambda e: e.tensor_scalar(stt[:, 1:2], stt[:, 0:1], 1.0 / D, EPS, ALU.mult, ALU.add), [stt], [stt])
                        P.op("act", lambda e: e.activation(stt[:, 2:3], stt[:, 1:2], AF.Sqrt), [stt], [stt])
                    pcs.append(pb1)

                    def pb2(xs=xs, a_=a_, stt=stt):
                        P.op("dve", lambda e: e.reciprocal(stt[:, 3:4], stt[:, 2:3]), [stt], [stt])
                        P.op("act", lambda e: e.activation(a_[:], xs[:], AF.Copy, scale=stt[:, 3:4]), [xs, stt], [a_])
                    pcs.append(pb2)

                    def pc(li=li, a_=a_, aTn=aTn):
                        def tr(e):
                            ins = None
                            for c in range(8):
                                ins = e.transpose(psT[:, c, :], a_[:, c * 128:(c + 1) * 128], ident[:])
                            return ins
                        P.op("pe", tr, [a_, ident], [psT])
                        P.op("dve", lambda e: e.tensor_tensor(aTn[:, :, li * 128:(li + 1) * 128], psT[:], bc(gmix_s[:].unsqueeze(2), [128, 8, 128]), ALU.mult), [psT, gmix_s], [aTn])
                    pcs.append(pc)
                return pcs

            tcnt = [0]
            for fn in A_pieces(0):
                fn()
            pending = []
            for gi in range(ngroups):
                if gi % 4 == 0:
                    P.new_epoch()
                tiles = [t for t in (2 * gi, 2 * gi + 1) if t < NT]
                n = 128 * len(tiles)
                tok0 = 128 * tiles[0]
                mxs = [mxb[t] for t in tiles]
                csb = cs[gi % 2]
                qk = qks[gi % 2]
                aT = aTs[gi % 2]
                vgs = vg[2 * (gi % 2):2 * (gi % 2) + 2]
                nextA = A_pieces(gi + 1) if gi + 1 < ngroups else []
                P.dma("act", csb[:, 0, 0:n], cosT[:, tok0:tok0 + n], writes=[csb])
                P.dma("act", csb[:, 1, 0:n], sinT[:, tok0:tok0 + n], writes=[csb])
                pi = 0
                for blk in range(14):
                    ps = psq[pi % 2]
                    pi += 1
                    col0 = blk * 128

                    def mm(e, ps=ps, col0=col0, n=n, aT=aT):
                        ins = None
                        for c in range(8):
                            ins = e.matmul(ps[:, 0:n], Win[:, c, col0:col0 + 128], aT[:, c, 0:n], start=(c == 0), stop=(c == 7))
                        return ins
                    P.op("pe", mm, [Win, aT], [ps])
                    if blk < 2:
                        P.op("act", lambda e, ps=ps, blk=blk, n=n, tok0=tok0: e.activation(mixedT[:, blk, tok0:tok0 + n], ps[:, 0:n], AF.Copy), [ps], mxs)
                    else:
                        hh = blk - 2
                        qr = qraw[hh % 2]
                        A = rA[hh % 2]
                        B = rB[hh % 2]
                        P.op("act", lambda e, ps=ps, qr=qr, n=n: e.activation(qr[:, 0:n], ps[:, 0:n], AF.Copy), [ps], [qr])
                        P.op("dve", lambda e, qr=qr, A=A, n=n, csb=csb: e.tensor_tensor(A[:, 0:n], qr[:, 0:n], csb[:, 0, 0:n], ALU.mult), [qr, csb], [A])
                        P.op("dve", lambda e, qr=qr, B=B, n=n, csb=csb: e.tensor_tensor(B[0:64, 0:n], qr[64:128, 0:n], csb[64:128, 1, 0:n], ALU.mult), [qr, csb], [B])
                        P.op("dve", lambda e, qr=qr, B=B, n=n, csb=csb: e.tensor_tensor(B[64:128, 0:n], qr[0:64, 0:n], csb[0:64, 1, 0:n], ALU.mult), [qr, csb, B], [B])
                        P.op("pool", lambda e, A=A, B=B, hh=hh, n=n, qk=qk: e.tensor_tensor(qk[:, hh, 0:n], A[:, 0:n], B[:, 0:n], ALU.add), [A, B], [qk])
                        do_conv(1)
                    for _ in range(2 if blk % 2 == 1 else 1):
                        if pending:
                            pending.pop(0)()
                    if nextA and blk % 2 == 0:
                        nextA.pop(0)()
                for li, t in enumerate(tiles):
                    vt = vgs[li]
                    for cb in range(3):
                        ps = psq[pi % 2]
                        pi += 1

                        def mmv(e, ps=ps, li=li, cb=cb, aT=aT):
                            ins = None
                            for c in range(8):
                                ins = e.matmul(ps[:, :], aT[:, c, li * 128:(li + 1) * 128], Win[:, c, 1792 + cb * 512:1792 + (cb + 1) * 512], start=(c == 0), stop=(c == 7))
                            return ins
                        P.op("pe", mmv, [Win, aT], [ps])
                        P.op("act", lambda e, ps=ps, vt=vt, cb=cb: e.activation(vt[:, cb * 512:(cb + 1) * 512], ps[:, :], AF.Copy), [ps], [vt])
                        for _ in range(2):
                            if pending:
                                pending.pop(0)()
                        if nextA:
                            nextA.pop(0)()
                while pending:
                    pending.pop(0)()
                while nextA:
                    nextA.pop(0)()
                pending = ret_pieces(tiles, qk, vgs)
            while pending:
                pending.pop(0)()
            do_conv(len(conv) + LAG + 1 - cvi[0] if cvi[0] < len(conv) + LAG else 0)
            P.emit(final_waits=cbf)
        nc.all_engine_barrier()
        for b in [mixedT, ident, gmix_s, gffn_s, rho, ucs, qre, qim, Bl_s, Cl_s, Dl_s, wglu_s] + mxb:
            b.lw = None
            b.rd = []

        with ExitStack() as es:
            P = Prog(nc, es, semes)
            Wout = P.buf("Wout", [128, 8, D], BF16)
            wr_s = P.buf("wr_s", [128, 8, 20], BF16)
            br_s = P.buf("br_s", [128, 20], F32)
            gfin_s = P.buf("gfin_s", [128, D], F32)
            h2 = [P.buf("h2_%d" % i, [128, D], F32) for i in range(4)]
            tbf = [P.buf("tbf0", [128, D], BF16)] * 2
            tT = P.buf("tT", [128, 8, TB], BF16)
            comb = P.buf("comb", [128, 5, NE], F32)
            rt = P.buf("rt", [128, 5, 64], F32)
            st2 = P.buf("st2", [128, 8, 5], F32)
            st3 = P.buf("st3", [128, 8, 5], F32)
            wg = [P.buf("wg%d" % i, [128, 8, 256], BF16) for i in range(2)]
            wu = [P.buf("wu%d" % i, [128, 8, 256], BF16) for i in range(2)]
            wd = [P.buf("wd%d" % i, [128, 2, D], BF16) for i in range(2)]
            sil = [P.buf("sil%d" % i, [128, TB], BF16) for i in range(2)]
            actT = [P.buf("actT%d" % i, [128, 2, TB], BF16) for i in range(2)]
            psDs = [P.buf("psD%d" % i, [128, D], F32, ps=True) for i in range(2)]
            psGU = [P.buf("psGU%d" % i, [128, TB], F32, ps=True) for i in range(3)]
            psS5 = P.buf("psS5", [128, TB], F32, ps=True)
            psT2b = psS5
            psT2 = psS5[:].bitcast(BF16).rearrange("p (c t) -> p c t", c=8)
            psR = psS5
            dcount = [0]
            gucount = [0]
            Dre = P.buf("Dre", [128, 8, TS], F32)
            Dim = P.buf("Dim", [128, 8, TS], F32)
            bur = [P.buf("bur%d" % i, [128, TS], F32) for i in range(2)]
            bui = [P.buf("bui%d" % i, [128, TS], F32) for i in range(2)]
            mrs = [P.buf("mr%d" % i, [128, TS], F32) for i in range(2)]
            mis = [P.buf("mi%d" % i, [128, TS], F32) for i in range(2)]
            wrs = [P.buf("Wr%d" % i, [128, TS], F32) for i in range(2)]
            wis = [P.buf("Wi%d" % i, [128, TS], F32) for i in range(2)]
            ta = P.buf("ta", [128, TS], F32)
            tc = P.buf("tc", [128, TS], F32)
            tds = [[P.buf("td%d_%d" % (i, q), [128, TS], F32) for q in range(4)] for i in range(2)]
            Xb = P.buf("Xb", [128, 8, 2, TS], BF16)
            carry = P.buf("carry", [128, 8, 2], F32)
            w0 = P.buf("w0", [128, 8, 2], F32)
            w0s = P.buf("w0s", [128, 8, 2], F32)
            geT = P.buf("geT", [128, 2, TS], BF16)
            ysb = [P.buf("ysb%d" % i, [128, TS], F32) for i in range(2)]
            y2 = [P.buf("y2_%d" % i, [128, TS], F32) for i in range(2)]
            zz = y2
            sgm = [P.buf("sgm%d" % i, [128, TS], F32) for i in range(2)]
            _tv = tT[:].bitcast(F32)
            t1 = _tv[:, :, 0:TS // 2]
            t2 = _tv[:, :, TS // 2:TS]

            class VBuf(Buf):
                def __init__(self, name, ap):
                    Buf.__init__(self, name, None)
                    self.ap_ = ap

                def __getitem__(self, k):
                    return self.ap_[k]
            h2x = VBuf("h2x", Dre[:].rearrange("p k t -> p (k t)")[:, 0:D])
            dimv = Dim[:].bitcast(BF16).rearrange("p k t -> p (k t)")
            tTx_ap = dimv[:, 0:1024].rearrange("p (c t) -> p c t", c=8)
            actTx_ap = [dimv[:, 1024 + 256 * i:1024 + 256 * (i + 1)].rearrange("p (f t) -> p f t", f=2) for i in range(2)]
            silx_ap = [dimv[:, 1536 + 128 * i:1536 + 128 * (i + 1)] for i in range(2)]
            tTxb = Buf("tTxb", None)
            actTxb = [Buf("actTxb%d" % i, None) for i in range(2)]
            silxb = [Buf("silxb%d" % i, None) for i in range(2)]

            def tTa(c, li):
                return tT[:, c, li * 128:(li + 1) * 128] if li < 4 else tTx_ap[:, c, :]

            def tTbuf(li):
                return tT if li < 4 else tTxb

            wov = w_out.rearrange("(c p) n -> p c n", p=128)
            for c in range(8):
                sb = h2[2 + c % 2]
                P.dma("sp", sb[:], wov[:, c, :], writes=[sb])
                P.op("pool" if c % 2 else "act", (lambda e, sb=sb, c=c: e.tensor_copy(Wout[:, c, :], sb[:])) if c % 2 else (lambda e, sb=sb, c=c: e.activation(Wout[:, c, :], sb[:], AF.Copy)), [sb], [Wout])
            P.dma("sp", h2[2][:, 0:160].rearrange("p (c f) -> p c f", c=8), wr.rearrange("(c p) f -> p c f", p=128), writes=[h2[2]])
            P.op("act", lambda e: e.activation(wr_s[:], h2[2][:, 0:160].rearrange("p (c f) -> p c f", c=8), AF.Copy), [h2[2]], [wr_s])
            P.dma("sp", br_s[:], br[:, :], writes=[br_s])
            P.dma("sp", gfin_s[:], gfin[:, :], writes=[gfin_s])
            P.op("dve", lambda e: e.memset(carry[:], 0.0), [], [carry])
            P.op("dve", lambda e: e.memset(Dre[:, :, 0:1], 1.0), [], [Dre])
            P.op("dve", lambda e: e.memset(Dim[:, :, 0:1], 0.0), [], [Dim])
            P.op("dve", lambda e: e.tensor_copy(Dre[:, :, 1:2], ucs[:, 0, :].unsqueeze(2)), [ucs, Dre], [Dre])
            P.op("dve", lambda e: e.tensor_copy(Dim[:, :, 1:2], ucs[:, 1, :].unsqueeze(2)), [ucs, Dim], [Dim])
            n = 2
            while n < TS:
                h = n // 2
                P.op("dve", lambda e, h=h: e.tensor_tensor(t1[:, :, 0:1], Dre[:, :, h:h + 1], Dre[:, :, h:h + 1], ALU.mult), [Dre], [tT])
                P.op("dve", lambda e, h=h: e.tensor_tensor(t2[:, :, 0:1], Dim[:, :, h:h + 1], Dim[:, :, h:h + 1], ALU.mult), [Dim], [tT])
                P.op("dve", lambda e, n=n: e.tensor_tensor(Dre[:, :, n:n + 1], t1[:, :, 0:1], t2[:, :, 0:1], ALU.subtract), [tT, Dre], [Dre])
                P.op("dve", lambda e, h=h: e.tensor_tensor(t1[:, :, 0:1], Dre[:, :, h:h + 1], Dim[:, :, h:h + 1], ALU.mult), [Dre, Dim], [tT])
                P.op("dve", lambda e, n=n: e.tensor_scalar(Dim[:, :, n:n + 1], t1[:, :, 0:1], 2.0, None, ALU.mult), [tT, Dim], [Dim])
                m = n - 1
                P.op("dve", lambda e, n=n, m=m: e.tensor_tensor(t1[:, :, 0:m], Dre[:, :, 1:n], bc(Dre[:, :, n:n + 1], [128, 8, m]), ALU.mult), [Dre], [tT])
                P.op("dve", lambda e, n=n, m=m: e.tensor_tensor(t2[:, :, 0:m], Dim[:, :, 1:n], bc(Dim[:, :, n:n + 1], [128, 8, m]), ALU.mult), [Dim], [tT])
                P.op("dve", lambda e, n=n, m=m: e.tensor_tensor(Dre[:, :, n + 1:2 * n], t1[:, :, 0:m], t2[:, :, 0:m], ALU.subtract), [tT, Dre], [Dre])
                P.op("dve", lambda e, n=n, m=m: e.tensor_tensor(t1[:, :, 0:m], Dre[:, :, 1:n], bc(Dim[:, :, n:n + 1], [128, 8, m]), ALU.mult), [Dre, Dim], [tT])
                P.op("dve", lambda e, n=n, m=m: e.tensor_tensor(t2[:, :, 0:m], Dim[:, :, 1:n], bc(Dre[:, :, n:n + 1], [128, 8, m]), ALU.mult), [Dre, Dim], [tT])
                P.op("dve", lambda e, n=n, m=m: e.tensor_tensor(Dim[:, :, n + 1:2 * n], t1[:, :, 0:m], t2[:, :, 0:m], ALU.add), [tT, Dim], [Dim])
                n *= 2

            def s5_sched(bs, off, sched, fast=False):
                e1 = "dve" if fast else "pool"
                c0 = bs * TS
                n = min(TS, LP - c0)
                tl = [mxb[t] for t in range(c0 // 128, (c0 + n) // 128)]

                def at(slot, fn):
                    sched.setdefault(slot, []).append(fn)
                def pre():
                    P.op("dve", lambda e: e.tensor_tensor(w0[:, :, 0], carry[:, :, 0], ucs[:, 0, :], ALU.mult), [carry, ucs], [w0])
                    P.op("dve", lambda e: e.tensor_tensor(w0[:, :, 1], carry[:, :, 1], ucs[:, 1, :], ALU.mult), [carry, ucs, w0], [w0])
                    P.op("dve", lambda e: e.tensor_tensor(w0s[:, :, 0], w0[:, :, 0], w0[:, :, 1], ALU.subtract), [w0], [w0s])
                    P.op("dve", lambda e: e.tensor_tensor(w0[:, :, 0], carry[:, :, 0], ucs[:, 1, :], ALU.mult), [carry, ucs, w0], [w0])
                    P.op("dve", lambda e: e.tensor_tensor(w0[:, :, 1], carry[:, :, 1], ucs[:, 0, :], ALU.mult), [carry, ucs, w0], [w0])
                    P.op("dve", lambda e: e.tensor_tensor(w0s[:, :, 1], w0[:, :, 0], w0[:, :, 1], ALU.add), [w0], [w0s])
                at(off + 1, pre)
                hA, hB = slice(0, n), slice(256, 256 + n)
                for k in range(8):
                    par = k % 2
                    j, kk = k // 4, k % 4
                    br_, bi_, mr_, mi_, wr_, wi_ = bur[par], bui[par], mrs[par], mis[par], wrs[par], wis[par]
                    d0, d1, d2, d3 = tds[par]

                    def st0(k=k, j=j, kk=kk, br_=br_, bi_=bi_):
                        def mm(e):
                            e.matmul(psS5[:, hA], Bl_s[:, (j * 8 + kk * 2) * 128:(j * 8 + kk * 2 + 1) * 128], mixedT[:, j, c0:c0 + n], start=True, stop=True)
                            return e.matmul(psS5[:, hB], Bl_s[:, (j * 8 + kk * 2 + 1) * 128:(j * 8 + kk * 2 + 2) * 128], mixedT[:, j, c0:c0 + n], start=True, stop=True)
                        P.op("pe", mm, [Bl_s] + tl, [psS5])
                        P.op("act", lambda e: e.activation(br_[:, 0:n], psS5[:, hA], AF.Copy), [psS5], [br_])
                        P.op("act", lambda e: e.activation(bi_[:, 0:n], psS5[:, hB], AF.Copy), [psS5], [bi_])

                    def st1(k=k, br_=br_, bi_=bi_, mr_=mr_, mi_=mi_):
                        P.op(e1, lambda e: e.tensor_tensor(ta[:, 0:n], br_[:, 0:n], Dre[:, k, 0:n], ALU.mult), [br_, Dre], [ta])
                        P.op(e1, lambda e: e.tensor_tensor(mr_[:, 0:n], bi_[:, 0:n], Dim[:, k, 0:n], ALU.mult), [bi_, Dim], [mr_])
                        P.op(e1, lambda e: e.tensor_tensor(mr_[:, 0:n], ta[:, 0:n], mr_[:, 0:n], ALU.add), [ta, mr_], [mr_])
                        P.op(e1, lambda e: e.tensor_tensor(tc[:, 0:n], bi_[:, 0:n], Dre[:, k, 0:n], ALU.mult), [bi_, Dre], [tc])
                        P.op(e1, lambda e: e.tensor_tensor(mi_[:, 0:n], br_[:, 0:n], Dim[:, k, 0:n], ALU.mult), [br_, Dim], [mi_])
                        P.op(e1, lambda e: e.tensor_tensor(mi_[:, 0:n], tc[:, 0:n], mi_[:, 0:n], ALU.subtract), [tc, mi_], [mi_])

                    def st2(k=k, mr_=mr_, mi_=mi_, wr_=wr_, wi_=wi_):
                        P.op("dve", lambda e: e.tensor_tensor_scan(wr_[:, 0:n], bc(rho[:, k:k + 1], [128, n]), mr_[:, 0:n], w0s[:, k, 0:1], ALU.mult, ALU.add), [rho, mr_, w0s], [wr_])
                        P.op("dve", lambda e: e.tensor_tensor_scan(wi_[:, 0:n], bc(rho[:, k:k + 1], [128, n]), mi_[:, 0:n], w0s[:, k, 1:2], ALU.mult, ALU.add), [rho, mi_, w0s], [wi_])

                    def st3(k=k, wr_=wr_, wi_=wi_, d0=d0, d1=d1, d2=d2, d3=d3):
                        P.op("pool", lambda e: e.tensor_tensor(d0[:, 0:n], wr_[:, 0:n], Dre[:, k, 0:n], ALU.mult), [wr_, Dre], [d0])
                        P.op("pool", lambda e: e.tensor_tensor(d1[:, 0:n], wi_[:, 0:n], Dim[:, k, 0:n], ALU.mult), [wi_, Dim], [d1])
                        P.op("pool", lambda e: e.tensor_tensor(Xb[:, k, 0, 0:n], d0[:, 0:n], d1[:, 0:n], ALU.subtract), [d0, d1], [Xb])
                        P.op("pool", lambda e: e.tensor_tensor(d2[:, 0:n], wr_[:, 0:n], Dim[:, k, 0:n], ALU.mult), [wr_, Dim], [d2])
                        P.op("pool", lambda e: e.tensor_tensor(d3[:, 0:n], wi_[:, 0:n], Dre[:, k, 0:n], ALU.mult), [wi_, Dre], [d3])

                    def st4(k=k, d0=d0, d1=d1, d2=d2, d3=d3):
                        P.op("dve", lambda e: e.scalar_tensor_tensor(Xb[:, k, 1, 0:n], d2[:, 0:n], -1.0, d3[:, 0:n], ALU.mult, ALU.subtract), [d2, d3, Xb], [Xb])
                        P.op("dve", lambda e: e.tensor_tensor(carry[:, k, 0:1], d0[:, n - 1:n], d1[:, n - 1:n], ALU.subtract), [d0, d1, carry], [carry])
                        P.op("dve", lambda e: e.tensor_tensor(carry[:, k, 1:2], d2[:, n - 1:n], d3[:, n - 1:n], ALU.add), [d2, d3, carry], [carry])
                    for si, fn in enumerate((st0, st1, st2, st3, st4)):
                        at(off + k + si, fn)
                T = off + 12
                hs = [hA, hB]

                def T0():
                    for j in range(2):
                        def mmy(e, j=j):
                            first = True
                            for kk in range(4):
                                k = 4 * j + kk
                                for c in range(2):
                                    e.matmul(psS5[:, hs[j]], Cl_s[:, (k * 2 + c) * 128:(k * 2 + c + 1) * 128], Xb[:, k, c, 0:n], start=first, stop=False)
                                    first = False
                            return e.matmul(psS5[:, hs[j]], Dl_s[:, j * 128:(j + 1) * 128], mixedT[:, j, c0:c0 + n], start=False, stop=True)
                        P.op("pe", mmy, [Cl_s, Xb, Dl_s] + tl, [psS5])
                    for j in range(2):
                        P.op("act", lambda e, j=j: e.activation(ysb[j][:, 0:n], psS5[:, hs[j]], AF.Copy), [psS5], [ysb[j]])
                        P.op("act", lambda e, j=j: e.activation(y2[j][:, 0:n], psS5[:, hs[j]], AF.Square), [psS5], [y2[j]])

                def T1():
                    for j in range(2):
                        P.op("pool", lambda e, j=j: e.tensor_scalar(y2[j][:, 0:n], y2[j][:, 0:n], 0.044715, 1.0, ALU.mult, ALU.add), [y2[j]], [y2[j]])

                def T2():
                    for j in range(2):
                        P.op("dve", lambda e, j=j: e.tensor_tensor(zz[j][:, 0:n], y2[j][:, 0:n], ysb[j][:, 0:n], ALU.mult), [y2[j], ysb[j]], [zz[j]])

                def T3():
                    for j in range(2):
                        P.op("act", lambda e, j=j: e.activation(sgm[j][:, 0:n], zz[j][:, 0:n], AF.Sigmoid, scale=GELU_C), [zz[j]], [sgm[j]])

                def T4():
                    for j in range(2):
                        P.op("dve", lambda e, j=j: e.tensor_tensor(geT[:, j, 0:n], sgm[j][:, 0:n], ysb[j][:, 0:n], ALU.mult), [sgm[j], ysb[j]], [geT])

                def T5():
                    for jo in range(2):
                        def mmg(e, jo=jo):
                            e.matmul(psS5[:, hs[jo]], wglu_s[:, 0, jo * 128:(jo + 1) * 128], geT[:, 0, 0:n], start=True, stop=False)
                            return e.matmul(psS5[:, hs[jo]], wglu_s[:, 1, jo * 128:(jo + 1) * 128], geT[:, 1, 0:n], start=False, stop=True)
                        P.op("pe", mmg, [wglu_s, geT], [psS5])
                    for jo in range(2):
                        P.op("act", lambda e, jo=jo: e.activation(sgm[jo][:, 0:n], psS5[:, hs[jo]], AF.Sigmoid), [psS5], [sgm[jo]])

                def T6():
                    for jo in range(2):
                        P.op("pool", lambda e, jo=jo: e.tensor_tensor(mixedT[:, jo, c0:c0 + n], sgm[jo][:, 0:n], geT[:, jo, 0:n], ALU.mult), [sgm[jo], geT], tl)
                for si, fn in enumerate((T0, T1, T2, T3, T4, T5, T6)):
                    at(T + si, fn)

            def make_sched(bl, fast=False, step=None):
                sched = {}
                off = 0
                for bs in bl:
                    if bs < NBS:
                        s5_sched(bs, off, sched, fast)
                        off += step if step is not None else (10 if fast else 14)
                return sched

            def run_slot(sched, slot):
                for fn in sched.pop(slot, []):
                    fn()

            def run_rest(sched):
                for slot in sorted(sched.keys()):
                    for fn in sched[slot]:
                        fn()
                sched.clear()

            RATIO = TB // TS
            run_rest(make_sched(range(0, RATIO), fast=True))
            wcount = 0
            tcount = 0
            NBM = NB - 1
            for b in range(NBM):
                if b % 2 == 0:
                    P.new_epoch()
                if b < NBM - 2:
                    sched = make_sched(range(RATIO * (b + 1), RATIO * (b + 2)))
                elif b == NBM - 2:
                    sched = make_sched(range(RATIO * (b + 1), NBS), step=10)
                else:
                    sched = {}
                slot = [0]
                c0 = b * TB
                n = TB
                tiles = list(range(4 * b, 4 * b + 4)) + ([NT - 1] if b == NBM - 1 else [])
                nl = len(tiles)
                h2c = h2 + ([h2x] if nl == 5 else [])
                if nl == 5:
                    P.op("dve", lambda e: e.memset(w0[:, 0:1, 0:1], 0.0), [], [Dre, Dim, w0, h2x, tTxb] + actTxb + silxb)
                for li, t in enumerate(tiles):
                    hb = h2c[li]
                    if t == 0:
                        P.op("pool", lambda e, hb=hb: e.memset(hb[:], 0.0), [], [hb])
                        P.dma("sp", hb[48:64, :], meta[:, :], writes=[hb])
                        P.dma("sp", hb[64:128, :], x[0:64, :], writes=[hb])
                    elif t == NT - 1:
                        P.op("pool", lambda e, hb=hb: e.memset(hb[:], 0.0), [], [hb])
                        P.dma("sp", hb[0:64, :], x[SEQ - 64:SEQ, :], writes=[hb])
                    else:
                        P.dma("sp", hb[:], x[128 * t - 64:128 * t + 64, :], writes=[hb])
                def head_norm(lo, hi):
                    P.op("dve", lambda e: e.tensor_scalar(st2[:, 1, lo:hi], st2[:, 0, lo:hi], 1.0 / D, EPS, ALU.mult, ALU.add), [st2], [st2])
                    P.op("act", lambda e: e.activation(st2[:, 2, lo:hi], st2[:, 1, lo:hi], AF.Sqrt), [st2], [st2])
                    P.op("dve", lambda e: e.reciprocal(st2[:, 3, lo:hi], st2[:, 2, lo:hi]), [st2], [st2])
                for li, t in enumerate(tiles):
                    hb = h2c[li]
                    if li < 2:
                        pa_, pb_ = (psGU[1], psGU[2]) if li == 0 else (psGU[0], psS5)

                        def mmo0(e, t=t, pa_=pa_, pb_=pb_):
                            ins = None
                            for half, pp in enumerate((pa_, pb_)):
                                for c in range(8):
                                    ins = e.matmul(pp[:, 0:512], mixedT[:, c, 128 * t:128 * (t + 1)], Wout[:, c, half * 512:(half + 1) * 512], start=(c == 0), stop=(c == 7))
                            return ins
                        P.op("pe", mmo0, [mxb[t], Wout], [pa_, pb_])
                        P.op("dve", lambda e, hb=hb, pa_=pa_: e.tensor_tensor(hb[:, 0:512], hb[:, 0:512], pa_[:, 0:512], ALU.add), [hb, pa_], [hb])
                        P.op("dve", lambda e, hb=hb, pb_=pb_: e.tensor_tensor(hb[:, 512:1024], hb[:, 512:1024], pb_[:, 0:512], ALU.add), [hb, pb_], [hb])
                    else:
                        psD = psDs[dcount[0] % 2]
                        dcount[0] += 1

                        def mmo(e, t=t, psD=psD):
                            ins = None
                            for half in range(2):
                                for c in range(8):
                                    ins = e.matmul(psD[:, half * 512:(half + 1) * 512], mixedT[:, c, 128 * t:128 * (t + 1)], Wout[:, c, half * 512:(half + 1) * 512], start=(c == 0), stop=(c == 7))
                            return ins
                        P.op("pe", mmo, [mxb[t], Wout], [psD])
                        P.op("dve", lambda e, hb=hb, psD=psD: e.tensor_tensor(hb[:], hb[:], psD[:], ALU.add), [hb, psD], [hb])
                    P.op("act", lambda e, hb=hb, li=li: e.activation(actT[0][:].rearrange("p a b -> p (a b)"), hb[:], AF.Square, accum_out=st2[:, 0, li:li + 1]), [hb], [actT[0], st2])
                    if li == 1 and nl > 2:
                        head_norm(0, 2)
                head_norm(2 if nl > 2 else 0, nl)
                tb_aps = [tbf[0][:], actT[1][:].rearrange("p a b -> p (a b)")]
                tb_bufs = [tbf[0], actT[1]]
                pT_aps = [psT2, psGU[0][:].bitcast(BF16).rearrange("p (c t) -> p c t", c=8)]
                pT_bufs = [psS5, psGU[0]]
                for li, t in enumerate(tiles):
                    hb = h2c[li]
                    kq = li % 2
                    tb_ap, tb_b, pT_ap, pT_b = tb_aps[kq], tb_bufs[kq], pT_aps[kq], pT_bufs[kq]
                    P.op("act", lambda e, hb=hb, li=li, tb_ap=tb_ap: e.activation(tb_ap, hb[:], AF.Copy, scale=st2[:, 3, li:li + 1]), [hb, st2], [tb_b])

                    def tr2(e, tb_ap=tb_ap, pT_ap=pT_ap):
                        ins = None
                        for c in range(8):
                            ins = e.transpose(pT_ap[:, c, :], tb_ap[:, c * 128:(c + 1) * 128], ident[:])
                        return ins
                    P.op("pe", tr2, [tb_b, ident], [pT_b])

                    tT3 = tT[:, :, li * 128:(li + 1) * 128] if li < 4 else tTx_ap
                    P.op("dve", lambda e, tT3=tT3, pT_ap=pT_ap: e.tensor_tensor(tT3, pT_ap, bc(gffn_s[:].unsqueeze(2), [128, 8, 128]), ALU.mult), [pT_b, gffn_s], [tTbuf(li)])

                def head_router(nl):
                    def mmr(e):
                        ins = None
                        for li in range(nl):
                            for c in range(8):
                                ins = e.matmul(psR[:, 20 * li:20 * li + 20], tTa(c, li), wr_s[:, c, :], start=(c == 0), stop=(c == 7))
                        return ins
                    P.op("pe", mmr, [tT, wr_s] + ([tTxb] if nl == 5 else []), [psR])
                    R_ = lambda a, b_: rt[:, 0:nl, a:b_]
                    cmv = comb[:, 0:nl, :]
                    P.op("dve", lambda e: e.tensor_tensor(R_(0, 20), psR[:, 0:20 * nl].rearrange("p (t f) -> p t f", f=20), bc(br_s[:].unsqueeze(1), [128, nl, 20]), ALU.add), [psR, br_s], [rt])
                    P.op("dve", lambda e: e.tensor_reduce(rt[:, 0:nl, 20], R_(0, 4), AX.X, ALU.max), [rt], [rt])
                    P.op("dve", lambda e: e.tensor_tensor(R_(21, 25), R_(0, 4), bc(R_(20, 21), [128, nl, 4]), ALU.is_ge), [rt], [rt])
                    P.op("dve", lambda e: e.tensor_tensor(R_(25, 29), R_(0, 4), bc(R_(20, 21), [128, nl, 4]), ALU.subtract), [rt], [rt])
                    P.op("act", lambda e: e.activation(R_(25, 29), R_(25, 29), AF.Exp), [rt], [rt])
                    P.op("dve", lambda e: e.tensor_reduce(rt[:, 0:nl, 29], R_(25, 29), AX.X, ALU.add), [rt], [rt])
                    P.op("dve", lambda e: e.reciprocal(R_(30, 31), R_(29, 30)), [rt], [rt])
                    P.op("dve", lambda e: e.tensor_tensor(cmv.rearrange("p t (e g) -> p t e g", g=4), R_(4, 20).rearrange("p t (g e) -> p t e g", e=4), bc(R_(21, 25).unsqueeze(2), [128, nl, 4, 4]), ALU.mult), [rt], [comb])
                    P.op("dve", lambda e: e.tensor_reduce(R_(31, 35), cmv.rearrange("p t (e g) -> p t e g", g=4), AX.X, ALU.add), [comb, rt], [rt])
                    P.op("dve", lambda e: e.tensor_reduce(rt[:, 0:nl, 35], R_(31, 35), AX.X, ALU.max), [rt], [rt])
                    P.op("dve", lambda e: e.tensor_tensor(R_(36, 40), R_(31, 35), bc(R_(35, 36), [128, nl, 4]), ALU.is_ge), [rt], [rt])
                    P.op("dve", lambda e: e.scalar_tensor_tensor(R_(40, 44), R_(36, 40), -1e30, R_(31, 35), ALU.mult, ALU.add), [rt], [rt])
                    P.op("dve", lambda e: e.tensor_reduce(rt[:, 0:nl, 44], R_(40, 44), AX.X, ALU.max), [rt], [rt])
                    P.op("dve", lambda e: e.tensor_tensor(R_(45, 49), R_(40, 44), bc(R_(44, 45), [128, nl, 4]), ALU.is_ge), [rt], [rt])
                    P.op("dve", lambda e: e.tensor_tensor(R_(49, 50), R_(44, 45), R_(35, 36), ALU.subtract), [rt], [rt])
                    P.op("act", lambda e: e.activation(R_(50, 51), R_(49, 50), AF.Exp), [rt], [rt])
                    P.op("dve", lambda e: e.tensor_scalar(R_(51, 52), R_(50, 51), 1.0, None, ALU.add), [rt], [rt])
                    P.op("dve", lambda e: e.reciprocal(R_(52, 53), R_(51, 52)), [rt], [rt])
                    P.op("dve", lambda e: e.tensor_tensor(R_(51, 52), R_(50, 51), R_(52, 53), ALU.mult), [rt], [rt])
                    P.op("dve", lambda e: e.tensor_tensor(R_(53, 57), R_(36, 40), bc(R_(52, 53), [128, nl, 4]), ALU.mult), [rt], [rt])
                    P.op("dve", lambda e: e.tensor_tensor(R_(57, 61), R_(45, 49), bc(R_(51, 52), [128, nl, 4]), ALU.mult), [rt], [rt])
                    P.op("dve", lambda e: e.tensor_tensor(R_(53, 57), R_(53, 57), R_(57, 61), ALU.add), [rt], [rt])
                    P.op("dve", lambda e: e.tensor_tensor(R_(57, 61), R_(21, 25), bc(R_(30, 31), [128, nl, 4]), ALU.mult), [rt], [rt])
                    P.op("dve", lambda e: e.tensor_tensor(cmv.rearrange("p t (g e) -> p t g e", e=4), bc(R_(57, 61).unsqueeze(3), [128, nl, 4, 4]), bc(R_(53, 57).unsqueeze(2), [128, nl, 4, 4]), ALU.mult), [rt], [comb])
                head_router(nl)

                def down(ex, lis, wi, at, h2c=h2c):
                    for li in lis:
                        psD = psDs[dcount[0] % 2]
                        dcount[0] += 1
                        atx = actTx_ap[ex % 2]

                        def mmd(e, at=at, atx=atx, wi=wi, li=li, psD=psD):
                            ins = None
                            for half in range(2):
                                for f in range(2):
                                    src = at[:, f, li * 128:(li + 1) * 128] if li < 4 else atx[:, f, :]
                                    ins = e.matmul(psD[:, half * 512:(half + 1) * 512], src, wd[wi][:, f, half * 512:(half + 1) * 512], start=(f == 0), stop=(f == 1))
                            return ins
                        P.op("pe", mmd, [at if li < 4 else actTxb[ex % 2], wd[wi]], [psD])
                        P.op("dve", lambda e, li=li, ex=ex, psD=psD, hb=h2c[li]: e.scalar_tensor_tensor(hb[:], psD[:], comb[:, li, ex:ex + 1], hb[:], ALU.mult, ALU.add), [psD, comb, h2c[li]], [h2c[li]])

                prev = None
                for ex in range(NE):
                    wi = wcount % 2
                    wcount += 1
                    P.dma("sp", wg[wi][:].rearrange("p c f -> p (c f)"), sc_gate[ex], writes=[wg[wi]])
                    P.dma("sp", wu[wi][:].rearrange("p c f -> p (c f)"), sc_up[ex], writes=[wu[wi]])
                    P.dma("sp", wd[wi][:].rearrange("p c f -> p (c f)"), sc_down[ex], writes=[wd[wi]])
                    at = actT[ex % 2]
                    for f in range(2):
                        pg_, pu_ = psGU[gucount[0] % 3], psGU[(gucount[0] + 1) % 3]
                        gucount[0] += 2

                        def mmg2(e, pg_=pg_, wi=wi, f=f, n=n):
                            ins = None
                            for c in range(8):
                                ins = e.matmul(pg_[:, 0:n], wg[wi][:, c, f * 128:(f + 1) * 128], tT[:, c, 0:n], start=(c == 0), stop=(c == 7))
                            return ins
                        P.op("pe", mmg2, [wg[wi], tT], [pg_])

                        def mmu2(e, pu_=pu_, wi=wi, f=f, n=n):
                            ins = None
                            for c in range(8):
                                ins = e.matmul(pu_[:, 0:n], wu[wi][:, c, f * 128:(f + 1) * 128], tT[:, c, 0:n], start=(c == 0), stop=(c == 7))
                            return ins
                        P.op("pe", mmu2, [wu[wi], tT], [pu_])
                        sl_ = sil[f]
                        P.op("act", lambda e, pg_=pg_, sl_=sl_, n=n: e.activation(sl_[:, 0:n], pg_[:, 0:n], AF.Silu), [pg_], [sl_])
                        P.op("dve", lambda e, pu_=pu_, sl_=sl_, at=at, f=f, n=n: e.tensor_tensor(at[:, f, 0:n], pu_[:, 0:n], sl_[:, 0:n], ALU.mult), [pu_, sl_], [at])
                        if nl == 5:
                            def mmx(e, wi=wi, f=f):
                                ins = None
                                for c in range(8):
                                    e.matmul(psS5[:, 0:128], wg[wi][:, c, f * 128:(f + 1) * 128], tTx_ap[:, c, :], start=(c == 0), stop=(c == 7))
                                for c in range(8):
                                    ins = e.matmul(psS5[:, 128:256], wu[wi][:, c, f * 128:(f + 1) * 128], tTx_ap[:, c, :], start=(c == 0), stop=(c == 7))
                                return ins
                            P.op("pe", mmx, [wg[wi], wu[wi], tTxb], [psS5])
                            P.op("act", lambda e, f=f: e.activation(silx_ap[f], psS5[:, 0:128], AF.Silu), [psS5], [silxb[f]])
                            P.op("dve", lambda e, f=f, ex=ex: e.tensor_tensor(actTx_ap[ex % 2][:, f, :], psS5[:, 128:256], silx_ap[f], ALU.mult), [psS5, silxb[f]], [actTxb[ex % 2]])
                        run_slot(sched, slot[0])
                        slot[0] += 1
                        if prev is not None:
                            lis = list(range(nl))[(f * ((nl + 1) // 2)):((f + 1) * ((nl + 1) // 2))] if nl > 1 else ([0] if f == 0 else [])
                            down(prev[0], lis, prev[1], prev[2])
                    prev = (ex, wi, at)
                down(prev[0], list(range(nl)), prev[1], prev[2])
                run_rest(sched)
                for li, t in enumerate(tiles):
                    hb = h2c[li]
                    P.op("act", lambda e, hb=hb, li=li: e.activation(actT[0][:].rearrange("p a b -> p (a b)"), hb[:], AF.Square, accum_out=st3[:, 0, li:li + 1]), [hb], [actT[0], st3])
                def fin_norm(nl):
                    P.op("dve", lambda e: e.tensor_scalar(st3[:, 1, 0:nl], st3[:, 0, 0:nl], 1.0 / D, EPS, ALU.mult, ALU.add), [st3], [st3])
                    P.op("act", lambda e: e.activation(st3[:, 2, 0:nl], st3[:, 1, 0:nl], AF.Sqrt), [st3], [st3])
                    P.op("dve", lambda e: e.reciprocal(st3[:, 3, 0:nl], st3[:, 2, 0:nl]), [st3], [st3])
                fin_norm(nl)
                for li, t in enumerate(tiles):
                    hb = h2c[li]
                    P.op("dve", lambda e, hb=hb, li=li: e.scalar_tensor_tensor(hb[:], hb[:], st3[:, 3, li:li + 1], gfin_s[:], ALU.mult, ALU.mult), [hb, st3, gfin_s], [hb])
                    if t == 0:
                        P.dma("act", out[0:64, :], hb[64:128, :], reads=[hb], sigbuf=hb)
                    elif t == NT - 1:
                        P.dma("act", out[SEQ - 64:SEQ, :], hb[0:64, :], reads=[hb], sigbuf=hb)
                    else:
                        P.dma("act", out[128 * t - 64:128 * t + 64, :], hb[:], reads=[hb], sigbuf=hb)
            P.emit(final_waits=h2 + [h2x])
    return nc


def _host_consts():
    c = {}
    d = np.arange(128)
    inv_freq = (10000.0 ** (-(np.arange(0, 128, 2, dtype=np.float32)) / np.float32(128))).astype(np.float32)
    pos = (np.arange(LP, dtype=np.float32) - np.float32(48.0)).astype(np.float32)
    ang = (pos[None, :] * inv_freq[d % 64][:, None]).astype(np.float32)
    c["cosT"] = np.cos(ang).astype(np.float32)
    sn = np.sin(ang).astype(np.float32)
    sn[64:] = -sn[64:]
    c["sinT"] = sn
    gamma = 1.0 - 2.0 ** (-5.0 - np.arange(NH, dtype=np.float64))
    j = np.arange(128)[:, None]
    i = np.arange(128)[None, :]
    mask = np.zeros((128, NH, 128), np.float64)
    same = (j // 64) == (i // 64)
    causal_ab = (j < 64) & (i >= 64)
    for h in range(NH):
        m = np.where(same, gamma[h] ** np.abs(i - j), 0.0)
        m = np.where(causal_ab, gamma[h] ** (i - j), m)
        mask[:, h, :] = m * (128.0 ** -0.5)
    c["maskT"] = mask.reshape(128, NH * 128).astype(np.float32)
    r = np.arange(128)[:, None]
    c["qdec"] = (gamma[None, :] ** (r + 1)).astype(np.float32)
    c["kdec"] = ((gamma[None, :] ** (127 - r)) * (128.0 ** -0.5)).astype(np.float32)
    c["identf"] = np.eye(128, dtype=np.float32)
    return c


_NC_CACHE = {}


def kernel(x, meta_tokens, norm_mix_g, w_in, ssm_lambda_re, ssm_lambda_im, ssm_log_dt,
           ssm_b_re, ssm_b_im, ssm_c_re, ssm_c_im, ssm_d, w_glu, w_out, norm_ffn_g,
           w_router_group, b_router_group, w_router_expert, b_router_expert,
           w_gate, w_up, w_down, norm_final_g):
    f = lambda a: np.ascontiguousarray(np.asarray(a, dtype=np.float32))
    x = f(x)
    shared = dict(_host_consts())
    shared["meta"] = f(meta_tokens)
    shared["gmix"] = f(np.asarray(norm_mix_g)[0].reshape(8, 128).T)
    shared["gffn"] = f(np.asarray(norm_ffn_g)[0].reshape(8, 128).T)
    shared["gfin"] = f(np.broadcast_to(np.asarray(norm_final_g)[None, :], (128, D)))
    shared["w_in"] = f(np.asarray(w_in)[0])
    shared["w_out"] = f(np.asarray(w_out)[0])
    shared["w_glu"] = f(np.asarray(w_glu)[0])
    shared["w_gate"] = f(np.asarray(w_gate)[0])
    shared["w_up"] = f(np.asarray(w_up)[0])
    shared["w_down"] = f(np.asarray(w_down)[0])
    wre = np.asarray(w_router_expert)[0]
    shared["wr"] = f(np.concatenate([np.asarray(w_router_group)[0], wre.transpose(1, 0, 2).reshape(D, 16)], axis=1))
    brv = np.concatenate([np.asarray(b_router_group)[0], np.asarray(b_router_expert)[0].reshape(16)])
    shared["br"] = f(np.broadcast_to(brv[None, :], (128, 20)))
    def srow(a):
        return f(np.asarray(a).reshape(8, 2, 64).transpose(1, 2, 0).reshape(128, 8))
    shared["lre"] = srow(np.asarray(ssm_lambda_re)[0])
    shared["lim"] = srow(np.asarray(ssm_lambda_im)[0])
    shared["ldt"] = f(np.broadcast_to(np.asarray(ssm_log_dt)[0].reshape(8, 2)[None, :, :], (64, 8, 2)).transpose(2, 0, 1).reshape(128, 8))
    bre = np.asarray(ssm_b_re)[0]
    bim = np.asarray(ssm_b_im)[0]
    cre = np.asarray(ssm_c_re)[0]
    cim = np.asarray(ssm_c_im)[0]
    dsk = np.asarray(ssm_d)[0]
    Blh = np.zeros((128, 2, 4, 2, 128), np.float32)
    Clh = np.zeros((128, 8, 2, 128), np.float32)
    Dlh = np.zeros((128, 2, 128), np.float32)
    for g in range(16):
        k, q = g // 2, g % 2
        j, kk = k // 4, k % 4
        gl = g - 8 * j
        rows = slice(q * 64, q * 64 + 64)
        cls = slice(gl * 16, gl * 16 + 16)
        Blh[cls, j, kk, 0, rows] = bre[g].T
        Blh[cls, j, kk, 1, rows] = bim[g].T
        Clh[rows, k, 0, cls] = cre[g].T
        Clh[rows, k, 1, cls] = cim[g].T
        for h in range(16):
            Dlh[gl * 16 + h, j, gl * 16 + h] = dsk[g, h]
    shared["Bl"] = Blh.reshape(128, 2048)
    shared["Cl"] = Clh.reshape(128, 2048)
    shared["Dl"] = Dlh.reshape(128, 256)
    if "nc" not in _NC_CACHE:
        _NC_CACHE["nc"] = build_nc()
    nc = _NC_CACHE["nc"]
    in_maps = []
    for b in range(8):
        m = dict(shared)
        m["x"] = np.ascontiguousarray(x[b])
        in_maps.append(m)
    res = run_bass_kernel_spmd(nc, in_maps, core_ids=list(range(8)))
    return np.stack([r["out"] for r in res.results], axis=0).astype(np.float32)
```
